# Optimizing a Trainium2 kernel written in Bass

```python
import jax, jax.numpy as jnp
from jax import lax
import numpy as np


D_MODEL = 1024
BATCH = 4
SEQ = 8192
DEPTH = 4

N_MIXERS = 2
N_ATTN_LAYERS = (DEPTH + 1) // 2
N_POOL_LAYERS = DEPTH // 2
N_HEADS = 16
QK_NOPE_DIM = 64
QK_ROPE_DIM = 32
V_HEAD_DIM = 64
Q_LORA_RANK = 256
KV_LORA_RANK = 128
ROPE_THETA = 10000.0
Q_BLOCK = 128
POOL_WINDOWS = (2, 4, 8, 16)
N_POOL_GROUPS = 4
POOL_GROUP_DIM = D_MODEL // N_POOL_GROUPS
N_EXPERT_GROUPS = 8
EXPERTS_PER_GROUP = 8
N_EXPERTS = N_EXPERT_GROUPS * EXPERTS_PER_GROUP
TOP_K_IN_GROUP = 2
D_EXPERT = 256
EXPERT_BLOCK = 128
DEEPNORM_ALPHA = (2 * DEPTH) ** 0.25
DEEPNORM_BETA = (8 * DEPTH) ** -0.25
LN_EPS = 1e-5
RMS_EPS = 1e-6

kernel_name = 'mla_pool_hier_moe_deepnorm'


def layer_norm(x, g, b):
    xf = x.astype(jnp.float32)
    mu = jnp.mean(xf, axis=-1, keepdims=True)
    var = jnp.mean(jnp.square(xf - mu), axis=-1, keepdims=True)
    y = (xf - mu) * lax.rsqrt(var + LN_EPS) * g.astype(jnp.float32) + b.astype(jnp.float32)
    return y.astype(x.dtype)


def rms_norm(x, g):
    xf = x.astype(jnp.float32)
    y = xf * lax.rsqrt(jnp.mean(jnp.square(xf), axis=-1, keepdims=True) + RMS_EPS)
    return (y * g.astype(jnp.float32)).astype(x.dtype)


def rope_tables(positions):
    inv_freq = ROPE_THETA ** (-jnp.arange(0, QK_ROPE_DIM, 2, dtype=jnp.float32) / QK_ROPE_DIM)
    ang = positions.astype(jnp.float32)[..., None] * inv_freq
    return jnp.cos(ang), jnp.sin(ang)


def apply_rope(x, cos, sin):
    xf = x.astype(jnp.float32)
    half = xf.shape[-1] // 2
    x1, x2 = xf[..., :half], xf[..., half:]
    y = jnp.concatenate([x1 * cos - x2 * sin, x2 * cos + x1 * sin], axis=-1)
    return y.astype(x.dtype)


def mla_mixer(x, cos, sin, w_in, q_norm, w_uq, kv_norm, w_ukv, w_o):
    B, S, _ = x.shape
    lat = x @ w_in
    c_q = rms_norm(lat[..., :Q_LORA_RANK], q_norm)
    c_kv = rms_norm(lat[..., Q_LORA_RANK:Q_LORA_RANK + KV_LORA_RANK], kv_norm)
    k_rope = apply_rope(lat[..., Q_LORA_RANK + KV_LORA_RANK:], cos, sin)
    q = (c_q @ w_uq).reshape(B, S, N_HEADS, QK_NOPE_DIM + QK_ROPE_DIM)
    q_nope = q[..., :QK_NOPE_DIM]
    q_rope = apply_rope(q[..., QK_NOPE_DIM:], cos[:, :, None, :], sin[:, :, None, :])
    kv = (c_kv @ w_ukv).reshape(B, S, N_HEADS, QK_NOPE_DIM + V_HEAD_DIM)
    k_nope, v = kv[..., :QK_NOPE_DIM], kv[..., QK_NOPE_DIM:]
    scale = (QK_NOPE_DIM + QK_ROPE_DIM) ** -0.5
    outs = []
    for qs in range(0, S, Q_BLOCK):
        qe = qs + Q_BLOCK
        s = (jnp.einsum('bqhd,bkhd->bhqk', q_nope[:, qs:qe], k_nope[:, :qe],
                        preferred_element_type=jnp.float32)
             + jnp.einsum('bqhr,bkr->bhqk', q_rope[:, qs:qe], k_rope[:, :qe],
                          preferred_element_type=jnp.float32)) * scale
        causal = jnp.arange(qe)[None, :] <= jnp.arange(qs, qe)[:, None]
        s = jnp.where(causal, s, -jnp.inf)
        p = jax.nn.softmax(s, axis=-1).astype(v.dtype)
        outs.append(jnp.einsum('bhqk,bkhd->bqhd', p, v[:, :qe]))
    o = jnp.concatenate(outs, axis=1).reshape(B, S, N_HEADS * V_HEAD_DIM)
    return o @ w_o


def pool_mixer(x, w, b, scale):
    B, S, D = x.shape
    xf = x.astype(jnp.float32)
    cs = jnp.concatenate([jnp.zeros((B, 1, D), jnp.float32), jnp.cumsum(xf, axis=1)], axis=1)
    t = jnp.arange(S)
    means = []
    for gi, win in enumerate(POOL_WINDOWS):
        sl = slice(gi * POOL_GROUP_DIM, (gi + 1) * POOL_GROUP_DIM)
        hi = cs[:, 1:, sl]
        lo = cs[:, jnp.maximum(t + 1 - win, 0), sl]
        cnt = jnp.minimum(t + 1, win).astype(jnp.float32)[:, None]
        means.append((hi - lo) / cnt)
    pooled = jnp.concatenate(means, axis=-1) - xf
    y = jnp.einsum('bsgc,gcd->bsgd', pooled.reshape(B, S, N_POOL_GROUPS, POOL_GROUP_DIM),
                   w.astype(jnp.float32)).reshape(B, S, D) + b.astype(jnp.float32)
    return (y * scale.astype(jnp.float32)).astype(x.dtype)


def hier_moe(h, w_grp, b_grp, w_exp, b_exp, w1, w3, w2):
    B, S, D = h.shape
    tok = h.reshape(-1, D)
    N = tok.shape[0]
    hf = tok.astype(jnp.float32)
    g_logits = hf @ w_grp.astype(jnp.float32) + b_grp.astype(jnp.float32)
    g_prob = jax.nn.softmax(g_logits, axis=-1)
    g_sel = jnp.argmax(g_logits, axis=-1)
    g_gate = jnp.take_along_axis(g_prob, g_sel[:, None], axis=-1)
    e_logits = (hf @ w_exp.astype(jnp.float32) + b_exp.astype(jnp.float32)).reshape(N, N_EXPERT_GROUPS, EXPERTS_PER_GROUP)
    e_logits = jnp.take_along_axis(e_logits, g_sel[:, None, None], axis=1)[:, 0]
    e_prob = jax.nn.softmax(e_logits, axis=-1)
    top_p, top_i = lax.top_k(e_prob, TOP_K_IN_GROUP)
    gate = g_gate * top_p / jnp.sum(top_p, axis=-1, keepdims=True)
    expert = g_sel[:, None] * EXPERTS_PER_GROUP + top_i
    A = N * TOP_K_IN_GROUP
    flat_e = expert.reshape(-1).astype(jnp.int32)
    flat_tok = jnp.repeat(jnp.arange(N, dtype=jnp.int32), TOP_K_IN_GROUP)
    flat_gate = gate.reshape(-1)
    order = jnp.argsort(flat_e)
    e_sorted, tok_sorted, gate_sorted = flat_e[order], flat_tok[order], flat_gate[order]
    counts = jnp.bincount(flat_e, length=N_EXPERTS)
    padded = (counts + EXPERT_BLOCK - 1) // EXPERT_BLOCK * EXPERT_BLOCK
    start = jnp.cumsum(counts) - counts
    pend = jnp.cumsum(padded)
    pstart = pend - padded
    dest = pstart[e_sorted] + (jnp.arange(A, dtype=jnp.int32) - start[e_sorted])
    n_blocks = (A + N_EXPERTS * (EXPERT_BLOCK - 1) + EXPERT_BLOCK - 1) // EXPERT_BLOCK
    rows = jnp.zeros((n_blocks * EXPERT_BLOCK, D), tok.dtype).at[dest].set(tok[tok_sorted])
    blk_e = jnp.minimum(jnp.searchsorted(pend, jnp.arange(n_blocks) * EXPERT_BLOCK, side='right'), N_EXPERTS - 1)

    def expert_block(args):
        xb, e = args
        hb = jax.nn.silu(xb @ w1[e]) * (xb @ w3[e])
        return hb @ w2[e]

    y_rows = lax.map(expert_block, (rows.reshape(n_blocks, EXPERT_BLOCK, D), blk_e)).reshape(-1, D)
    y = y_rows[dest].astype(jnp.float32) * gate_sorted[:, None]
    out = jnp.zeros((N, D), jnp.float32).at[tok_sorted].add(y)
    return out.reshape(B, S, D).astype(h.dtype)


def setup_inputs(seed: int = 0) -> dict:
    key = jax.random.key(seed)
    ks = jax.random.split(key, 24)
    f32 = jnp.float32
    nrm = lambda k, shape, s: jax.random.normal(k, shape, f32) * s
    LAT = Q_LORA_RANK + KV_LORA_RANK + QK_ROPE_DIM
    return {
        'x': jax.random.normal(ks[0], (BATCH, SEQ, D_MODEL), f32),
        'positions': (jnp.arange(SEQ, dtype=jnp.int32)[None, :]
                      + jax.random.randint(ks[1], (BATCH, 1), 0, 4096, dtype=jnp.int32)),
        'ln_g': 1.0 + nrm(ks[2], (DEPTH, 2, D_MODEL), 0.02),
        'ln_b': nrm(ks[3], (DEPTH, 2, D_MODEL), 0.02),
        'mla_w_in': nrm(ks[4], (N_ATTN_LAYERS, D_MODEL, LAT), D_MODEL ** -0.5),
        'mla_q_norm': 1.0 + nrm(ks[5], (N_ATTN_LAYERS, Q_LORA_RANK), 0.02),
        'mla_w_uq': nrm(ks[6], (N_ATTN_LAYERS, Q_LORA_RANK, N_HEADS * (QK_NOPE_DIM + QK_ROPE_DIM)), Q_LORA_RANK ** -0.5),
        'mla_kv_norm': 1.0 + nrm(ks[7], (N_ATTN_LAYERS, KV_LORA_RANK), 0.02),
        'mla_w_ukv': nrm(ks[8], (N_ATTN_LAYERS, KV_LORA_RANK, N_HEADS * (QK_NOPE_DIM + V_HEAD_DIM)), KV_LORA_RANK ** -0.5),
        'mla_w_o': nrm(ks[9], (N_ATTN_LAYERS, N_HEADS * V_HEAD_DIM, D_MODEL), (N_HEADS * V_HEAD_DIM) ** -0.5 * DEEPNORM_BETA),
        'pool_w': nrm(ks[10], (N_POOL_LAYERS, N_POOL_GROUPS, POOL_GROUP_DIM, POOL_GROUP_DIM), POOL_GROUP_DIM ** -0.5 * DEEPNORM_BETA),
        'pool_b': nrm(ks[11], (N_POOL_LAYERS, D_MODEL), 0.02),
        'pool_scale': 1.0 + nrm(ks[12], (N_POOL_LAYERS, D_MODEL), 0.1),
        'moe_w_grp': nrm(ks[13], (DEPTH, D_MODEL, N_EXPERT_GROUPS), D_MODEL ** -0.5),
        'moe_b_grp': nrm(ks[14], (DEPTH, N_EXPERT_GROUPS), 0.01),
        'moe_w_exp': nrm(ks[15], (DEPTH, D_MODEL, N_EXPERTS), D_MODEL ** -0.5),
        'moe_b_exp': nrm(ks[16], (DEPTH, N_EXPERTS), 0.01),
        'moe_w1': nrm(ks[17], (DEPTH, N_EXPERTS, D_MODEL, D_EXPERT), D_MODEL ** -0.5),
        'moe_w3': nrm(ks[18], (DEPTH, N_EXPERTS, D_MODEL, D_EXPERT), D_MODEL ** -0.5),
        'moe_w2': nrm(ks[19], (DEPTH, N_EXPERTS, D_EXPERT, D_MODEL), D_EXPERT ** -0.5 * DEEPNORM_BETA),
    }


def reference(x, positions, ln_g, ln_b, mla_w_in, mla_q_norm, mla_w_uq, mla_kv_norm, mla_w_ukv, mla_w_o,
              pool_w, pool_b, pool_scale, moe_w_grp, moe_b_grp, moe_w_exp, moe_b_exp, moe_w1, moe_w3, moe_w2):
    cos, sin = rope_tables(positions)
    for i in range(DEPTH):
        j = i // N_MIXERS
        if i % N_MIXERS == 0:
            f = mla_mixer(x, cos, sin, mla_w_in[j], mla_q_norm[j], mla_w_uq[j],
                          mla_kv_norm[j], mla_w_ukv[j], mla_w_o[j])
        else:
            f = pool_mixer(x, pool_w[j], pool_b[j], pool_scale[j])
        x = layer_norm(DEEPNORM_ALPHA * x + f, ln_g[i, 0], ln_b[i, 0])
        f = hier_moe(x, moe_w_grp[i], moe_b_grp[i], moe_w_exp[i], moe_b_exp[i],
                     moe_w1[i], moe_w3[i], moe_w2[i])
        x = layer_norm(DEEPNORM_ALPHA * x + f, ln_g[i, 1], ln_b[i, 1])
    return x
```

```python
import contextlib
import numpy as np
import concourse.bass as bass
import concourse.mybir as mybir
from concourse.bass_utils import run_bass_kernel_spmd

F32 = mybir.dt.float32
BF16 = mybir.dt.bfloat16
I32 = mybir.dt.int32
AF = mybir.ActivationFunctionType
ALU = mybir.AluOpType
AX = mybir.AxisListType

ENGS = ("pe", "act", "dve", "pool", "sp")

D = 1024
NH = 16
DEPTH = 4
NE = 64
MODE = "seq"
NT = 64
TOK = NT * 128
CAP = 384
NKV = 64
BAND = 4


def set_mode(mode):
    global MODE, NT, TOK, CAP, BAND
    MODE = mode
    NT = 64 if mode == "seq" else 32
    TOK = NT * 128
    CAP = 384 if mode == "seq" else 256
    BAND = 4 if mode == "seq" else 8
DBG = {}
ALPHA = float((2 * DEPTH) ** 0.25)
SCALE = float(96 ** -0.5)
LN_EPS = 1e-5
RMS_EPS = 1e-6
TWO_PI = float(2 * np.pi)
C1 = 6.28125
C2 = float(2 * np.pi - 6.28125)


class _Op:
    __slots__ = ("eng", "fn", "reads", "writes", "dma", "idx", "waits", "sig", "count", "dsem", "dval", "deps", "wdeps")

    def __init__(self, eng, fn, reads, writes, dma):
        self.eng, self.fn, self.reads, self.writes, self.dma = eng, fn, tuple(reads), tuple(writes), dma
        self.sig = False
        self.count = 0
        self.dsem = None
        self.dval = 0
        self.waits = []
        self.deps = ()


class Sched:
    def __init__(self, nc, es):
        self.nc = nc
        self.ops = []
        self.cnt = {e: 0 for e in ENGS}
        self.stream_n = {}
        self.stream_sems = {}
        self.eng_sem = {e: es.enter_context(nc.semaphore("s_" + e)) for e in ENGS}
        self.sem_pool = [es.enter_context(nc.semaphore("dsem%d" % k)) for k in range(90)]
        self.es = es
        self.nblocks = 0

    def op(self, eng, fn, reads=(), writes=()):
        o = _Op(eng, fn, reads, writes, None)
        self.ops.append(o)
        return o

    def dma(self, eng, fn, reads=(), writes=(), stream="d", K=2):
        o = _Op(eng, fn, reads, writes, (eng + "_" + stream, K))
        self.ops.append(o)
        return o

    def capture(self, fn):
        n0 = len(self.ops)
        fn()
        got = self.ops[n0:]
        del self.ops[n0:]
        return got

    def emit_zipped(self, *lists):
        its = [list(l) for l in lists]
        total = sum(len(l) for l in its)
        pos = [0] * len(its)
        while sum(pos) < total:
            best, bi = None, -1
            for i, l in enumerate(its):
                if pos[i] < len(l):
                    frac = pos[i] / max(1, len(l))
                    if best is None or frac < best:
                        best, bi = frac, i
            self.ops.append(its[bi][pos[bi]])
            pos[bi] += 1

    def flush(self):
        nc = self.nc
        ops = self.ops
        self.ops = []
        if not ops:
            return
        for i, o in enumerate(ops):
            o.idx = i
        last_w, rd_cmp, rd_dma, hist = {}, {}, {}, {}
        for o in ops:
            deps = set()
            wdeps = set()
            for k in o.reads:
                if k in last_w:
                    wdeps.add(last_w[k])
            for k in o.writes:
                if k in last_w:
                    wdeps.add(last_w[k])
                d = rd_cmp.get(k)
                if d:
                    deps.update(d.values())
                l = rd_dma.get(k)
                if l:
                    deps.update(l)
            deps.update(wdeps)
            if o.dma is not None:
                st, K = o.dma
                n = self.stream_n.get(st, 0)
                self.stream_n[st] = n + 1
                h = hist.setdefault(st, {})
                if (n % K) in h:
                    deps.add(h[n % K])
                h[n % K] = o.idx
                o.dsem = (st, n % K)
                o.dval = 16 * (n // K + 1)
                if o.dsem not in self.stream_sems:
                    self.stream_sems[o.dsem] = self.sem_pool.pop()
            for k in o.reads:
                if o.dma is not None:
                    rd_dma.setdefault(k, []).append(o.idx)
                else:
                    rd_cmp.setdefault(k, {})[o.eng] = o.idx
            for k in o.writes:
                last_w[k] = o.idx
                rd_cmp[k] = {}
                rd_dma[k] = []
            deps.discard(o.idx)
            wdeps.discard(o.idx)
            o.deps = deps
            o.wdeps = wdeps
        fin = _Op("sp", None, (), (), None)
        fin.idx = len(ops)
        fin.deps = set(i for h in hist.values() for i in h.values())
        fin.wdeps = set()
        ops.append(fin)
        waited = {e: {f: -1 for f in ENGS} for e in ENGS}
        waited_dma = {e: set() for e in ENGS}
        for o in ops:
            best = {}
            for i in sorted(o.deps):
                p = ops[i]
                if p.dma is not None:
                    if i not in waited_dma[o.eng]:
                        waited_dma[o.eng].add(i)
                        o.waits.append(("dma", i))
                else:
                    if p.eng == o.eng and o.eng == "pe":
                        continue
                    if i > waited[o.eng][p.eng] and i > best.get(p.eng, -1):
                        best[p.eng] = i
            for f, i in best.items():
                waited[o.eng][f] = i
                ops[i].sig = True
                o.waits.append(("eng", i))
        for o in ops:
            if o.sig:
                self.cnt[o.eng] += 1
                o.count = self.cnt[o.eng]
        eng_sem, stream_sems = self.eng_sem, self.stream_sems
        self.nblocks += 1
        with nc.Block() as block:
            def run(ename):
                def body(eng):
                    for o in ops:
                        if o.eng != ename:
                            continue
                        for kind, i in o.waits:
                            p = ops[i]
                            if kind == "dma":
                                eng.wait_ge(stream_sems[p.dsem], p.dval)
                            else:
                                eng.wait_ge(eng_sem[p.eng], p.count)
                        if o.fn is None:
                            continue
                        ins = o.fn(eng)
                        if o.dma is not None:
                            ins.then_inc(stream_sems[o.dsem], 16)
                        elif o.sig:
                            ins.then_inc(eng_sem[o.eng], 1)
                return body
            block.tensor(run("pe"))
            block.scalar(run("act"))
            block.vector(run("dve"))
            block.gpsimd(run("pool"))
            block.sync(run("sp"))


class _View:
    def __init__(self, t, n, dims):
        self.t, self.n, self.dims = t, n, dims

    def __getitem__(self, key):
        v = self.t[:, 0:self.n]
        if self.dims is not None:
            v = v.rearrange("p (a b) -> p a b", a=self.dims[0])
        return v[key]


class KB:
    def __init__(self, nc, es):
        self.nc = nc
        self.es = es
        self.S = Sched(nc, es)
        self.uid = 0

    def sb(self, stack, name, shape, dt=F32):
        self.uid += 1
        return stack.enter_context(self.nc.sbuf_tensor("%s_%d" % (name, self.uid), shape, dt))

    def ps(self, stack, name, shape, dt=F32):
        self.uid += 1
        esz = 4 if dt == F32 else 2
        n = int(np.prod(shape[1:]))
        per_bank = 2048 // esz
        npad = (n + per_bank - 1) // per_bank * per_bank
        t = stack.enter_context(self.nc.psum_tensor("%s_%d" % (name, self.uid), [128, npad], dt))
        if len(shape) == 2:
            return _View(t, n, None)
        return _View(t, n, shape[1:])

    def mm(self, out, lhsT, rhs, start=True, stop=True, r=(), w=()):
        self.S.op("pe", lambda e: e.matmul(out, lhsT=lhsT, rhs=rhs, start=start, stop=stop), r, w)

    def tr(self, out, in_, ident, r=(), w=()):
        self.S.op("pe", lambda e: e.transpose(out=out, in_=in_, identity=ident), r, w)

    def act(self, out, in_, func, r=(), w=(), bias=None, scale=None, accum_out=None):
        kw = {}
        if bias is not None:
            kw["bias"] = bias
        if scale is not None:
            kw["scale"] = scale
        if accum_out is not None:
            kw["accum_out"] = accum_out
        self.S.op("act", lambda e: e.activation(out=out, in_=in_, func=func, **kw), r, w)

    def cp(self, eng, out, in_, r=(), w=()):
        if eng == "act":
            self.S.op("act", lambda e: e.copy(out=out, in_=in_), r, w)
        else:
            self.S.op(eng, lambda e: e.tensor_copy(out=out, in_=in_), r, w)

    def tt(self, eng, out, in0, in1, op, r=(), w=()):
        self.S.op(eng, lambda e: e.tensor_tensor(out=out, in0=in0, in1=in1, op=op), r, w)

    def ts(self, eng, out, in0, s1, op0, s2=None, op1=None, r=(), w=(), accum_out=None):
        kw = {}
        if op1 is not None:
            kw["op1"] = op1
        if accum_out is not None:
            kw["accum_out"] = accum_out
        self.S.op(eng, lambda e: e.tensor_scalar(out=out, in0=in0, scalar1=s1, scalar2=s2, op0=op0, **kw), r, w)

    def stt(self, eng, out, in0, scalar, in1, op0, op1, r=(), w=(), accum_out=None):
        kw = {}
        if accum_out is not None:
            kw["accum_out"] = accum_out
        self.S.op(eng, lambda e: e.scalar_tensor_tensor(out=out, in0=in0, scalar=scalar, in1=in1, op0=op0, op1=op1, **kw), r, w)

    def red(self, eng, out, in_, op, r=(), w=()):
        self.S.op(eng, lambda e: e.tensor_reduce(out=out, in_=in_, axis=AX.X, op=op), r, w)

    def recip(self, out, in_, r=(), w=()):
        self.S.op("dve", lambda e: e.reciprocal(out=out, in_=in_), r, w)

    def memset(self, eng, ap, val, w=()):
        self.S.op(eng, lambda e: e.memset(ap, val), (), w)

    def dma(self, eng, out, in_, r=(), w=(), stream="d", K=2):
        self.S.dma(eng, lambda e: e.dma_start(out=out, in_=in_), r, w, stream, K)

    def gather(self, out, src, idx, r=(), w=(), stream="g", K=2):
        self.S.dma("pool", lambda e: e.indirect_dma_start(out=out, out_offset=None, in_=src,
                                                          in_offset=bass.IndirectOffsetOnAxis(ap=idx, axis=0)), r, w, stream, K)

    def scatter(self, dst, in_, idx, r=(), w=(), stream="sc", K=2):
        self.S.dma("pool", lambda e: e.indirect_dma_start(out=dst, out_offset=bass.IndirectOffsetOnAxis(ap=idx, axis=0),
                                                          in_=in_, in_offset=None), r, w, stream, K)


def build_program(steps, want):
    nc = bass.Bass("TRN2", target_bir_lowering=False)
    es = contextlib.ExitStack()
    kb = KB(nc, es)
    S = kb.S
    layers = sorted(set(i for _, i in steps))
    mla_layers = sorted(set(i for k, i in steps if k in ("P1", "ATT", "ATTO")))
    pool_layers = sorted(set(i for k, i in steps if k == "POOL"))
    moe_layers = sorted(set(i for k, i in steps if k in ("ATT", "POOL", "EXPO")))

    def din(name, shape, dt=F32):
        return nc.dram_tensor(name, list(shape), dt, kind="ExternalInput").ap()

    def dout(name, shape, dt=F32):
        return nc.dram_tensor(name, list(shape), dt, kind="ExternalOutput").ap()

    def dint(name, shape, dt=F32):
        return nc.dram_tensor(name, list(shape), dt, kind="Internal").ap()

    x_in = din("x_in", [TOK, D])
    cst = din("cst", [128, 3 * 128])
    identf_d = din("identf", [128, 128])
    W = {}
    if mla_layers:
        pos_d = din("pos", [128, NT], I32)
        invf_d = din("invf", [128, 16])
    for i in mla_layers:
        W["w_in", i] = din("w_in%d" % i, [D, 416])
        W["w_uq", i] = din("w_uq%d" % i, [256, 1536])
        W["w_uqr", i] = din("w_uqr%d" % i, [256, 1536])
        W["w_ukv", i] = din("w_ukv%d" % i, [128, 2048])
        W["qn", i] = din("qn%d" % i, [256])
        W["kvn", i] = din("kvn%d" % i, [128])
        if ("ATT", i) in steps or ("ATTO", i) in steps:
            W["w_o", i] = din("w_o%d" % i, [D, D])
            if MODE == "pair":
                W["kv_gath", i] = din("kv_gath%d" % i, [2, 2, 128, TOK], BF16)
    if any(k in ("ATT", "ATTO") for k, _ in steps):
        amask_d = din("amask", [128, BAND, 512])
    for i in pool_layers:
        W["pw", i] = din("pw%d" % i, [4, 256, 256])
        W["pb", i] = din("pb%d" % i, [D])
        W["psc", i] = din("psc%d" % i, [D])
        if MODE == "pair":
            W["halo", i] = din("halo%d" % i, [4, 128, D])
    if pool_layers:
        pmat_d = din("pmat", [128, 4 * 128 * 2 + 8 * 4 * 128])
    for i in moe_layers:
        W["lng", i] = din("lng%d" % i, [2, D])
        W["lnb", i] = din("lnb%d" % i, [2, D])
        W["wr", i] = din("wr%d" % i, [D, 72])
        W["br", i] = din("br%d" % i, [72])
        W["w1", i] = din("w1_%d" % i, [NE, 128, 2048])
        W["w3", i] = din("w3_%d" % i, [NE, 128, 2048])
        W["w2", i] = din("w2_%d" % i, [NE, 256, D])
    if moe_layers:
        ecap_d = din("ecap", [128, NE])
    outs = {}
    if "kv_own" in want:
        outs["kv_own"] = dout("kv_own", [2, 128, TOK], BF16)
    else:
        outs["kv_own"] = dint("kv_own_i", [2, 128, TOK], BF16)
    if "x_out" in want:
        outs["x_out"] = dout("x_out", [TOK, D])
    if "dbg" in want:
        outs["dbg"] = dout("dbg", [TOK, 8])
    if moe_layers:
        x1_dram = dint("x1_dram", [TOK, D])
        rows_dram = din("rows_in", [NE * CAP, D], BF16) if "y_out" in want else dint("rows_dram", [NE * CAP, D], BF16)
        y_dram = dout("y_out", [NE * CAP, D]) if "y_out" in want else dint("y_dram", [NE * CAP, D])
        xa_dram = dint("xa_dram", [TOK, D])
        xb_dram = dint("xb_dram", [TOK, D])
    if any(k in ("ATT", "ATTO") for k, _ in steps):
        o_dram = dout("o_out", [D, TOK], BF16) if "o_out" in want else dint("o_dram", [D, TOK], BF16)

    g = contextlib.ExitStack()
    cst_bf = kb.sb(g, "cst_bf", [128, 3 * 128], BF16)
    ident_bf = cst_bf[:, 0:128]
    ones_bf = cst_bf[:, 128:256]
    ustrict_bf = cst_bf[:, 256:384]
    ident_f = kb.sb(g, "ident_f", [128, 128])
    kb.dma("pool", cst_bf[:], cst, w=["cst"], stream="c0", K=1)
    kb.dma("sp", ident_f[:], identf_d, w=["identf"], stream="c1", K=1)
    S.flush()

    if mla_layers:
        ccs = kb.sb(g, "ccs", [128, NT, 32])
        ssn = kb.sb(g, "ssn", [128, NT, 32])
        cosT = kb.sb(g, "cosT", [128, TOK], BF16)
        sinT = kb.sb(g, "sinT", [128, TOK], BF16)
        cqT = kb.sb(g, "cqT", [128, 2, TOK], BF16)
        with contextlib.ExitStack() as ph:
            posi = kb.sb(ph, "posi", [128, NT], I32)
            posf = kb.sb(ph, "posf", [128, NT])
            invf = kb.sb(ph, "invf", [128, 16])
            ang = kb.sb(ph, "ang", [128, NT, 16])
            yk = kb.sb(ph, "yk", [128, NT * 16])
            ki = kb.sb(ph, "ki", [128, NT * 16], I32)
            kf = kb.sb(ph, "kf", [128, NT * 16])
            rr = kb.sb(ph, "rr", [128, NT * 16])
            sres = kb.sb(ph, "sres", [128, NT, 16])
            cres = kb.sb(ph, "cres", [128, NT, 16])
            stg = kb.sb(ph, "stg", [128, 2, 96])
            ptab = kb.ps(ph, "ptab", [128, 4, 128])
            kb.dma("sp", posi[:], pos_d, w=["posi"], stream="c0", K=1)
            kb.dma("sp", invf[:], invf_d, w=["invf"], stream="c1", K=1)
            kb.cp("dve", posf[:], posi[:], r=["posi"], w=["posf"])
            for m in range(NT):
                kb.ts("dve", ang[:, m, :], invf[:], posf[:, m:m + 1], ALU.mult, r=["posf", "invf"], w=["ang"])
            angf = ang[:].rearrange("p m i -> p (m i)")

            kb.ts("dve", yk[:], angf, 1.0 / TWO_PI, ALU.mult, r=["ang"], w=["yk"])
            kb.cp("dve", ki[:], yk[:], r=["yk"], w=["ki"])
            kb.cp("dve", kf[:], ki[:], r=["ki"], w=["kf"])
            kb.stt("dve", rr[:], kf[:], -C1, angf, ALU.mult, ALU.add, r=["kf", "ang"], w=["rr"])
            kb.stt("dve", rr[:], kf[:], -C2, rr[:], ALU.mult, ALU.add, r=["kf", "rr"], w=["rr"])
            kb.ts("dve", rr[:], rr[:], 3.14159, ALU.min, -3.14159, ALU.max, r=["rr"], w=["rr"])
            kb.act(sres[:].rearrange("p m i -> p (m i)"), rr[:], AF.Sin, r=["rr"], w=["sres"])
            kb.stt("dve", yk[:], rr[:], -1.0, rr[:], ALU.mult, ALU.max, r=["rr"], w=["yk"])
            kb.ts("dve", yk[:], yk[:], -1.0, ALU.mult, float(np.pi / 2), ALU.add, r=["yk"], w=["yk"])
            kb.act(cres[:].rearrange("p m i -> p (m i)"), yk[:], AF.Sin, r=["yk"], w=["cres"])
            kb.cp("dve", ccs[:, :, 0:16], cres[:], r=["cres"], w=["ccs"])
            kb.cp("dve", ccs[:, :, 16:32], cres[:], r=["cres"], w=["ccs"])
            kb.ts("dve", ssn[:, :, 0:16], sres[:], -1.0, ALU.mult, r=["sres"], w=["ssn"])
            kb.cp("dve", ssn[:, :, 16:32], sres[:], r=["sres"], w=["ssn"])
            kb.memset("pool", stg[:], 0.0, w=["stg0", "stg1"])
            for tab, dstT, nm in ((ccs, cosT, "cosT"), (ssn, sinT, "sinT")):
                for m4 in range(NT // 4):
                    for j in range(4):
                        m = m4 * 4 + j
                        s = m % 2
                        kb.ts("dve", stg[:, s, 64:96], tab[:, m, :], SCALE, ALU.mult, r=["ccs", "ssn"], w=["stg%d" % s])
                        kb.tr(ptab[0:96, j, :], stg[:, s, :], ident_f[:], r=["stg%d" % s, "identf"], w=["ptab"])
                    kb.cp("act", dstT[64:96, m4 * 512:(m4 + 1) * 512],
                          ptab[64:96, :, :].rearrange("p a b -> p (a b)"), r=["ptab"], w=[nm])
            S.flush()

    def layernorm(st, uu, xo, lng_b, lnb_b, tag, ukey, wkey):
        junk, s12, sm = st["junk"], st["s12"], st["sm"]
        u = uu[:, 0, :]
        kb.act(uu[:, 1, :], u, AF.Square, r=[ukey], w=[tag + "usq"])
        kb.red("dve", s12[:], uu[:], ALU.add, r=[ukey, tag + "usq"], w=[tag + "sm"])
        kb.ts("dve", sm[:, 0:2], s12[:], 1.0 / D, ALU.mult, r=[tag + "sm"], w=[tag + "sm"])
        kb.tt("dve", sm[:, 2:3], sm[:, 0:1], sm[:, 0:1], ALU.mult, r=[tag + "sm"], w=[tag + "sm"])
        kb.stt("dve", sm[:, 3:4], sm[:, 1:2], LN_EPS, sm[:, 2:3], ALU.add, ALU.subtract, r=[tag + "sm"], w=[tag + "sm"])
        kb.act(sm[:, 4:5], sm[:, 3:4], AF.Ln, r=[tag + "sm"], w=[tag + "sm"])
        kb.act(sm[:, 5:6], sm[:, 4:5], AF.Exp, r=[tag + "sm"], w=[tag + "sm"], scale=-0.5)
        kb.stt("dve", sm[:, 6:7], sm[:, 0:1], -1.0, sm[:, 5:6], ALU.mult, ALU.mult, r=[tag + "sm"], w=[tag + "sm"])
        kb.act(junk[:], u, AF.Identity, r=[ukey, tag + "sm"], w=[tag + "junk"], bias=sm[:, 6:7], scale=sm[:, 5:6])
        kb.tt("dve", junk[:], junk[:], lng_b, ALU.mult, r=[tag + "junk", "lnw"], w=[tag + "junk"])
        kb.tt("dve", xo, junk[:], lnb_b, ALU.add, r=[tag + "junk", "lnw"], w=[wkey])

    def phase_P1(i, x_src):
        with contextlib.ExitStack() as ph:
            w_in_bf = kb.sb(ph, "w_in_bf", [128, 8, 416], BF16)
            qn_b = kb.sb(ph, "qn_b", [128, 256])
            kvn_b = kb.sb(ph, "kvn_b", [128, 128])
            xt = [kb.sb(ph, "xt", [128, D]) for _ in range(2)]
            xb = [kb.sb(ph, "xb", [128, D], BF16) for _ in range(2)]
            xT = [kb.sb(ph, "xT", [128, 8, 128], BF16) for _ in range(2)]
            kvt = [kb.sb(ph, "kvt", [128, 160]) for _ in range(2)]
            cqn = [kb.sb(ph, "cqn", [128, 256], BF16) for _ in range(2)]
            junk = kb.sb(ph, "junk", [128, 384])
            ssq = [kb.sb(ph, "ssq", [128, 8]) for _ in range(2)]
            t1 = [kb.sb(ph, "t1", [128, 32]) for _ in range(2)]
            t2 = [kb.sb(ph, "t2", [128, 32]) for _ in range(2)]
            pT = [kb.ps(ph, "pT", [128, 8, 128], BF16) for _ in range(2)]
            plat = [kb.ps(ph, "plat", [128, 416]) for _ in range(2)]
            pcq = kb.ps(ph, "pcq", [128, 2, 128], BF16)
            pkvT = kb.ps(ph, "pkvT", [128, 2, 128], BF16)
            kvtb = [kb.sb(ph, "kvtb", [128, 160], BF16) for _ in range(2)]
            kvTs = [kb.sb(ph, "kvTs", [128, 2, 128], BF16) for _ in range(2)]
            kb.memset("pool", kvTs[0][:], 0.0, w=["kvTs0"])
            kb.memset("pool", kvTs[1][:], 0.0, w=["kvTs1"])
            epst = kb.sb(ph, "epst", [128, 1])
            kb.memset("pool", epst[:], RMS_EPS, w=["epst"])
            kb.dma("pool", w_in_bf[:], W["w_in", i].rearrange("(c p) n -> p c n", p=128), w=["w_in"], stream="c0", K=1)
            kb.dma("sp", qn_b[:], W["qn", i].partition_broadcast(128), w=["qn"], stream="c1", K=1)
            kb.dma("sp", kvn_b[:], W["kvn", i].partition_broadcast(128), w=["kvn"], stream="c2", K=1)

            def load(m):
                s = m % 2
                kb.dma("sp", xt[s][:], x_src[m * 128:(m + 1) * 128, :], w=["xt%d" % s], stream="ldx", K=2)

            load(0)
            for m in range(NT):
                s = m % 2
                if m + 1 < NT:
                    load(m + 1)
                X, XB, XT, PT, PL = "xt%d" % s, "xb%d" % s, "xT%d" % s, "pT%d" % s, "plat%d" % s
                kb.cp("act", xb[s][:], xt[s][:], r=[X], w=[XB])
                for c in range(8):
                    kb.tr(pT[s][:, c, :], xb[s][:, c * 128:(c + 1) * 128], ident_bf, r=[XB, "cst"], w=[PT])
                kb.cp("dve", xT[s][:], pT[s][:], r=[PT], w=[XT])
                for c in range(8):
                    kb.mm(plat[s][:], xT[s][:, c, :], w_in_bf[:, c, :], start=(c == 0), stop=(c == 7), r=[XT, "w_in"], w=[PL])
                kb.act(junk[:, 0:384], plat[s][:, 0:384], AF.Square, r=[PL], w=["junk"])
                kb.red("dve", ssq[s][:, 4:7], junk[:, 0:384].rearrange("p (a b) -> p a b", a=3), ALU.add, r=["junk"], w=["ssq%d" % s])
                kb.tt("dve", ssq[s][:, 0:1], ssq[s][:, 4:5], ssq[s][:, 5:6], ALU.add, r=["ssq%d" % s], w=["ssq%d" % s])
                kb.cp("dve", ssq[s][:, 1:2], ssq[s][:, 6:7], r=["ssq%d" % s], w=["ssq%d" % s])
                kb.act(ssq[s][:, 2:3], ssq[s][:, 0:1], AF.Ln, r=["ssq%d" % s, "epst"], w=["ssq%d" % s], bias=epst[:, 0:1], scale=1.0 / 256)
                kb.act(ssq[s][:, 3:4], ssq[s][:, 1:2], AF.Ln, r=["ssq%d" % s, "epst"], w=["ssq%d" % s], bias=epst[:, 0:1], scale=1.0 / 128)
                kb.act(ssq[s][:, 2:4], ssq[s][:, 2:4], AF.Exp, r=["ssq%d" % s], w=["ssq%d" % s], scale=-0.5)
                kb.stt("dve", cqn[s][:], plat[s][:, 0:256], ssq[s][:, 2:3], qn_b[:], ALU.mult, ALU.mult,
                       r=[PL, "ssq%d" % s, "qn"], w=["cqn%d" % s])
                kb.stt("dve", kvt[s][:, 0:128], plat[s][:, 256:384], ssq[s][:, 3:4], kvn_b[:], ALU.mult, ALU.mult,
                       r=[PL, "ssq%d" % s, "kvn"], w=["kvt%d" % s])
                kb.tt("dve", t1[s][:], plat[s][:, 384:416], ccs[:, m, :], ALU.mult, r=[PL, "ccs", "junk"], w=["t1%d" % s])
                kb.tt("dve", t2[s][:, 0:16], plat[s][:, 400:416], ssn[:, m, 0:16], ALU.mult, r=[PL, "ssn"], w=["t2%d" % s])
                kb.tt("dve", t2[s][:, 16:32], plat[s][:, 384:400], ssn[:, m, 16:32], ALU.mult, r=[PL, "ssn"], w=["t2%d" % s])
                kb.tt("dve", kvt[s][:, 128:160], t1[s][:], t2[s][:], ALU.add, r=["t1%d" % s, "t2%d" % s], w=["kvt%d" % s])
                for cc in range(2):
                    kb.tr(pcq[:, cc, :], cqn[s][:, cc * 128:(cc + 1) * 128], ident_bf, r=["cqn%d" % s, "cst"], w=["pcq"])
                kb.cp("act", cqT[:, :, m * 128:(m + 1) * 128], pcq[:], r=["pcq"], w=["cqT"])
                kb.cp("dve", kvtb[s][:], kvt[s][:], r=["kvt%d" % s], w=["kvtb%d" % s])
                kb.tr(pkvT[:, 0, :], kvtb[s][:, 0:128], ident_bf, r=["kvtb%d" % s, "cst"], w=["pkvT"])
                kb.tr(pkvT[0:96, 1, :], kvtb[s][:, 64:160], ident_bf, r=["kvtb%d" % s, "cst"], w=["pkvT"])
                kb.cp("act", kvTs[s][:, 0, :], pkvT[:, 0, :], r=["pkvT"], w=["kvTs%d" % s])
                kb.cp("act", kvTs[s][64:96, 1, :], pkvT[64:96, 1, :], r=["pkvT"], w=["kvTs%d" % s])
                kb.dma("sp", outs["kv_own"][:, :, m * 128:(m + 1) * 128].rearrange("a p t -> p a t"), kvTs[s][:],
                       r=["kvTs%d" % s], w=[], stream="stkv", K=2)
            S.flush()

    def tail_setup(ph, i, which):
        T = {}
        T["lng_b"] = kb.sb(ph, "lng_b", [128, D])
        T["lnb_b"] = kb.sb(ph, "lnb_b", [128, D])
        kb.dma("sp", T["lng_b"][:], W["lng", i][which, :].partition_broadcast(128), w=["lnw"], stream="c0", K=1)
        kb.dma("sp", T["lnb_b"][:], W["lnb", i][which, :].partition_broadcast(128), w=["lnw"], stream="c1", K=1)
        return T

    def router_setup(ph, i):
        R = {}
        R["wr"] = kb.sb(ph, "wr", [128, 8, 72])
        R["br_b"] = kb.sb(ph, "br_b", [128, 72])
        R["ecap"] = kb.sb(ph, "ecap", [128, NE])
        R["cnt"] = kb.sb(ph, "cnt", [128, NE])
        kb.dma("sp", R["wr"][:], W["wr", i].rearrange("(c p) n -> p c n", p=128), w=["wr"], stream="c2", K=1)
        kb.dma("sp", R["br_b"][:], W["br", i].partition_broadcast(128), w=["br"], stream="c3", K=1)
        kb.dma("sp", R["ecap"][:], ecap_d, w=["ecap"], stream="c4", K=1)
        kb.memset("pool", R["cnt"][:], 0.0, w=["cnt"])
        R["x1T"] = kb.sb(ph, "x1T", [128, 8, 128])
        R["lg"] = kb.sb(ph, "lg", [128, 72])
        R["sm"] = kb.sb(ph, "rsm", [128, 16])
        R["goh"] = kb.sb(ph, "goh", [128, 8])
        R["gd"] = kb.sb(ph, "gd", [128, 8])
        R["tmp64"] = kb.sb(ph, "tmp64", [128, 8, 8])
        R["els"] = kb.sb(ph, "els", [128, 8])
        R["els2"] = kb.sb(ph, "els2", [128, 8])
        R["oh"] = [kb.sb(ph, "oh", [128, 8]) for _ in range(2)]
        R["oh64"] = [kb.sb(ph, "oh64", [128, 8, 8]) for _ in range(2)]
        R["A"] = kb.sb(ph, "A", [128, NE], BF16)
        R["pos"] = kb.sb(ph, "pos", [128, NE])
        R["destf"] = kb.sb(ph, "destf", [128, 2])
        R["xbf"] = [kb.sb(ph, "xbf", [128, D], BF16) for _ in range(2)]
        R["zt"] = kb.sb(ph, "zt", [128, 4096], BF16)
        kb.memset("pool", R["zt"][:], 0.0, w=["zt"])
        rz = rows_dram.rearrange("(n p f) d -> n p (f d)", p=128, f=4)
        for n_ in range(NE * CAP // 512):
            kb.dma("sp", rz[n_], R["zt"][:], r=["zt"], w=["rz"], stream="rz", K=4)
        R["pxT"] = kb.ps(ph, "pxT", [128, 8, 128])
        R["plg"] = kb.ps(ph, "plg", [128, 256])
        return R

    def router_tile(R, m, x1, x1key, RI, RG):
        s = m % 2
        for c in range(8):
            kb.tr(R["pxT"][:, c, :], x1[:, c * 128:(c + 1) * 128], ident_f[:], r=[x1key, "identf"], w=["pxT"])
        kb.cp("dve", R["x1T"][:, 0:4, :], R["pxT"][:, 0:4, :], r=["pxT"], w=["x1Ta"])
        kb.cp("act", R["x1T"][:, 4:8, :], R["pxT"][:, 4:8, :], r=["pxT"], w=["x1Tb"])
        for c in range(8):
            kb.mm(R["plg"][:, 0:72], R["x1T"][:, c, :], R["wr"][:, c, :], start=(c == 0), stop=(c == 7),
                  r=["x1Ta", "x1Tb", "wr"], w=["plg"])
        lg, sm = R["lg"], R["sm"]
        kb.tt("dve", lg[:], R["plg"][:, 0:72], R["br_b"][:], ALU.add, r=["plg", "br"], w=["lg"])
        kb.red("dve", sm[:, 0:1], lg[:, 0:8], ALU.max, r=["lg"], w=["rsm"])
        kb.ts("dve", R["goh"][:], lg[:, 0:8], sm[:, 0:1], ALU.is_equal, r=["lg", "rsm"], w=["goh"])
        kb.ts("dve", R["gd"][:], lg[:, 0:8], sm[:, 0:1], ALU.subtract, r=["lg", "rsm"], w=["gd"])
        kb.act(R["gd"][:], R["gd"][:], AF.Exp, r=["gd"], w=["gd"])
        kb.red("dve", sm[:, 1:2], R["gd"][:], ALU.add, r=["gd"], w=["rsm"])
        kb.recip(sm[:, 2:3], sm[:, 1:2], r=["rsm"], w=["rsm"])
        kb.tt("dve", R["tmp64"][:], lg[:, 8:72].rearrange("p (g j) -> p g j", g=8),
              R["goh"][:].unsqueeze(2).to_broadcast([128, 8, 8]), ALU.mult, r=["lg", "goh"], w=["tmp64"])
        kb.red("dve", R["els"][:], R["tmp64"][:].rearrange("p g j -> p j g"), ALU.add, r=["tmp64"], w=["els"])
        kb.red("dve", sm[:, 3:4], R["els"][:], ALU.max, r=["els"], w=["rsm"])
        kb.ts("dve", R["oh"][0][:], R["els"][:], sm[:, 3:4], ALU.is_equal, r=["els", "rsm"], w=["oh0"])
        kb.stt("dve", R["els2"][:], R["oh"][0][:], -1e30, R["els"][:], ALU.mult, ALU.add, r=["oh0", "els"], w=["els2"])
        kb.red("dve", sm[:, 4:5], R["els2"][:], ALU.max, r=["els2"], w=["rsm"])
        kb.ts("dve", R["oh"][1][:], R["els2"][:], sm[:, 4:5], ALU.is_equal, r=["els2", "rsm"], w=["oh1"])
        kb.tt("dve", sm[:, 5:6], sm[:, 4:5], sm[:, 3:4], ALU.subtract, r=["rsm"], w=["rsm"])
        kb.act(sm[:, 6:7], sm[:, 5:6], AF.Exp, r=["rsm"], w=["rsm"])
        kb.ts("dve", sm[:, 7:8], sm[:, 6:7], 1.0, ALU.add, r=["rsm"], w=["rsm"])
        kb.recip(sm[:, 8:9], sm[:, 7:8], r=["rsm"], w=["rsm"])
        kb.tt("dve", RG[:, m, 0:1], sm[:, 2:3], sm[:, 8:9], ALU.mult, r=["rsm"], w=["RG"])
        kb.tt("dve", RG[:, m, 1:2], RG[:, m, 0:1], sm[:, 6:7], ALU.mult, r=["rsm", "RG"], w=["RG"])
        for k in range(2):
            kb.tt("dve", R["oh64"][k][:], R["goh"][:].unsqueeze(2).to_broadcast([128, 8, 8]),
                  R["oh"][k][:].unsqueeze(1).to_broadcast([128, 8, 8]), ALU.mult, r=["goh", "oh%d" % k], w=["oh64%d" % k])
        kb.tt("dve", R["A"][:], R["oh64"][0][:].rearrange("p g j -> p (g j)"), R["oh64"][1][:].rearrange("p g j -> p (g j)"),
              ALU.add, r=["oh640", "oh641"], w=["A"])
        kb.mm(R["plg"][:, 128:192], ustrict_bf, R["A"][:], r=["A", "cst"], w=["ppos"])
        kb.mm(R["plg"][:, 192:256], ones_bf, R["A"][:], r=["A", "cst"], w=["ppos"])
        kb.tt("dve", R["pos"][:], R["plg"][:, 128:192], R["cnt"][:], ALU.add, r=["ppos", "cnt"], w=["pos"])
        kb.tt("dve", R["cnt"][:], R["cnt"][:], R["plg"][:, 192:256], ALU.add, r=["ppos", "cnt"], w=["cnt"])
        kb.ts("dve", R["pos"][:], R["pos"][:], float(CAP - 1), ALU.min, r=["pos"], w=["pos"])
        kb.tt("dve", R["pos"][:], R["pos"][:], R["ecap"][:], ALU.add, r=["pos", "ecap"], w=["pos"])
        for k in range(2):
            kb.tt("dve", R["oh64"][k][:].rearrange("p g j -> p (g j)"), R["oh64"][k][:].rearrange("p g j -> p (g j)"),
                  R["pos"][:], ALU.mult, r=["oh64%d" % k, "pos"], w=["oh64%d" % k])
            kb.red("dve", R["destf"][:, k:k + 1], R["oh64"][k][:].rearrange("p g j -> p (g j)"), ALU.add,
                   r=["oh64%d" % k], w=["destf"])
        kb.cp("dve", RI[:, m, :], R["destf"][:], r=["destf"], w=["RI"])
        kb.cp("act", R["xbf"][s][:], x1, r=[x1key], w=["xbf%d" % s])
        for k in range(2):
            kb.scatter(rows_dram, R["xbf"][s][:], RI[:, m, k:k + 1], r=["xbf%d" % s, "RI", "rz"], w=[], stream="sc%d" % k, K=2)

    def phase_experts(i):
        NSB = CAP // 128
        with contextlib.ExitStack() as ph:
            w1b = [kb.sb(ph, "w1b", [128, 8, 256], BF16) for _ in range(2)]
            w3b = [kb.sb(ph, "w3b", [128, 8, 256], BF16) for _ in range(2)]
            w2b = [kb.sb(ph, "w2b", [128, 2, D], BF16) for _ in range(2)]
            w2f = [kb.sb(ph, "w2f", [128, 2, D]) for _ in range(2)]
            rows = [kb.sb(ph, "rows", [128, NSB, D], BF16) for _ in range(2)]
            xgT = [kb.sb(ph, "xgT", [128, 8, 128], BF16) for _ in range(2)]
            sl = [kb.sb(ph, "sl", [128, 2, 128]) for _ in range(2)]
            hT = [kb.sb(ph, "hT", [128, 2, 128], BF16) for _ in range(2)]
            ys = [kb.sb(ph, "ys", [128, D]) for _ in range(3)]
            pT = [kb.ps(ph, "pT", [128, 8, 128], BF16) for _ in range(2)]
            phh = [kb.ps(ph, "phh", [128, 4, 128]) for _ in range(2)]
            py = [kb.ps(ph, "py", [128, D]) for _ in range(2)]

            def load(e):
                s = e % 2
                for hh_ in range(2):
                    kb.dma("pool", w1b[s][:, 4 * hh_:4 * hh_ + 4, :].rearrange("p c f -> p (c f)"), W["w1", i][e][:, 1024 * hh_:1024 * hh_ + 1024],
                           w=["w13a%d" % s], stream="w1%d" % hh_, K=2)
                    kb.dma("pool", w3b[s][:, 4 * hh_:4 * hh_ + 4, :].rearrange("p c f -> p (c f)"), W["w3", i][e][:, 1024 * hh_:1024 * hh_ + 1024],
                           w=["w13b%d" % s], stream="w3%d" % hh_, K=2)
                kb.dma("sp", w2f[s][:], W["w2", i][e].rearrange("(c p) n -> p c n", p=128), w=["w2f%d" % s], stream="w2", K=2)
                kb.cp("act", w2b[s][:], w2f[s][:], r=["w2f%d" % s], w=["w2b%d" % s])
                kb.dma("sp", rows[s][:], rows_dram[e * CAP:(e + 1) * CAP, :].rearrange("(sb p) d -> p sb d", p=128),
                       w=["rows%d" % s], stream="ldr", K=2)

            load(0)
            blk = 0
            NER = DBG.get('ne', NE)
            for e in range(NER):
                s = e % 2
                if e + 1 < NER:
                    load(e + 1)
                for sbk in range(NSB):
                    b2 = blk % 2
                    b3 = blk % 3
                    blk += 1
                    for c in range(8):
                        kb.tr(pT[b2][:, c, :], rows[s][:, sbk, c * 128:(c + 1) * 128], ident_bf, r=["rows%d" % s, "cst"], w=["pT%d" % b2])
                    kb.cp("dve" if b2 == 0 else "act", xgT[b2][:], pT[b2][:], r=["pT%d" % b2], w=["xgTa%d" % b2, "xgTb%d" % b2])
                    for fi in range(4):
                        for c in range(8):
                            wsrc = w1b[s] if fi < 2 else w3b[s]
                            kb.mm(phh[b2][:, fi, :], wsrc[:, c, (fi % 2) * 128:(fi % 2 + 1) * 128], xgT[b2][:, c, :], start=(c == 0), stop=(c == 7),
                                  r=["w13a%d" % s, "w13b%d" % s, "xgTa%d" % b2, "xgTb%d" % b2], w=["phh%d" % b2])
                    kb.act(sl[b2][:], phh[b2][:, 0:2, :], AF.Silu, r=["phh%d" % b2], w=["sl%d" % b2])
                    kb.tt("dve", hT[b2][:], sl[b2][:], phh[b2][:, 2:4, :], ALU.mult, r=["sl%d" % b2, "phh%d" % b2], w=["hT%d" % b2])
                    for half in range(2):
                        for fc in range(2):
                            kb.mm(py[b2][:, half * 512:(half + 1) * 512], hT[b2][:, fc, :],
                                  w2b[s][:, fc, half * 512:(half + 1) * 512], start=(fc == 0), stop=(fc == 1),
                                  r=["hT%d" % b2, "w2b%d" % s], w=["py%d" % b2])
                    kb.cp("act", ys[b3][:, 0:512], py[b2][:, 0:512], r=["py%d" % b2], w=["ysa%d" % b3])
                    kb.cp("dve", ys[b3][:, 512:1024], py[b2][:, 512:1024], r=["py%d" % b2], w=["ysb%d" % b3])
                    kb.dma("sp", y_dram[e * CAP + sbk * 128: e * CAP + (sbk + 1) * 128, :], ys[b3][:], r=["ysa%d" % b3, "ysb%d" % b3], w=[],
                           stream="sty", K=3)
            S.flush()

    def phase_combine(i, RI, RG, x_dst):
        NB = 4
        with contextlib.ExitStack() as ph:
            T = tail_setup(ph, i, 1)
            y0 = [kb.sb(ph, "y0", [128, D]) for _ in range(NB)]
            y1 = [kb.sb(ph, "y1", [128, D]) for _ in range(NB)]
            x1t = [kb.sb(ph, "x1t", [128, D]) for _ in range(NB)]
            u = [kb.sb(ph, "u", [128, 2, D]) for _ in range(NB)]
            st = [dict(junk=kb.sb(ph, "junk", [128, D]), s12=kb.sb(ph, "s12", [128, 2]), sm=kb.sb(ph, "sm", [128, 8])) for _ in range(NB)]

            def load(m):
                s = m % NB
                kb.gather(y0[s][:], y_dram, RI[:, m, 0:1], r=["RI", "y_dram"], w=["y0%d" % s], stream="g0", K=NB)
                kb.gather(y1[s][:], y_dram, RI[:, m, 1:2], r=["RI", "y_dram"], w=["y1%d" % s], stream="g1", K=NB)
                kb.dma("sp", x1t[s][:], x1_dram[m * 128:(m + 1) * 128, :], r=["x1_dram"], w=["x1t%d" % s], stream="ldxc", K=NB)

            def body(m):
                s = m % NB
                kb.act(u[s][:, 0, :], x1t[s][:], AF.Copy, r=["x1t%d" % s], w=["u%d" % s], scale=ALPHA)
                kb.stt("dve", u[s][:, 0, :], y0[s][:], RG[:, m, 0:1], u[s][:, 0, :], ALU.mult, ALU.add, r=["y0%d" % s, "u%d" % s, "RG"], w=["u%d" % s])
                kb.stt("dve", u[s][:, 0, :], y1[s][:], RG[:, m, 1:2], u[s][:, 0, :], ALU.mult, ALU.add, r=["y1%d" % s, "u%d" % s, "RG"], w=["u%d" % s])
                layernorm(st[s], u[s], st[s]["junk"][:], T["lng_b"][:], T["lnb_b"][:], "c%d" % s, "u%d" % s, "c%djunk" % s)
                kb.dma("sp", x_dst[m * 128:(m + 1) * 128, :], st[s]["junk"][:], r=["c%djunk" % s], w=[], stream="stx", K=NB)

            load(0)
            load(1)
            for m in range(0, NT, 2):
                for mm in (m + 2, m + 3):
                    if mm < NT:
                        load(mm)
                a = S.capture(lambda: body(m))
                b = S.capture(lambda: body(m + 1))
                S.emit_zipped(a, b)
            S.flush()

    def phase_att(i, x_src, RI, RG, attn_only=False):
        with contextlib.ExitStack() as pa:
            ckvT = kb.sb(pa, "ckvT", [128, 8192], BF16)
            KT = [kb.sb(pa, "KT", [128, 8192], BF16) for _ in range(2)]
            V = [kb.sb(pa, "V", [128, 64, 128], BF16) for _ in range(2)]
            wuq = kb.sb(pa, "wuq", [128, 2, 1536], BF16)
            wuqr = kb.sb(pa, "wuqr", [128, 2, 1536], BF16)
            wukv = kb.sb(pa, "wukv", [128, 2048], BF16)
            amask = kb.sb(pa, "amask", [128, BAND, 512], BF16)
            sk = DBG.get('skip', '')
            if 'q' not in sk:
                kb.dma("pool", wuq[:], W["w_uq", i].rearrange("(c p) n -> p c n", p=128), w=["wuq"], stream="c0", K=1)
                kb.dma("pool", wuqr[:], W["w_uqr", i].rearrange("(c p) n -> p c n", p=128), w=["wuqr"], stream="c1", K=1)
            if 'k' not in sk:
                for a_ in range(2):
                    kb.dma("pool", wukv[:, a_ * 1024:(a_ + 1) * 1024], W["w_ukv", i][:, a_ * 1024:(a_ + 1) * 1024], w=["wukv"], stream="c2", K=1)
            if 'a' not in sk:
                for a_ in range(BAND // 2):
                    kb.dma("pool", amask[:, 2 * a_:2 * a_ + 2, :], amask_d[:, 2 * a_:2 * a_ + 2, :], w=["amask"], stream="c3", K=1)
            if 'm' not in sk:
                kb.memset("pool", V[0][:, :, 64:128], 1.0, w=["V0"])
                kb.memset("pool", V[1][:, :, 64:128], 1.0, w=["V1"])
            if MODE == "pair":
                kvg = W["kv_gath", i]
                for r_ in range(2):
                    kb.dma("sp", ckvT[:].rearrange("p (lt r t) -> p lt r t", r=2, t=128)[:, :, r_, :],
                           kvg[r_, 0].rearrange("p (lt t) -> p lt t", t=128), w=["ckvT"], stream="ldkv%d" % r_, K=1)
                    for hb_ in range(2):
                        kb.dma("sp", KT[hb_][64:96, :].rearrange("p (lt r t) -> p lt r t", r=2, t=128)[:, :, r_, :],
                               kvg[r_, 1, 64:96, :].rearrange("p (lt t) -> p lt t", t=128), w=["KT%dr" % hb_], stream="ldkr%d%d" % (r_, hb_), K=1)
            else:
                kb.dma("sp", ckvT[:], outs["kv_own"][0], w=["ckvT"], stream="ldkv0", K=1)
                for hb_ in range(2):
                    kb.dma("sp", KT[hb_][64:96, :], outs["kv_own"][1, 64:96, :], w=["KT%dr" % hb_], stream="ldkr0%d" % hb_, K=1)
            S.flush()
            if DBG.get('stop') == 'P2a':
                return
            with contextlib.ExitStack() as ph:
                qT = [kb.sb(ph, "qT", [128, 512], BF16) for _ in range(2)]
                tq1 = kb.sb(ph, "tq1", [128, 512])
                tq2 = kb.sb(ph, "tq2", [128, 512])
                pt = [kb.sb(ph, "pt", [128, 512], BF16) for _ in range(5)]
                rl = kb.sb(ph, "rl", [128, 512])
                oT = [kb.sb(ph, "oT", [128, 512], BF16) for _ in range(2)]
                psS = [kb.ps(ph, "psS", [128, 512]) for _ in range(4)]
                po = [kb.ps(ph, "po", [128, 512]) for _ in range(2)]
                pqA = kb.ps(ph, "pqA", [128, 512])
                pqB = kb.ps(ph, "pqB", [128, 512])
                pkv = pqB
                def build_kv(h):
                    hb = h % 2
                    KTh, Vh = KT[hb], V[hb]
                    for kc in range(16):
                        kb.mm(pkv[0:64, :], wukv[:, h * 128:h * 128 + 64], ckvT[:, kc * 512:(kc + 1) * 512], r=["wukv", "ckvT"], w=["pqB"])
                        kb.cp("dve", KTh[0:64, kc * 512:(kc + 1) * 512], pkv[0:64, :], r=["pqB"], w=["KT%dn" % hb])
                    for k8 in range(8):
                        for j in range(8):
                            kt = k8 * 8 + j
                            kb.mm(pkv[:, j * 64:(j + 1) * 64], ckvT[:, kt * 128:(kt + 1) * 128], wukv[:, h * 128 + 64:h * 128 + 128],
                                  r=["wukv", "ckvT"], w=["pqB"])
                        kb.cp("dve", Vh[:, k8 * 8:(k8 + 1) * 8, 0:64], pkv[:].rearrange("p (a b) -> p a b", a=8),
                              r=["pqB"], w=["V%d" % hb])

                def build_q(h, G, qs):
                    for cc in range(2):
                        kb.mm(pqA[0:96, :], wuq[:, cc, h * 96:(h + 1) * 96], cqT[:, cc, G * 512:(G + 1) * 512], start=(cc == 0), stop=(cc == 1),
                              r=["wuq", "cqT"], w=["pqA"])
                    for cc in range(2):
                        kb.mm(pqB[0:96, :], wuqr[:, cc, h * 96:(h + 1) * 96], cqT[:, cc, G * 512:(G + 1) * 512], start=(cc == 0), stop=(cc == 1),
                              r=["wuqr", "cqT"], w=["pqB"])
                    kb.ts("dve", qT[qs][0:64, :], pqA[0:64, :], SCALE, ALU.mult, r=["pqA"], w=["qTn%d" % qs])
                    kb.tt("dve", tq1[64:96, :], pqB[64:96, :], sinT[64:96, G * 512:(G + 1) * 512], ALU.mult, r=["pqB", "sinT"], w=["tq1"])
                    kb.tt("dve", tq2[64:96, :], pqA[64:96, :], cosT[64:96, G * 512:(G + 1) * 512], ALU.mult, r=["pqA", "cosT"], w=["tq2"])
                    kb.tt("dve", qT[qs][64:96, :], tq1[64:96, :], tq2[64:96, :], ALU.add, r=["tq1", "tq2"], w=["qTr%d" % qs])

                groups = [(h, G) for h in range(NH) for G in range(NT // 4)]
                blk = 0
                build_kv(0)
                build_q(0, 0, 0)
                for gi_, (h, G) in enumerate(groups):
                    hb = h % 2
                    qs = gi_ % 2
                    KTh, Vh = KT[hb], V[hb]
                    nk = BAND * G + BAND
                    pos_ = po[qs]
                    bufs = [((blk + k) % 4, (blk + k) % 5) for k in range(nk)]
                    blk += nk

                    def qk(k):
                        b3, b4 = bufs[k]
                        kb.mm(psS[b3][:], KTh[0:96, k * 128:(k + 1) * 128], qT[qs][0:96, :],
                              r=["KT%dn" % hb, "KT%dr" % hb, "qTn%d" % qs, "qTr%d" % qs], w=["psS%d" % b3])

                    for k_ in range(min(3, nk)):
                        qk(k_)
                    for kt in range(nk):
                        b3, b4 = bufs[kt]
                        kb.act(pt[b4][:], psS[b3][:], AF.Exp, r=["psS%d" % b3], w=["pt%d" % b4])
                        if kt >= BAND * G:
                            kb.tt("pool", pt[b4][:], pt[b4][:], amask[:, kt - BAND * G, :], ALU.mult, r=["pt%d" % b4, "amask"], w=["pt%d" % b4])
                        kb.mm(pos_[:], Vh[:, kt, :], pt[b4][:], start=(kt == 0), stop=(kt == nk - 1),
                              r=["V%d" % hb, "pt%d" % b4], w=["po%d" % qs])
                        if kt + 3 < nk:
                            qk(kt + 3)
                        if kt == 0 and gi_ + 1 < len(groups):
                            nh, nG = groups[gi_ + 1]
                            if nh != h:
                                build_kv(nh)
                            build_q(nh, nG, (gi_ + 1) % 2)
                    kb.recip(rl[0:64, :], pos_[64:128, :], r=["po%d" % qs], w=["rl"])
                    kb.tt("dve", oT[qs][0:64, :], pos_[0:64, :], rl[0:64, :], ALU.mult, r=["po%d" % qs, "rl"], w=["oT%d" % qs])
                    kb.dma("sp", o_dram[h * 64:(h + 1) * 64, G * 512:(G + 1) * 512], oT[qs][0:64, :], r=["oT%d" % qs], w=[],
                           stream="sto", K=2)
                S.flush()
        if attn_only:
            return
        with contextlib.ExitStack() as ph:
            T = tail_setup(ph, i, 0)
            R = router_setup(ph, i)
            wo = kb.sb(ph, "wo", [128, 8, D], BF16)
            kb.dma("pool", wo[:], W["w_o", i].rearrange("(c p) n -> p c n", p=128), w=["wo"], stream="c5", K=1)
            oTt = [kb.sb(ph, "oTt", [128, 8, 512], BF16) for _ in range(2)]
            xt = [kb.sb(ph, "xt", [128, D]) for _ in range(2)]
            u = [kb.sb(ph, "u", [128, 2, D]) for _ in range(2)]
            x1 = [kb.sb(ph, "x1", [128, D]) for _ in range(2)]
            st = [dict(junk=kb.sb(ph, "junk", [128, D]), s12=kb.sb(ph, "s12", [128, 2]), sm=kb.sb(ph, "sm", [128, 8])) for _ in range(2)]
            pao = [kb.ps(ph, "pao", [128, D]) for _ in range(2)]
            o4 = o_dram.rearrange("(c p) t -> p c t", p=128)

            def load(m):
                s = m % 2
                if m % 4 == 0:
                    gq = (m // 4) % 2
                    kb.dma("sp", oTt[gq][:], o4[:, :, m * 128:(m + 4) * 128], r=["o_dram"], w=["oTt%d" % gq], stream="ldo", K=2)
                kb.dma("sp", xt[s][:], x_src[m * 128:(m + 1) * 128, :], w=["xt%d" % s], stream="ldx", K=2)

            def front(m):
                s = m % 2
                gq = (m // 4) % 2
                for half in range(2):
                    for c in range(8):
                        kb.mm(pao[s][:, half * 512:(half + 1) * 512], oTt[gq][:, c, (m % 4) * 128:(m % 4 + 1) * 128], wo[:, c, half * 512:(half + 1) * 512],
                              start=(c == 0), stop=(c == 7), r=["oTt%d" % gq, "wo"], w=["pao%d" % s])
                kb.stt("dve", u[s][:, 0, :], xt[s][:], ALPHA, pao[s][:], ALU.mult, ALU.add, r=["xt%d" % s, "pao%d" % s], w=["u%d" % s])
                layernorm(st[s], u[s], x1[s][:], T["lng_b"][:], T["lnb_b"][:], "a%d" % s, "u%d" % s, "x1%d" % s)
                kb.dma("sp", x1_dram[m * 128:(m + 1) * 128, :], x1[s][:], r=["x1%d" % s], w=[], stream="stx1", K=2)

            load(0)
            for m in range(NT):
                s = m % 2
                if m + 1 < NT:
                    load(m + 1)
                a = S.capture(lambda: front(m))
                if m >= 1:
                    b = S.capture(lambda: router_tile(R, m - 1, x1[1 - s][:], "x1%d" % (1 - s), RI, RG))
                    S.emit_zipped(a, b)
                else:
                    S.emit_zipped(a)
            router_tile(R, NT - 1, x1[(NT - 1) % 2][:], "x1%d" % ((NT - 1) % 2), RI, RG)
            S.flush()

    def phase_pool(i, x_src, RI, RG):
        with contextlib.ExitStack() as ph:
            T = tail_setup(ph, i, 0)
            R = router_setup(ph, i)
            NPM = 4 * 128 * 2 + 8 * 4 * 128
            pmat = kb.sb(ph, "pmat", [128, NPM], BF16)
            for a_ in range(NPM // 1024):
                kb.dma("pool", pmat[:, a_ * 1024:(a_ + 1) * 1024], pmat_d[:, a_ * 1024:(a_ + 1) * 1024], w=["pmat"], stream="c5", K=1)
            Mdiag = pmat[:, 0:512].rearrange("p (g t) -> p g t", g=4)
            Mfirst = pmat[:, 512:1024].rearrange("p (g t) -> p g t", g=4)
            Mhalo = pmat[:, 1024:NPM].rearrange("p (j g t) -> p j g t", j=8, g=4)
            pwb = kb.sb(ph, "pwb", [128, 8, 256], BF16)
            kb.dma("pool", pwb[:], W["pw", i].rearrange("g (cc p) d -> p (g cc) d", p=128), w=["pwb"], stream="c6", K=1)
            pb_b = kb.sb(ph, "pb_b", [128, D])
            psc_b = kb.sb(ph, "psc_b", [128, D])
            kb.dma("sp", pb_b[:], W["pb", i].partition_broadcast(128), w=["pb"], stream="c7", K=1)
            kb.dma("sp", psc_b[:], W["psc", i].partition_broadcast(128), w=["psc"], stream="c8", K=1)
            hb8 = [kb.sb(ph, "hb8", [128, D], BF16) for _ in range(2)]
            kb.memset("pool", hb8[0][:], 0.0, w=["hb80"])
            xt = [kb.sb(ph, "xt", [128, D]) for _ in range(2)]
            xb = [kb.sb(ph, "xb", [128, D], BF16) for _ in range(2)]
            plT = [kb.sb(ph, "plT", [128, 8, 128], BF16) for _ in range(2)]
            f = [kb.sb(ph, "f", [128, D]) for _ in range(2)]
            u = [kb.sb(ph, "u", [128, 2, D]) for _ in range(2)]
            x1 = [kb.sb(ph, "x1", [128, D]) for _ in range(2)]
            st = [dict(junk=kb.sb(ph, "junk", [128, D]), s12=kb.sb(ph, "s12", [128, 2]), sm=kb.sb(ph, "sm", [128, 8])) for _ in range(2)]
            pp = kb.ps(ph, "pp", [128, 8, 128])
            pyy = kb.ps(ph, "pyy", [128, D])

            def load(m):
                s = m % 2
                if m % 8 == 0:
                    hq = (m // 8) % 2
                    if MODE == "pair":
                        kb.dma("pool", hb8[hq][:], W["halo", i][m // 8], w=["hb8%d" % hq], stream="ldh", K=2)
                    else:
                        for j_ in range(8):
                            if m + j_ == 0:
                                continue
                            r0 = (m + j_) * 128 - 16
                            kb.dma("pool", hb8[hq][16 * j_:16 * j_ + 16, :], x_src[r0:r0 + 16, :], w=["hb8%d" % hq], stream="ldh%d" % j_, K=2)
                kb.dma("sp", xt[s][:], x_src[m * 128:(m + 1) * 128, :], w=["xt%d" % s], stream="ldx", K=2)

            def front(m):
                s = m % 2
                hq = (m // 8) % 2
                kb.cp("act", xb[s][:], xt[s][:], r=["xt%d" % s], w=["xb%d" % s])
                Md = Mfirst if m == 0 else Mdiag
                for fc in range(8):
                    gi = fc // 2
                    kb.mm(pp[:, fc, :], xb[s][:, fc * 128:(fc + 1) * 128], Md[:, gi, :], start=True, stop=False, r=["xb%d" % s, "pmat"], w=["pp"])
                    kb.mm(pp[:, fc, :], hb8[hq][:, fc * 128:(fc + 1) * 128], Mhalo[:, m % 8, gi, :], start=False, stop=True,
                          r=["hb8%d" % hq, "pmat"], w=["pp"])
                kb.cp("dve", plT[s][:], pp[:], r=["pp"], w=["plT%d" % s])
                for gi in range(4):
                    for cc in range(2):
                        kb.mm(pyy[:, gi * 256:(gi + 1) * 256], plT[s][:, 2 * gi + cc, :], pwb[:, 2 * gi + cc, :], start=(cc == 0), stop=(cc == 1),
                              r=["plT%d" % s, "pwb"], w=["pyy"])
                kb.tt("dve", f[s][:], pyy[:], pb_b[:], ALU.add, r=["pyy", "pb"], w=["f%d" % s])
                kb.tt("dve", f[s][:], f[s][:], psc_b[:], ALU.mult, r=["f%d" % s, "psc"], w=["f%d" % s])
                kb.stt("dve", u[s][:, 0, :], xt[s][:], ALPHA, f[s][:], ALU.mult, ALU.add, r=["xt%d" % s, "f%d" % s], w=["u%d" % s])
                layernorm(st[s], u[s], x1[s][:], T["lng_b"][:], T["lnb_b"][:], "a%d" % s, "u%d" % s, "x1%d" % s)
                kb.dma("sp", x1_dram[m * 128:(m + 1) * 128, :], x1[s][:], r=["x1%d" % s], w=[], stream="stx1", K=2)

            load(0)
            for m in range(NT):
                s = m % 2
                if m + 1 < NT:
                    load(m + 1)
                a = S.capture(lambda: front(m))
                if m >= 1:
                    b = S.capture(lambda: router_tile(R, m - 1, x1[1 - s][:], "x1%d" % (1 - s), RI, RG))
                    S.emit_zipped(a, b)
                else:
                    S.emit_zipped(a)
            router_tile(R, NT - 1, x1[(NT - 1) % 2][:], "x1%d" % ((NT - 1) % 2), RI, RG)
            S.flush()

    x_cur = x_in
    nstep = 0
    for kind, i in steps:
        if kind == "P1":
            phase_P1(i, x_cur)
        elif kind == "ATTO":
            phase_att(i, x_cur, None, None, attn_only=True)
        elif kind == "EXPO":
            phase_experts(i)
        else:
            nstep += 1
            last = (kind, i) == [s_ for s_ in steps if s_[0] not in ("P1", "ATTO", "EXPO")][-1]
            x_dst = outs["x_out"] if (last and "x_out" in outs) else (xa_dram if nstep % 2 else xb_dram)
            with contextlib.ExitStack() as pr:
                RI = kb.sb(pr, "RI", [128, NT, 2], I32)
                RG = kb.sb(pr, "RG", [128, NT, 2])
                if kind == "ATT":
                    phase_att(i, x_cur, RI, RG)
                else:
                    phase_pool(i, x_cur, RI, RG)
                if DBG.get('stop') != 'P3':
                    phase_experts(i)
                if DBG.get('stop') not in ('P3', 'EXP'):
                    phase_combine(i, RI, RG, x_dst)
            x_cur = x_dst
    g.close()
    es.close()
    return nc


def _consts():
    ident = np.eye(128, dtype=np.float32)
    ones = np.ones((128, 128), np.float32)
    ustrict = np.triu(np.ones((128, 128), np.float32), 1)
    return np.concatenate([ident, ones, ustrict], axis=1), ident


def _pool_mats(r):
    wins = (2, 4, 8, 16)
    Mdiag = np.zeros((128, 4, 128), np.float32)
    Mfirst = np.zeros((128, 4, 128), np.float32)
    Mh = np.zeros((16, 4, 128), np.float32)
    for gi, w in enumerate(wins):
        for t in range(128):
            for s_ in range(t - w + 1, t + 1):
                if s_ >= 0:
                    Mdiag[s_, gi, t] += 1.0 / w
                else:
                    Mh[16 + s_, gi, t] += 1.0 / w
            cnt = min(t + 1, w)
            for s_ in range(max(t - w + 1, 0), t + 1):
                Mfirst[s_, gi, t] += 1.0 / cnt
            Mdiag[t, gi, t] -= 1.0
            Mfirst[t, gi, t] -= 1.0
    if r == 1:
        Mfirst = Mdiag.copy()
    Mhalo = np.zeros((128, 8, 4, 128), np.float32)
    for j in range(8):
        Mhalo[16 * j:16 * j + 16, j] = Mh
    return np.concatenate([Mdiag.reshape(128, -1), Mfirst.reshape(128, -1), Mhalo.reshape(128, -1)], axis=1)


def _amask_seq():
    m = np.zeros((128, 4, 512), np.float32)
    kp = np.arange(128)[:, None]
    qp = np.arange(128)[None, :]
    for kk in range(4):
        for j in range(4):
            if kk < j:
                m[:, kk, j * 128:(j + 1) * 128] = 1.0
            elif kk == j:
                m[:, kk, j * 128:(j + 1) * 128] = (kp <= qp).astype(np.float32)
    return m


def _amask(r):
    m = np.zeros((128, 8, 512), np.float32)
    kp = np.arange(128)[:, None]
    for kk in range(8):
        for j in range(4):
            qp = np.arange(128)[None, :]
            d = 2 * j + r
            if kk < d:
                blk = np.ones((128, 128), np.float32)
            elif kk == d:
                blk = (kp <= qp).astype(np.float32)
            else:
                blk = np.zeros((128, 128), np.float32)
            m[:, kk, j * 128:(j + 1) * 128] = blk
    return m


def _pmajor(w):
    w = np.asarray(w, np.float32)
    return np.ascontiguousarray(w.reshape(NE, 8, 128, 256).transpose(0, 2, 1, 3)).reshape(NE, 128, 2048)


_PROG_CACHE = {}


def _get_prog(steps, want):
    key = (tuple(steps), tuple(sorted(want)))
    if key not in _PROG_CACHE:
        _PROG_CACHE[key] = build_program(list(steps), set(want))
    return _PROG_CACHE[key]


def _own_rows(a, b, r):
    t = a[b].reshape(64, 128, *a.shape[2:])
    return np.ascontiguousarray(t[r::2].reshape(TOK, *a.shape[2:]))


def kernel_pair(x, positions, ln_g, ln_b, mla_w_in, mla_q_norm, mla_w_uq, mla_kv_norm, mla_w_ukv, mla_w_o,
           pool_w, pool_b, pool_scale, moe_w_grp, moe_b_grp, moe_w_exp, moe_b_exp, moe_w1, moe_w3, moe_w2, _nlayers=DEPTH):
    set_mode("pair")
    f32 = np.float32
    x = np.asarray(x, f32)
    positions = np.asarray(positions, np.int32)
    cst, ident = _consts()
    invf = (10000.0 ** (-np.arange(0, 32, 2, dtype=f32) / f32(32))).astype(f32)
    invf_b = np.ascontiguousarray(np.broadcast_to(invf[None, :], (128, 16)))
    ecap = np.ascontiguousarray(np.broadcast_to((np.arange(NE, dtype=f32) * CAP)[None, :], (128, NE)))
    cores = [(b, r) for b in range(4) for r in range(2)]

    def common(b, r):
        return {"cst": cst, "identf": ident}

    def mla_w(j, att):
        wuq = np.asarray(mla_w_uq[j], f32)
        wr_ = wuq.reshape(256, NH, 96).copy()
        wr_[:, :, 64:80] = wuq.reshape(256, NH, 96)[:, :, 80:96]
        wr_[:, :, 80:96] = wuq.reshape(256, NH, 96)[:, :, 64:80]
        i = 2 * j
        d = {"w_in%d" % i: np.asarray(mla_w_in[j], f32), "w_uq%d" % i: wuq, "w_uqr%d" % i: wr_.reshape(256, 1536),
             "w_ukv%d" % i: np.asarray(mla_w_ukv[j], f32), "qn%d" % i: np.asarray(mla_q_norm[j], f32),
             "kvn%d" % i: np.asarray(mla_kv_norm[j], f32)}
        if att:
            d["w_o%d" % i] = np.asarray(mla_w_o[j], f32)
        return d

    def moe_w(i):
        return {"lng%d" % i: np.asarray(ln_g[i], f32), "lnb%d" % i: np.asarray(ln_b[i], f32),
                "wr%d" % i: np.ascontiguousarray(np.concatenate([moe_w_grp[i], moe_w_exp[i]], axis=1).astype(f32)),
                "br%d" % i: np.concatenate([moe_b_grp[i], moe_b_exp[i]]).astype(f32),
                "w1_%d" % i: np.asarray(moe_w1[i], f32), "w3_%d" % i: np.asarray(moe_w3[i], f32), "w2_%d" % i: np.asarray(moe_w2[i], f32),
                "ecap": ecap}

    def pos_in(b, r):
        p = _own_rows(positions[:, :, None], b, r)[:, 0]
        return {"pos": np.ascontiguousarray(p.reshape(NT, 128).T), "invf": invf_b}

    def run(steps, want, maps):
        nc = _get_prog(steps, want)
        res = run_bass_kernel_spmd(nc, maps, core_ids=list(range(8)))
        return res.results

    xs = [_own_rows(x, b, r) for (b, r) in cores]
    for i in range(_nlayers):
        j = i // 2
        if i % 2 == 0:
            maps = []
            for ci, (b, r) in enumerate(cores):
                d = {"x_in": xs[ci]}
                d.update(common(b, r)); d.update(pos_in(b, r)); d.update(mla_w(j, False))
                maps.append(d)
            res = run((("P1", i),), ("kv_own",), maps)
            kvo = [np.asarray(res[ci]["kv_own"]) for ci in range(8)]
            maps = []
            for ci, (b, r) in enumerate(cores):
                d = {"x_in": xs[ci]}
                d.update(common(b, r)); d.update(pos_in(b, r)); d.update(mla_w(j, True)); d.update(moe_w(i))
                d["kv_gath%d" % i] = np.stack([kvo[2 * b], kvo[2 * b + 1]], axis=0)
                d["amask"] = _amask(r)
                maps.append(d)
            res = run((("P1", i), ("ATT", i)), ("x_out",), maps)
        else:
            maps = []
            for ci, (b, r) in enumerate(cores):
                d = {"x_in": xs[ci]}
                d.update(common(b, r)); d.update(moe_w(i))
                d["pw%d" % i] = np.asarray(pool_w[j], f32)
                d["pb%d" % i] = np.asarray(pool_b[j], f32)
                d["psc%d" % i] = np.asarray(pool_scale[j], f32)
                d["pmat"] = _pool_mats(r)
                part = xs[2 * b + (1 - r)].reshape(NT, 128, D)[:, 112:128, :]
                halo = np.zeros((NT, 16, D), f32)
                if r == 1:
                    halo[:] = part
                else:
                    halo[1:] = part[:-1]
                d["halo%d" % i] = np.ascontiguousarray(halo.reshape(4, 128, D))
                maps.append(d)
            res = run((("POOL", i),), ("x_out",), maps)
        xs = [np.asarray(res[ci]["x_out"]) for ci in range(8)]
    out = np.zeros((4, 64, 128, D), f32)
    for ci, (b, r) in enumerate(cores):
        out[b, r::2] = xs[ci].reshape(NT, 128, D)
    return out.reshape(4, 8192, D)


def kernel(x, positions, ln_g, ln_b, mla_w_in, mla_q_norm, mla_w_uq, mla_kv_norm, mla_w_ukv, mla_w_o,
           pool_w, pool_b, pool_scale, moe_w_grp, moe_b_grp, moe_w_exp, moe_b_exp, moe_w1, moe_w3, moe_w2, _nlayers=DEPTH):
    set_mode("seq")
    f32 = np.float32
    x = np.asarray(x, f32)
    positions = np.asarray(positions, np.int32)
    cst, ident = _consts()
    invf = (10000.0 ** (-np.arange(0, 32, 2, dtype=f32) / f32(32))).astype(f32)
    invf_b = np.ascontiguousarray(np.broadcast_to(invf[None, :], (128, 16)))
    ecap = np.ascontiguousarray(np.broadcast_to((np.arange(NE, dtype=f32) * CAP)[None, :], (128, NE)))
    shared = {"cst": cst, "identf": ident, "invf": invf_b, "ecap": ecap, "amask": _amask_seq(), "pmat": _pool_mats(0)}
    steps = []
    for i in range(_nlayers):
        j = i // 2
        if i % 2 == 0:
            steps += [("P1", i), ("ATT", i)]
            wuq = np.asarray(mla_w_uq[j], f32)
            wr_ = wuq.reshape(256, NH, 96).copy()
            wr_[:, :, 64:80] = wuq.reshape(256, NH, 96)[:, :, 80:96]
            wr_[:, :, 80:96] = wuq.reshape(256, NH, 96)[:, :, 64:80]
            shared.update({"w_in%d" % i: np.asarray(mla_w_in[j], f32), "w_uq%d" % i: wuq, "w_uqr%d" % i: wr_.reshape(256, 1536),
                           "w_ukv%d" % i: np.asarray(mla_w_ukv[j], f32), "qn%d" % i: np.asarray(mla_q_norm[j], f32),
                           "kvn%d" % i: np.asarray(mla_kv_norm[j], f32), "w_o%d" % i: np.asarray(mla_w_o[j], f32)})
        else:
            steps += [("POOL", i)]
            shared.update({"pw%d" % i: np.asarray(pool_w[j], f32), "pb%d" % i: np.asarray(pool_b[j], f32),
                           "psc%d" % i: np.asarray(pool_scale[j], f32)})
        shared.update({"lng%d" % i: np.asarray(ln_g[i], f32), "lnb%d" % i: np.asarray(ln_b[i], f32),
                       "wr%d" % i: np.ascontiguousarray(np.concatenate([moe_w_grp[i], moe_w_exp[i]], axis=1).astype(f32)),
                       "br%d" % i: np.concatenate([moe_b_grp[i], moe_b_exp[i]]).astype(f32),
                       "w1_%d" % i: _pmajor(moe_w1[i]), "w3_%d" % i: _pmajor(moe_w3[i]),
                       "w2_%d" % i: np.asarray(moe_w2[i], f32)})
    if _nlayers < 2:
        shared.pop("pmat")
    maps = []
    for c in range(8):
        b = c % 4
        d = dict(shared)
        d["x_in"] = np.ascontiguousarray(x[b])
        d["pos"] = np.ascontiguousarray(positions[b].reshape(NT, 128).T)
        maps.append(d)
    nc = _get_prog(tuple(steps), ("x_out",))
    res = run_bass_kernel_spmd(nc, maps, core_ids=list(range(8)))
    return np.stack([np.asarray(res.results[b]["x_out"]) for b in range(4)], axis=0)
```

```python
import contextlib
import numpy as np
import concourse.bass as bass
import concourse.mybir as mybir
from concourse.bass_utils import run_bass_kernel_spmd

F32 = mybir.dt.float32
BF16 = mybir.dt.bfloat16
I32 = mybir.dt.int32
AF = mybir.ActivationFunctionType
ALU = mybir.AluOpType
AX = mybir.AxisListType

ENGS = ("pe", "act", "dve", "pool", "sp")

D = 1024
NH = 16
DEPTH = 4
NE = 64
MODE = "seq"
NT = 64
TOK = NT * 128
CAP = 384
NKV = 64
BAND = 4


def set_mode(mode):
    global MODE, NT, TOK, CAP, BAND
    MODE = mode
    NT = 64 if mode == "seq" else 32
    TOK = NT * 128
    CAP = 384 if mode == "seq" else 256
    BAND = 4 if mode == "seq" else 8
DBG = {}
ALPHA = float((2 * DEPTH) ** 0.25)
SCALE = float(96 ** -0.5)
LN_EPS = 1e-5
RMS_EPS = 1e-6
TWO_PI = float(2 * np.pi)
C1 = 6.28125
C2 = float(2 * np.pi - 6.28125)


class _Op:
    __slots__ = ("eng", "fn", "reads", "writes", "dma", "idx", "waits", "sig", "count", "dsem", "dval", "deps", "wdeps")

    def __init__(self, eng, fn, reads, writes, dma):
        self.eng, self.fn, self.reads, self.writes, self.dma = eng, fn, tuple(reads), tuple(writes), dma
        self.sig = False
        self.count = 0
        self.dsem = None
        self.dval = 0
        self.waits = []
        self.deps = ()


class Sched:
    def __init__(self, nc, es):
        self.nc = nc
        self.ops = []
        self.cnt = {e: 0 for e in ENGS}
        self.stream_n = {}
        self.stream_sems = {}
        self.eng_sem = {e: es.enter_context(nc.semaphore("s_" + e)) for e in ENGS}
        self.sem_pool = [es.enter_context(nc.semaphore("dsem%d" % k)) for k in range(90)]
        self.es = es
        self.nblocks = 0

    def op(self, eng, fn, reads=(), writes=()):
        o = _Op(eng, fn, reads, writes, None)
        self.ops.append(o)
        return o

    def dma(self, eng, fn, reads=(), writes=(), stream="d", K=2):
        o = _Op(eng, fn, reads, writes, (eng + "_" + stream, K))
        self.ops.append(o)
        return o

    def capture(self, fn):
        n0 = len(self.ops)
        fn()
        got = self.ops[n0:]
        del self.ops[n0:]
        return got

    def emit_zipped(self, *lists):
        its = [list(l) for l in lists]
        total = sum(len(l) for l in its)
        pos = [0] * len(its)
        while sum(pos) < total:
            best, bi = None, -1
            for i, l in enumerate(its):
                if pos[i] < len(l):
                    frac = pos[i] / max(1, len(l))
                    if best is None or frac < best:
                        best, bi = frac, i
            self.ops.append(its[bi][pos[bi]])
            pos[bi] += 1

    def flush(self):
        nc = self.nc
        ops = self.ops
        self.ops = []
        if not ops:
            return
        for i, o in enumerate(ops):
            o.idx = i
        last_w, rd_cmp, rd_dma, hist = {}, {}, {}, {}
        for o in ops:
            deps = set()
            wdeps = set()
            for k in o.reads:
                if k in last_w:
                    wdeps.add(last_w[k])
            for k in o.writes:
                if k in last_w:
                    wdeps.add(last_w[k])
                d = rd_cmp.get(k)
                if d:
                    deps.update(d.values())
                l = rd_dma.get(k)
                if l:
                    deps.update(l)
            deps.update(wdeps)
            if o.dma is not None:
                st, K = o.dma
                n = self.stream_n.get(st, 0)
                self.stream_n[st] = n + 1
                h = hist.setdefault(st, {})
                if (n % K) in h:
                    deps.add(h[n % K])
                h[n % K] = o.idx
                o.dsem = (st, n % K)
                o.dval = 16 * (n // K + 1)
                if o.dsem not in self.stream_sems:
                    self.stream_sems[o.dsem] = self.sem_pool.pop()
            for k in o.reads:
                if o.dma is not None:
                    rd_dma.setdefault(k, []).append(o.idx)
                else:
                    rd_cmp.setdefault(k, {})[o.eng] = o.idx
            for k in o.writes:
                last_w[k] = o.idx
                rd_cmp[k] = {}
                rd_dma[k] = []
            deps.discard(o.idx)
            wdeps.discard(o.idx)
            o.deps = deps
            o.wdeps = wdeps
        fin = _Op("sp", None, (), (), None)
        fin.idx = len(ops)
        fin.deps = set(i for h in hist.values() for i in h.values())
        fin.wdeps = set()
        ops.append(fin)
        waited = {e: {f: -1 for f in ENGS} for e in ENGS}
        waited_dma = {e: set() for e in ENGS}
        for o in ops:
            best = {}
            for i in sorted(o.deps):
                p = ops[i]
                if p.dma is not None:
                    if i not in waited_dma[o.eng]:
                        waited_dma[o.eng].add(i)
                        o.waits.append(("dma", i))
                else:
                    if p.eng == o.eng and o.eng == "pe":
                        continue
                    if i > waited[o.eng][p.eng] and i > best.get(p.eng, -1):
                        best[p.eng] = i
            for f, i in best.items():
                waited[o.eng][f] = i
                ops[i].sig = True
                o.waits.append(("eng", i))
        for o in ops:
            if o.sig:
                self.cnt[o.eng] += 1
                o.count = self.cnt[o.eng]
        eng_sem, stream_sems = self.eng_sem, self.stream_sems
        self.nblocks += 1
        with nc.Block() as block:
            def run(ename):
                def body(eng):
                    for o in ops:
                        if o.eng != ename:
                            continue
                        for kind, i in o.waits:
                            p = ops[i]
                            if kind == "dma":
                                eng.wait_ge(stream_sems[p.dsem], p.dval)
                            else:
                                eng.wait_ge(eng_sem[p.eng], p.count)
                        if o.fn is None:
                            continue
                        ins = o.fn(eng)
                        if o.dma is not None:
                            ins.then_inc(stream_sems[o.dsem], 16)
                        elif o.sig:
                            ins.then_inc(eng_sem[o.eng], 1)
                return body
            block.tensor(run("pe"))
            block.scalar(run("act"))
            block.vector(run("dve"))
            block.gpsimd(run("pool"))
            block.sync(run("sp"))


class _View:
    def __init__(self, t, n, dims):
        self.t, self.n, self.dims = t, n, dims

    def __getitem__(self, key):
        v = self.t[:, 0:self.n]
        if self.dims is not None:
            v = v.rearrange("p (a b) -> p a b", a=self.dims[0])
        return v[key]


class KB:
    def __init__(self, nc, es):
        self.nc = nc
        self.es = es
        self.S = Sched(nc, es)
        self.uid = 0

    def sb(self, stack, name, shape, dt=F32):
        self.uid += 1
        return stack.enter_context(self.nc.sbuf_tensor("%s_%d" % (name, self.uid), shape, dt))

    def ps(self, stack, name, shape, dt=F32):
        self.uid += 1
        esz = 4 if dt == F32 else 2
        n = int(np.prod(shape[1:]))
        per_bank = 2048 // esz
        npad = (n + per_bank - 1) // per_bank * per_bank
        t = stack.enter_context(self.nc.psum_tensor("%s_%d" % (name, self.uid), [128, npad], dt))
        if len(shape) == 2:
            return _View(t, n, None)
        return _View(t, n, shape[1:])

    def mm(self, out, lhsT, rhs, start=True, stop=True, r=(), w=()):
        self.S.op("pe", lambda e: e.matmul(out, lhsT=lhsT, rhs=rhs, start=start, stop=stop), r, w)

    def tr(self, out, in_, ident, r=(), w=()):
        self.S.op("pe", lambda e: e.transpose(out=out, in_=in_, identity=ident), r, w)

    def act(self, out, in_, func, r=(), w=(), bias=None, scale=None, accum_out=None):
        kw = {}
        if bias is not None:
            kw["bias"] = bias
        if scale is not None:
            kw["scale"] = scale
        if accum_out is not None:
            kw["accum_out"] = accum_out
        self.S.op("act", lambda e: e.activation(out=out, in_=in_, func=func, **kw), r, w)

    def cp(self, eng, out, in_, r=(), w=()):
        if eng == "act":
            self.S.op("act", lambda e: e.copy(out=out, in_=in_), r, w)
        else:
            self.S.op(eng, lambda e: e.tensor_copy(out=out, in_=in_), r, w)

    def tt(self, eng, out, in0, in1, op, r=(), w=()):
        self.S.op(eng, lambda e: e.tensor_tensor(out=out, in0=in0, in1=in1, op=op), r, w)

    def ts(self, eng, out, in0, s1, op0, s2=None, op1=None, r=(), w=(), accum_out=None):
        kw = {}
        if op1 is not None:
            kw["op1"] = op1
        if accum_out is not None:
            kw["accum_out"] = accum_out
        self.S.op(eng, lambda e: e.tensor_scalar(out=out, in0=in0, scalar1=s1, scalar2=s2, op0=op0, **kw), r, w)

    def stt(self, eng, out, in0, scalar, in1, op0, op1, r=(), w=(), accum_out=None):
        kw = {}
        if accum_out is not None:
            kw["accum_out"] = accum_out
        self.S.op(eng, lambda e: e.scalar_tensor_tensor(out=out, in0=in0, scalar=scalar, in1=in1, op0=op0, op1=op1, **kw), r, w)

    def red(self, eng, out, in_, op, r=(), w=()):
        self.S.op(eng, lambda e: e.tensor_reduce(out=out, in_=in_, axis=AX.X, op=op), r, w)

    def recip(self, out, in_, r=(), w=()):
        self.S.op("dve", lambda e: e.reciprocal(out=out, in_=in_), r, w)

    def memset(self, eng, ap, val, w=()):
        self.S.op(eng, lambda e: e.memset(ap, val), (), w)

    def dma(self, eng, out, in_, r=(), w=(), stream="d", K=2):
        self.S.dma(eng, lambda e: e.dma_start(out=out, in_=in_), r, w, stream, K)

    def gather(self, out, src, idx, r=(), w=(), stream="g", K=2):
        self.S.dma("pool", lambda e: e.indirect_dma_start(out=out, out_offset=None, in_=src,
                                                          in_offset=bass.IndirectOffsetOnAxis(ap=idx, axis=0)), r, w, stream, K)

    def scatter(self, dst, in_, idx, r=(), w=(), stream="sc", K=2):
        self.S.dma("pool", lambda e: e.indirect_dma_start(out=dst, out_offset=bass.IndirectOffsetOnAxis(ap=idx, axis=0),
                                                          in_=in_, in_offset=None), r, w, stream, K)


def build_program(steps, want):
    nc = bass.Bass("TRN2", target_bir_lowering=False)
    es = contextlib.ExitStack()
    kb = KB(nc, es)
    S = kb.S
    layers = sorted(set(i for _, i in steps))
    mla_layers = sorted(set(i for k, i in steps if k in ("P1", "ATT", "ATTO")))
    pool_layers = sorted(set(i for k, i in steps if k == "POOL"))
    moe_layers = sorted(set(i for k, i in steps if k in ("ATT", "POOL", "EXPO")))

    def din(name, shape, dt=F32):
        return nc.dram_tensor(name, list(shape), dt, kind="ExternalInput").ap()

    def dout(name, shape, dt=F32):
        return nc.dram_tensor(name, list(shape), dt, kind="ExternalOutput").ap()

    def dint(name, shape, dt=F32):
        return nc.dram_tensor(name, list(shape), dt, kind="Internal").ap()

    x_in = din("x_in", [TOK, D])
    cst = din("cst", [128, 3 * 128])
    identf_d = din("identf", [128, 128])
    W = {}
    if mla_layers:
        pos_d = din("pos", [128, NT], I32)
        invf_d = din("invf", [128, 16])
    for i in mla_layers:
        W["w_in", i] = din("w_in%d" % i, [D, 416])
        W["w_uq", i] = din("w_uq%d" % i, [256, 1536])
        W["w_uqr", i] = din("w_uqr%d" % i, [256, 1536])
        W["w_ukv", i] = din("w_ukv%d" % i, [128, 2048])
        W["qn", i] = din("qn%d" % i, [256])
        W["kvn", i] = din("kvn%d" % i, [128])
        if ("ATT", i) in steps or ("ATTO", i) in steps:
            W["w_o", i] = din("w_o%d" % i, [D, D])
            if MODE == "pair":
                W["kv_gath", i] = din("kv_gath%d" % i, [2, 2, 128, TOK], BF16)
    if any(k in ("ATT", "ATTO") for k, _ in steps):
        amask_d = din("amask", [128, BAND, 512])
    for i in pool_layers:
        W["pw", i] = din("pw%d" % i, [4, 256, 256])
        W["pb", i] = din("pb%d" % i, [D])
        W["psc", i] = din("psc%d" % i, [D])
        if MODE == "pair":
            W["halo", i] = din("halo%d" % i, [4, 128, D])
    if pool_layers:
        pmat_d = din("pmat", [128, 4 * 128 * 2 + 8 * 4 * 128])
    for i in moe_layers:
        W["lng", i] = din("lng%d" % i, [2, D])
        W["lnb", i] = din("lnb%d" % i, [2, D])
        W["wr", i] = din("wr%d" % i, [D, 72])
        W["br", i] = din("br%d" % i, [72])
        W["w1", i] = din("w1_%d" % i, [NE, 128, 2048])
        W["w3", i] = din("w3_%d" % i, [NE, 128, 2048])
        W["w2", i] = din("w2_%d" % i, [NE, 256, D])
    if moe_layers:
        ecap_d = din("ecap", [128, NE])
    outs = {}
    if "kv_own" in want:
        outs["kv_own"] = dout("kv_own", [2, 128, TOK], BF16)
    else:
        outs["kv_own"] = dint("kv_own_i", [2, 128, TOK], BF16)
    if "x_out" in want:
        outs["x_out"] = dout("x_out", [TOK, D])
    if "dbg" in want:
        outs["dbg"] = dout("dbg", [TOK, 8])
    if moe_layers:
        x1_dram = dint("x1_dram", [TOK, D])
        rows_dram = din("rows_in", [NE * CAP, D], BF16) if "y_out" in want else dint("rows_dram", [NE * CAP, D], BF16)
        y_dram = dout("y_out", [NE * CAP, D]) if "y_out" in want else dint("y_dram", [NE * CAP, D])
        xa_dram = dint("xa_dram", [TOK, D])
        xb_dram = dint("xb_dram", [TOK, D])
    if any(k in ("ATT", "ATTO") for k, _ in steps):
        o_dram = dout("o_out", [D, TOK], BF16) if "o_out" in want else dint("o_dram", [D, TOK], BF16)

    g = contextlib.ExitStack()
    cst_bf = kb.sb(g, "cst_bf", [128, 3 * 128], BF16)
    ident_bf = cst_bf[:, 0:128]
    ones_bf = cst_bf[:, 128:256]
    ustrict_bf = cst_bf[:, 256:384]
    ident_f = kb.sb(g, "ident_f", [128, 128])
    kb.dma("pool", cst_bf[:], cst, w=["cst"], stream="c0", K=1)
    kb.dma("sp", ident_f[:], identf_d, w=["identf"], stream="c1", K=1)
    S.flush()

    if mla_layers:
        ccs = kb.sb(g, "ccs", [128, NT, 32])
        ssn = kb.sb(g, "ssn", [128, NT, 32])
        cosT = kb.sb(g, "cosT", [128, TOK], BF16)
        sinT = kb.sb(g, "sinT", [128, TOK], BF16)
        cqT = kb.sb(g, "cqT", [128, 2, TOK], BF16)
        with contextlib.ExitStack() as ph:
            posi = kb.sb(ph, "posi", [128, NT], I32)
            posf = kb.sb(ph, "posf", [128, NT])
            invf = kb.sb(ph, "invf", [128, 16])
            ang = kb.sb(ph, "ang", [128, NT, 16])
            yk = kb.sb(ph, "yk", [128, NT * 16])
            ki = kb.sb(ph, "ki", [128, NT * 16], I32)
            kf = kb.sb(ph, "kf", [128, NT * 16])
            rr = kb.sb(ph, "rr", [128, NT * 16])
            sres = kb.sb(ph, "sres", [128, NT, 16])
            cres = kb.sb(ph, "cres", [128, NT, 16])
            stg = kb.sb(ph, "stg", [128, 2, 96])
            ptab = kb.ps(ph, "ptab", [128, 4, 128])
            kb.dma("sp", posi[:], pos_d, w=["posi"], stream="c0", K=1)
            kb.dma("sp", invf[:], invf_d, w=["invf"], stream="c1", K=1)
            kb.cp("dve", posf[:], posi[:], r=["posi"], w=["posf"])
            for m in range(NT):
                kb.ts("dve", ang[:, m, :], invf[:], posf[:, m:m + 1], ALU.mult, r=["posf", "invf"], w=["ang"])
            angf = ang[:].rearrange("p m i -> p (m i)")

            kb.ts("dve", yk[:], angf, 1.0 / TWO_PI, ALU.mult, r=["ang"], w=["yk"])
            kb.cp("dve", ki[:], yk[:], r=["yk"], w=["ki"])
            kb.cp("dve", kf[:], ki[:], r=["ki"], w=["kf"])
            kb.stt("dve", rr[:], kf[:], -C1, angf, ALU.mult, ALU.add, r=["kf", "ang"], w=["rr"])
            kb.stt("dve", rr[:], kf[:], -C2, rr[:], ALU.mult, ALU.add, r=["kf", "rr"], w=["rr"])
            kb.ts("dve", rr[:], rr[:], 3.14159, ALU.min, -3.14159, ALU.max, r=["rr"], w=["rr"])
            kb.act(sres[:].rearrange("p m i -> p (m i)"), rr[:], AF.Sin, r=["rr"], w=["sres"])
            kb.stt("dve", yk[:], rr[:], -1.0, rr[:], ALU.mult, ALU.max, r=["rr"], w=["yk"])
            kb.ts("dve", yk[:], yk[:], -1.0, ALU.mult, float(np.pi / 2), ALU.add, r=["yk"], w=["yk"])
            kb.act(cres[:].rearrange("p m i -> p (m i)"), yk[:], AF.Sin, r=["yk"], w=["cres"])
            kb.cp("dve", ccs[:, :, 0:16], cres[:], r=["cres"], w=["ccs"])
            kb.cp("dve", ccs[:, :, 16:32], cres[:], r=["cres"], w=["ccs"])
            kb.ts("dve", ssn[:, :, 0:16], sres[:], -1.0, ALU.mult, r=["sres"], w=["ssn"])
            kb.cp("dve", ssn[:, :, 16:32], sres[:], r=["sres"], w=["ssn"])
            kb.memset("pool", stg[:], 0.0, w=["stg0", "stg1"])
            for tab, dstT, nm in ((ccs, cosT, "cosT"), (ssn, sinT, "sinT")):
                for m4 in range(NT // 4):
                    for j in range(4):
                        m = m4 * 4 + j
                        s = m % 2
                        kb.ts("dve", stg[:, s, 64:96], tab[:, m, :], SCALE, ALU.mult, r=["ccs", "ssn"], w=["stg%d" % s])
                        kb.tr(ptab[0:96, j, :], stg[:, s, :], ident_f[:], r=["stg%d" % s, "identf"], w=["ptab"])
                    kb.cp("act", dstT[64:96, m4 * 512:(m4 + 1) * 512],
                          ptab[64:96, :, :].rearrange("p a b -> p (a b)"), r=["ptab"], w=[nm])
            S.flush()

    def layernorm(st, uu, xo, lng_b, lnb_b, tag, ukey, wkey):
        junk, s12, sm = st["junk"], st["s12"], st["sm"]
        u = uu[:, 0, :]
        kb.act(uu[:, 1, :], u, AF.Square, r=[ukey], w=[tag + "usq"])
        kb.red("dve", s12[:], uu[:], ALU.add, r=[ukey, tag + "usq"], w=[tag + "sm"])
        kb.ts("dve", sm[:, 0:2], s12[:], 1.0 / D, ALU.mult, r=[tag + "sm"], w=[tag + "sm"])
        kb.tt("dve", sm[:, 2:3], sm[:, 0:1], sm[:, 0:1], ALU.mult, r=[tag + "sm"], w=[tag + "sm"])
        kb.stt("dve", sm[:, 3:4], sm[:, 1:2], LN_EPS, sm[:, 2:3], ALU.add, ALU.subtract, r=[tag + "sm"], w=[tag + "sm"])
        kb.act(sm[:, 4:5], sm[:, 3:4], AF.Ln, r=[tag + "sm"], w=[tag + "sm"])
        kb.act(sm[:, 5:6], sm[:, 4:5], AF.Exp, r=[tag + "sm"], w=[tag + "sm"], scale=-0.5)
        kb.stt("dve", sm[:, 6:7], sm[:, 0:1], -1.0, sm[:, 5:6], ALU.mult, ALU.mult, r=[tag + "sm"], w=[tag + "sm"])
        kb.act(junk[:], u, AF.Identity, r=[ukey, tag + "sm"], w=[tag + "junk"], bias=sm[:, 6:7], scale=sm[:, 5:6])
        kb.tt("dve", junk[:], junk[:], lng_b, ALU.mult, r=[tag + "junk", "lnw"], w=[tag + "junk"])
        kb.tt("dve", xo, junk[:], lnb_b, ALU.add, r=[tag + "junk", "lnw"], w=[wkey])

    def phase_P1(i, x_src):
        with contextlib.ExitStack() as ph:
            w_in_bf = kb.sb(ph, "w_in_bf", [128, 8, 416], BF16)
            qn_b = kb.sb(ph, "qn_b", [128, 256])
            kvn_b = kb.sb(ph, "kvn_b", [128, 128])
            xt = [kb.sb(ph, "xt", [128, D]) for _ in range(2)]
            xb = [kb.sb(ph, "xb", [128, D], BF16) for _ in range(2)]
            xT = [kb.sb(ph, "xT", [128, 8, 128], BF16) for _ in range(2)]
            kvt = [kb.sb(ph, "kvt", [128, 160]) for _ in range(2)]
            cqn = [kb.sb(ph, "cqn", [128, 256], BF16) for _ in range(2)]
            junk = kb.sb(ph, "junk", [128, 384])
            ssq = [kb.sb(ph, "ssq", [128, 8]) for _ in range(2)]
            t1 = [kb.sb(ph, "t1", [128, 32]) for _ in range(2)]
            t2 = [kb.sb(ph, "t2", [128, 32]) for _ in range(2)]
            pT = [kb.ps(ph, "pT", [128, 8, 128], BF16) for _ in range(2)]
            plat = [kb.ps(ph, "plat", [128, 416]) for _ in range(2)]
            pcq = kb.ps(ph, "pcq", [128, 2, 128], BF16)
            pkvT = kb.ps(ph, "pkvT", [128, 2, 128], BF16)
            kvtb = [kb.sb(ph, "kvtb", [128, 160], BF16) for _ in range(2)]
            kvTs = [kb.sb(ph, "kvTs", [128, 2, 128], BF16) for _ in range(2)]
            kb.memset("pool", kvTs[0][:], 0.0, w=["kvTs0"])
            kb.memset("pool", kvTs[1][:], 0.0, w=["kvTs1"])
            epst = kb.sb(ph, "epst", [128, 1])
            kb.memset("pool", epst[:], RMS_EPS, w=["epst"])
            kb.dma("pool", w_in_bf[:], W["w_in", i].rearrange("(c p) n -> p c n", p=128), w=["w_in"], stream="c0", K=1)
            kb.dma("sp", qn_b[:], W["qn", i].partition_broadcast(128), w=["qn"], stream="c1", K=1)
            kb.dma("sp", kvn_b[:], W["kvn", i].partition_broadcast(128), w=["kvn"], stream="c2", K=1)

            def load(m):
                s = m % 2
                kb.dma("sp", xt[s][:], x_src[m * 128:(m + 1) * 128, :], w=["xt%d" % s], stream="ldx", K=2)

            load(0)
            for m in range(NT):
                s = m % 2
                if m + 1 < NT:
                    load(m + 1)
                X, XB, XT, PT, PL = "xt%d" % s, "xb%d" % s, "xT%d" % s, "pT%d" % s, "plat%d" % s
                kb.cp("act", xb[s][:], xt[s][:], r=[X], w=[XB])
                for c in range(8):
                    kb.tr(pT[s][:, c, :], xb[s][:, c * 128:(c + 1) * 128], ident_bf, r=[XB, "cst"], w=[PT])
                kb.cp("dve", xT[s][:], pT[s][:], r=[PT], w=[XT])
                for c in range(8):
                    kb.mm(plat[s][:], xT[s][:, c, :], w_in_bf[:, c, :], start=(c == 0), stop=(c == 7), r=[XT, "w_in"], w=[PL])
                kb.act(junk[:, 0:384], plat[s][:, 0:384], AF.Square, r=[PL], w=["junk"])
                kb.red("dve", ssq[s][:, 4:7], junk[:, 0:384].rearrange("p (a b) -> p a b", a=3), ALU.add, r=["junk"], w=["ssq%d" % s])
                kb.tt("dve", ssq[s][:, 0:1], ssq[s][:, 4:5], ssq[s][:, 5:6], ALU.add, r=["ssq%d" % s], w=["ssq%d" % s])
                kb.cp("dve", ssq[s][:, 1:2], ssq[s][:, 6:7], r=["ssq%d" % s], w=["ssq%d" % s])
                kb.act(ssq[s][:, 2:3], ssq[s][:, 0:1], AF.Ln, r=["ssq%d" % s, "epst"], w=["ssq%d" % s], bias=epst[:, 0:1], scale=1.0 / 256)
                kb.act(ssq[s][:, 3:4], ssq[s][:, 1:2], AF.Ln, r=["ssq%d" % s, "epst"], w=["ssq%d" % s], bias=epst[:, 0:1], scale=1.0 / 128)
                kb.act(ssq[s][:, 2:4], ssq[s][:, 2:4], AF.Exp, r=["ssq%d" % s], w=["ssq%d" % s], scale=-0.5)
                kb.stt("dve", cqn[s][:], plat[s][:, 0:256], ssq[s][:, 2:3], qn_b[:], ALU.mult, ALU.mult,
                       r=[PL, "ssq%d" % s, "qn"], w=["cqn%d" % s])
                kb.stt("dve", kvt[s][:, 0:128], plat[s][:, 256:384], ssq[s][:, 3:4], kvn_b[:], ALU.mult, ALU.mult,
                       r=[PL, "ssq%d" % s, "kvn"], w=["kvt%d" % s])
                kb.tt("dve", t1[s][:], plat[s][:, 384:416], ccs[:, m, :], ALU.mult, r=[PL, "ccs", "junk"], w=["t1%d" % s])
                kb.tt("dve", t2[s][:, 0:16], plat[s][:, 400:416], ssn[:, m, 0:16], ALU.mult, r=[PL, "ssn"], w=["t2%d" % s])
                kb.tt("dve", t2[s][:, 16:32], plat[s][:, 384:400], ssn[:, m, 16:32], ALU.mult, r=[PL, "ssn"], w=["t2%d" % s])
                kb.tt("dve", kvt[s][:, 128:160], t1[s][:], t2[s][:], ALU.add, r=["t1%d" % s, "t2%d" % s], w=["kvt%d" % s])
                for cc in range(2):
                    kb.tr(pcq[:, cc, :], cqn[s][:, cc * 128:(cc + 1) * 128], ident_bf, r=["cqn%d" % s, "cst"], w=["pcq"])
                kb.cp("act", cqT[:, :, m * 128:(m + 1) * 128], pcq[:], r=["pcq"], w=["cqT"])
                kb.cp("dve", kvtb[s][:], kvt[s][:], r=["kvt%d" % s], w=["kvtb%d" % s])
                kb.tr(pkvT[:, 0, :], kvtb[s][:, 0:128], ident_bf, r=["kvtb%d" % s, "cst"], w=["pkvT"])
                kb.tr(pkvT[0:96, 1, :], kvtb[s][:, 64:160], ident_bf, r=["kvtb%d" % s, "cst"], w=["pkvT"])
                kb.cp("act", kvTs[s][:, 0, :], pkvT[:, 0, :], r=["pkvT"], w=["kvTs%d" % s])
                kb.cp("act", kvTs[s][64:96, 1, :], pkvT[64:96, 1, :], r=["pkvT"], w=["kvTs%d" % s])
                kb.dma("sp", outs["kv_own"][:, :, m * 128:(m + 1) * 128].rearrange("a p t -> p a t"), kvTs[s][:],
                       r=["kvTs%d" % s], w=[], stream="stkv", K=2)
            S.flush()

    def tail_setup(ph, i, which):
        T = {}
        T["lng_b"] = kb.sb(ph, "lng_b", [128, D])
        T["lnb_b"] = kb.sb(ph, "lnb_b", [128, D])
        kb.dma("sp", T["lng_b"][:], W["lng", i][which, :].partition_broadcast(128), w=["lnw"], stream="c0", K=1)
        kb.dma("sp", T["lnb_b"][:], W["lnb", i][which, :].partition_broadcast(128), w=["lnw"], stream="c1", K=1)
        return T

    def router_setup(ph, i):
        R = {}
        R["wr"] = kb.sb(ph, "wr", [128, 8, 72])
        R["br_b"] = kb.sb(ph, "br_b", [128, 72])
        R["ecap"] = kb.sb(ph, "ecap", [128, NE])
        R["cnt"] = kb.sb(ph, "cnt", [128, NE])
        kb.dma("sp", R["wr"][:], W["wr", i].rearrange("(c p) n -> p c n", p=128), w=["wr"], stream="c2", K=1)
        kb.dma("sp", R["br_b"][:], W["br", i].partition_broadcast(128), w=["br"], stream="c3", K=1)
        kb.dma("sp", R["ecap"][:], ecap_d, w=["ecap"], stream="c4", K=1)
        kb.memset("pool", R["cnt"][:], 0.0, w=["cnt"])
        R["x1T"] = kb.sb(ph, "x1T", [128, 8, 128])
        R["lg"] = kb.sb(ph, "lg", [128, 72])
        R["sm"] = kb.sb(ph, "rsm", [128, 16])
        R["goh"] = kb.sb(ph, "goh", [128, 8])
        R["gd"] = kb.sb(ph, "gd", [128, 8])
        R["tmp64"] = kb.sb(ph, "tmp64", [128, 8, 8])
        R["els"] = kb.sb(ph, "els", [128, 8])
        R["els2"] = kb.sb(ph, "els2", [128, 8])
        R["oh"] = [kb.sb(ph, "oh", [128, 8]) for _ in range(2)]
        R["oh64"] = [kb.sb(ph, "oh64", [128, 8, 8]) for _ in range(2)]
        R["A"] = kb.sb(ph, "A", [128, NE], BF16)
        R["pos"] = kb.sb(ph, "pos", [128, NE])
        R["destf"] = kb.sb(ph, "destf", [128, 2])
        R["xbf"] = [kb.sb(ph, "xbf", [128, D], BF16) for _ in range(2)]
        R["zt"] = kb.sb(ph, "zt", [128, 4096], BF16)
        kb.memset("pool", R["zt"][:], 0.0, w=["zt"])
        rz = rows_dram.rearrange("(n p f) d -> n p (f d)", p=128, f=4)
        for n_ in range(NE * CAP // 512):
            kb.dma("sp", rz[n_], R["zt"][:], r=["zt"], w=["rz"], stream="rz", K=4)
        R["pxT"] = kb.ps(ph, "pxT", [128, 8, 128])
        R["plg"] = kb.ps(ph, "plg", [128, 256])
        return R

    def router_tile(R, m, x1, x1key, RI, RG):
        s = m % 2
        for c in range(8):
            kb.tr(R["pxT"][:, c, :], x1[:, c * 128:(c + 1) * 128], ident_f[:], r=[x1key, "identf"], w=["pxT"])
        kb.cp("dve", R["x1T"][:, 0:4, :], R["pxT"][:, 0:4, :], r=["pxT"], w=["x1Ta"])
        kb.cp("act", R["x1T"][:, 4:8, :], R["pxT"][:, 4:8, :], r=["pxT"], w=["x1Tb"])
        for c in range(8):
            kb.mm(R["plg"][:, 0:72], R["x1T"][:, c, :], R["wr"][:, c, :], start=(c == 0), stop=(c == 7),
                  r=["x1Ta", "x1Tb", "wr"], w=["plg"])
        lg, sm = R["lg"], R["sm"]
        kb.tt("dve", lg[:], R["plg"][:, 0:72], R["br_b"][:], ALU.add, r=["plg", "br"], w=["lg"])
        kb.red("dve", sm[:, 0:1], lg[:, 0:8], ALU.max, r=["lg"], w=["rsm"])
        kb.ts("dve", R["goh"][:], lg[:, 0:8], sm[:, 0:1], ALU.is_equal, r=["lg", "rsm"], w=["goh"])
        kb.ts("dve", R["gd"][:], lg[:, 0:8], sm[:, 0:1], ALU.subtract, r=["lg", "rsm"], w=["gd"])
        kb.act(R["gd"][:], R["gd"][:], AF.Exp, r=["gd"], w=["gd"])
        kb.red("dve", sm[:, 1:2], R["gd"][:], ALU.add, r=["gd"], w=["rsm"])
        kb.recip(sm[:, 2:3], sm[:, 1:2], r=["rsm"], w=["rsm"])
        kb.tt("dve", R["tmp64"][:], lg[:, 8:72].rearrange("p (g j) -> p g j", g=8),
              R["goh"][:].unsqueeze(2).to_broadcast([128, 8, 8]), ALU.mult, r=["lg", "goh"], w=["tmp64"])
        kb.red("dve", R["els"][:], R["tmp64"][:].rearrange("p g j -> p j g"), ALU.add, r=["tmp64"], w=["els"])
        kb.red("dve", sm[:, 3:4], R["els"][:], ALU.max, r=["els"], w=["rsm"])
        kb.ts("dve", R["oh"][0][:], R["els"][:], sm[:, 3:4], ALU.is_equal, r=["els", "rsm"], w=["oh0"])
        kb.stt("dve", R["els2"][:], R["oh"][0][:], -1e30, R["els"][:], ALU.mult, ALU.add, r=["oh0", "els"], w=["els2"])
        kb.red("dve", sm[:, 4:5], R["els2"][:], ALU.max, r=["els2"], w=["rsm"])
        kb.ts("dve", R["oh"][1][:], R["els2"][:], sm[:, 4:5], ALU.is_equal, r=["els2", "rsm"], w=["oh1"])
        kb.tt("dve", sm[:, 5:6], sm[:, 4:5], sm[:, 3:4], ALU.subtract, r=["rsm"], w=["rsm"])
        kb.act(sm[:, 6:7], sm[:, 5:6], AF.Exp, r=["rsm"], w=["rsm"])
        kb.ts("dve", sm[:, 7:8], sm[:, 6:7], 1.0, ALU.add, r=["rsm"], w=["rsm"])
        kb.recip(sm[:, 8:9], sm[:, 7:8], r=["rsm"], w=["rsm"])
        kb.tt("dve", RG[:, m, 0:1], sm[:, 2:3], sm[:, 8:9], ALU.mult, r=["rsm"], w=["RG"])
        kb.tt("dve", RG[:, m, 1:2], RG[:, m, 0:1], sm[:, 6:7], ALU.mult, r=["rsm", "RG"], w=["RG"])
        for k in range(2):
            kb.tt("dve", R["oh64"][k][:], R["goh"][:].unsqueeze(2).to_broadcast([128, 8, 8]),
                  R["oh"][k][:].unsqueeze(1).to_broadcast([128, 8, 8]), ALU.mult, r=["goh", "oh%d" % k], w=["oh64%d" % k])
        kb.tt("dve", R["A"][:], R["oh64"][0][:].rearrange("p g j -> p (g j)"), R["oh64"][1][:].rearrange("p g j -> p (g j)"),
              ALU.add, r=["oh640", "oh641"], w=["A"])
        kb.mm(R["plg"][:, 128:192], ustrict_bf, R["A"][:], r=["A", "cst"], w=["ppos"])
        kb.mm(R["plg"][:, 192:256], ones_bf, R["A"][:], r=["A", "cst"], w=["ppos"])
        kb.tt("dve", R["pos"][:], R["plg"][:, 128:192], R["cnt"][:], ALU.add, r=["ppos", "cnt"], w=["pos"])
        kb.tt("dve", R["cnt"][:], R["cnt"][:], R["plg"][:, 192:256], ALU.add, r=["ppos", "cnt"], w=["cnt"])
        kb.ts("dve", R["pos"][:], R["pos"][:], float(CAP - 1), ALU.min, r=["pos"], w=["pos"])
        kb.tt("dve", R["pos"][:], R["pos"][:], R["ecap"][:], ALU.add, r=["pos", "ecap"], w=["pos"])
        for k in range(2):
            kb.tt("dve", R["oh64"][k][:].rearrange("p g j -> p (g j)"), R["oh64"][k][:].rearrange("p g j -> p (g j)"),
                  R["pos"][:], ALU.mult, r=["oh64%d" % k, "pos"], w=["oh64%d" % k])
            kb.red("dve", R["destf"][:, k:k + 1], R["oh64"][k][:].rearrange("p g j -> p (g j)"), ALU.add,
                   r=["oh64%d" % k], w=["destf"])
        kb.cp("dve", RI[:, m, :], R["destf"][:], r=["destf"], w=["RI"])
        kb.cp("act", R["xbf"][s][:], x1, r=[x1key], w=["xbf%d" % s])
        for k in range(2):
            kb.scatter(rows_dram, R["xbf"][s][:], RI[:, m, k:k + 1], r=["xbf%d" % s, "RI", "rz"], w=[], stream="sc%d" % k, K=2)

    def phase_experts(i):
        NSB = CAP // 128
        with contextlib.ExitStack() as ph:
            w1b = [kb.sb(ph, "w1b", [128, 8, 256], BF16) for _ in range(2)]
            w3b = [kb.sb(ph, "w3b", [128, 8, 256], BF16) for _ in range(2)]
            w2b = [kb.sb(ph, "w2b", [128, 2, D], BF16) for _ in range(2)]
            rows = [kb.sb(ph, "rows", [128, NSB, D], BF16) for _ in range(2)]
            xgT = [kb.sb(ph, "xgT", [128, 8, 128], BF16) for _ in range(2)]
            sl = [kb.sb(ph, "sl", [128, 2, 128]) for _ in range(2)]
            hT = [kb.sb(ph, "hT", [128, 2, 128], BF16) for _ in range(2)]
            ys = [kb.sb(ph, "ys", [128, D]) for _ in range(3)]
            pT = [kb.ps(ph, "pT", [128, 8, 128], BF16) for _ in range(2)]
            phh = [kb.ps(ph, "phh", [128, 4, 128]) for _ in range(2)]
            py = [kb.ps(ph, "py", [128, D]) for _ in range(2)]

            def load(e):
                s = e % 2
                for hh_ in range(2):
                    kb.dma("pool", w1b[s][:, 4 * hh_:4 * hh_ + 4, :].rearrange("p c f -> p (c f)"), W["w1", i][e][:, 1024 * hh_:1024 * hh_ + 1024],
                           w=["w13a%d" % s], stream="w1%d" % hh_, K=2)
                    kb.dma("pool", w3b[s][:, 4 * hh_:4 * hh_ + 4, :].rearrange("p c f -> p (c f)"), W["w3", i][e][:, 1024 * hh_:1024 * hh_ + 1024],
                           w=["w13b%d" % s], stream="w3%d" % hh_, K=2)
                kb.dma("pool", w2b[s][:], W["w2", i][e].rearrange("(c p) n -> p c n", p=128), w=["w2b%d" % s], stream="w2", K=2)
                kb.dma("sp", rows[s][:], rows_dram[e * CAP:(e + 1) * CAP, :].rearrange("(sb p) d -> p sb d", p=128),
                       w=["rows%d" % s], stream="ldr", K=2)

            load(0)
            blk = 0
            NER = DBG.get('ne', NE)
            for e in range(NER):
                s = e % 2
                if e + 1 < NER:
                    load(e + 1)
                for sbk in range(NSB):
                    b2 = blk % 2
                    b3 = blk % 3
                    blk += 1
                    for c in range(8):
                        kb.tr(pT[b2][:, c, :], rows[s][:, sbk, c * 128:(c + 1) * 128], ident_bf, r=["rows%d" % s, "cst"], w=["pT%d" % b2])
                    kb.cp("dve" if b2 == 0 else "act", xgT[b2][:], pT[b2][:], r=["pT%d" % b2], w=["xgTa%d" % b2, "xgTb%d" % b2])
                    for fi in range(4):
                        for c in range(8):
                            wsrc = w1b[s] if fi < 2 else w3b[s]
                            kb.mm(phh[b2][:, fi, :], wsrc[:, c, (fi % 2) * 128:(fi % 2 + 1) * 128], xgT[b2][:, c, :], start=(c == 0), stop=(c == 7),
                                  r=["w13a%d" % s, "w13b%d" % s, "xgTa%d" % b2, "xgTb%d" % b2], w=["phh%d" % b2])
                    kb.act(sl[b2][:], phh[b2][:, 0:2, :], AF.Silu, r=["phh%d" % b2], w=["sl%d" % b2])
                    kb.tt("dve", hT[b2][:], sl[b2][:], phh[b2][:, 2:4, :], ALU.mult, r=["sl%d" % b2, "phh%d" % b2], w=["hT%d" % b2])
                    for half in range(2):
                        for fc in range(2):
                            kb.mm(py[b2][:, half * 512:(half + 1) * 512], hT[b2][:, fc, :],
                                  w2b[s][:, fc, half * 512:(half + 1) * 512], start=(fc == 0), stop=(fc == 1),
                                  r=["hT%d" % b2, "w2b%d" % s], w=["py%d" % b2])
                    kb.cp("act", ys[b3][:, 0:512], py[b2][:, 0:512], r=["py%d" % b2], w=["ysa%d" % b3])
                    kb.cp("dve", ys[b3][:, 512:1024], py[b2][:, 512:1024], r=["py%d" % b2], w=["ysb%d" % b3])
                    kb.dma("sp", y_dram[e * CAP + sbk * 128: e * CAP + (sbk + 1) * 128, :], ys[b3][:], r=["ysa%d" % b3, "ysb%d" % b3], w=[],
                           stream="sty", K=3)
            S.flush()

    def phase_combine(i, RI, RG, x_dst):
        NB = 4
        with contextlib.ExitStack() as ph:
            T = tail_setup(ph, i, 1)
            y0 = [kb.sb(ph, "y0", [128, D]) for _ in range(NB)]
            y1 = [kb.sb(ph, "y1", [128, D]) for _ in range(NB)]
            x1t = [kb.sb(ph, "x1t", [128, D]) for _ in range(NB)]
            u = [kb.sb(ph, "u", [128, 2, D]) for _ in range(NB)]
            st = [dict(junk=kb.sb(ph, "junk", [128, D]), s12=kb.sb(ph, "s12", [128, 2]), sm=kb.sb(ph, "sm", [128, 8])) for _ in range(NB)]

            def load(m):
                s = m % NB
                kb.gather(y0[s][:], y_dram, RI[:, m, 0:1], r=["RI", "y_dram"], w=["y0%d" % s], stream="g0", K=NB)
                kb.gather(y1[s][:], y_dram, RI[:, m, 1:2], r=["RI", "y_dram"], w=["y1%d" % s], stream="g1", K=NB)
                kb.dma("sp", x1t[s][:], x1_dram[m * 128:(m + 1) * 128, :], r=["x1_dram"], w=["x1t%d" % s], stream="ldxc", K=NB)

            def body(m):
                s = m % NB
                kb.act(u[s][:, 0, :], x1t[s][:], AF.Copy, r=["x1t%d" % s], w=["u%d" % s], scale=ALPHA)
                kb.stt("dve", u[s][:, 0, :], y0[s][:], RG[:, m, 0:1], u[s][:, 0, :], ALU.mult, ALU.add, r=["y0%d" % s, "u%d" % s, "RG"], w=["u%d" % s])
                kb.stt("dve", u[s][:, 0, :], y1[s][:], RG[:, m, 1:2], u[s][:, 0, :], ALU.mult, ALU.add, r=["y1%d" % s, "u%d" % s, "RG"], w=["u%d" % s])
                layernorm(st[s], u[s], st[s]["junk"][:], T["lng_b"][:], T["lnb_b"][:], "c%d" % s, "u%d" % s, "c%djunk" % s)
                kb.dma("sp", x_dst[m * 128:(m + 1) * 128, :], st[s]["junk"][:], r=["c%djunk" % s], w=[], stream="stx", K=NB)

            load(0)
            load(1)
            for m in range(0, NT, 2):
                for mm in (m + 2, m + 3):
                    if mm < NT:
                        load(mm)
                a = S.capture(lambda: body(m))
                b = S.capture(lambda: body(m + 1))
                S.emit_zipped(a, b)
            S.flush()

    def phase_att(i, x_src, RI, RG, attn_only=False):
        with contextlib.ExitStack() as pa:
            ckvT = kb.sb(pa, "ckvT", [128, 8192], BF16)
            KT = [kb.sb(pa, "KT", [128, 8192], BF16) for _ in range(2)]
            V = [kb.sb(pa, "V", [128, 64, 128], BF16) for _ in range(2)]
            wuq = kb.sb(pa, "wuq", [128, 2, 1536], BF16)
            wuqr = kb.sb(pa, "wuqr", [128, 2, 1536], BF16)
            wukv = kb.sb(pa, "wukv", [128, 2048], BF16)
            amask = kb.sb(pa, "amask", [128, BAND, 512], BF16)
            sk = DBG.get('skip', '')
            if 'q' not in sk:
                kb.dma("pool", wuq[:], W["w_uq", i].rearrange("(c p) n -> p c n", p=128), w=["wuq"], stream="c0", K=1)
                kb.dma("pool", wuqr[:], W["w_uqr", i].rearrange("(c p) n -> p c n", p=128), w=["wuqr"], stream="c1", K=1)
            if 'k' not in sk:
                for a_ in range(2):
                    kb.dma("pool", wukv[:, a_ * 1024:(a_ + 1) * 1024], W["w_ukv", i][:, a_ * 1024:(a_ + 1) * 1024], w=["wukv"], stream="c2", K=1)
            if 'a' not in sk:
                for a_ in range(BAND // 2):
                    kb.dma("pool", amask[:, 2 * a_:2 * a_ + 2, :], amask_d[:, 2 * a_:2 * a_ + 2, :], w=["amask"], stream="c3", K=1)
            if 'm' not in sk:
                kb.memset("pool", V[0][:, :, 64:128], 1.0, w=["V0"])
                kb.memset("pool", V[1][:, :, 64:128], 1.0, w=["V1"])
            if MODE == "pair":
                kvg = W["kv_gath", i]
                for r_ in range(2):
                    kb.dma("sp", ckvT[:].rearrange("p (lt r t) -> p lt r t", r=2, t=128)[:, :, r_, :],
                           kvg[r_, 0].rearrange("p (lt t) -> p lt t", t=128), w=["ckvT"], stream="ldkv%d" % r_, K=1)
                    for hb_ in range(2):
                        kb.dma("sp", KT[hb_][64:96, :].rearrange("p (lt r t) -> p lt r t", r=2, t=128)[:, :, r_, :],
                               kvg[r_, 1, 64:96, :].rearrange("p (lt t) -> p lt t", t=128), w=["KT%dr" % hb_], stream="ldkr%d%d" % (r_, hb_), K=1)
            else:
                kb.dma("sp", ckvT[:], outs["kv_own"][0], w=["ckvT"], stream="ldkv0", K=1)
                for hb_ in range(2):
                    kb.dma("sp", KT[hb_][64:96, :], outs["kv_own"][1, 64:96, :], w=["KT%dr" % hb_], stream="ldkr0%d" % hb_, K=1)
            S.flush()
            if DBG.get('stop') == 'P2a':
                return
            with contextlib.ExitStack() as ph:
                qT = [kb.sb(ph, "qT", [128, 512], BF16) for _ in range(2)]
                tq1 = kb.sb(ph, "tq1", [128, 512])
                tq2 = kb.sb(ph, "tq2", [128, 512])
                pt = [kb.sb(ph, "pt", [128, 512], BF16) for _ in range(5)]
                rl = kb.sb(ph, "rl", [128, 512])
                oT = [kb.sb(ph, "oT", [128, 512], BF16) for _ in range(2)]
                psS = [kb.ps(ph, "psS", [128, 512]) for _ in range(4)]
                po = [kb.ps(ph, "po", [128, 512]) for _ in range(2)]
                pqA = kb.ps(ph, "pqA", [128, 512])
                pqB = kb.ps(ph, "pqB", [128, 512])
                pkv = pqB
                def build_kv(h):
                    hb = h % 2
                    KTh, Vh = KT[hb], V[hb]
                    for kc in range(16):
                        kb.mm(pkv[0:64, :], wukv[:, h * 128:h * 128 + 64], ckvT[:, kc * 512:(kc + 1) * 512], r=["wukv", "ckvT"], w=["pqB"])
                        kb.cp("dve", KTh[0:64, kc * 512:(kc + 1) * 512], pkv[0:64, :], r=["pqB"], w=["KT%dn" % hb])
                    for k8 in range(8):
                        for j in range(8):
                            kt = k8 * 8 + j
                            kb.mm(pkv[:, j * 64:(j + 1) * 64], ckvT[:, kt * 128:(kt + 1) * 128], wukv[:, h * 128 + 64:h * 128 + 128],
                                  r=["wukv", "ckvT"], w=["pqB"])
                        kb.cp("dve", Vh[:, k8 * 8:(k8 + 1) * 8, 0:64], pkv[:].rearrange("p (a b) -> p a b", a=8),
                              r=["pqB"], w=["V%d" % hb])

                def build_q(h, G, qs):
                    for cc in range(2):
                        kb.mm(pqA[0:96, :], wuq[:, cc, h * 96:(h + 1) * 96], cqT[:, cc, G * 512:(G + 1) * 512], start=(cc == 0), stop=(cc == 1),
                              r=["wuq", "cqT"], w=["pqA"])
                    for cc in range(2):
                        kb.mm(pqB[0:96, :], wuqr[:, cc, h * 96:(h + 1) * 96], cqT[:, cc, G * 512:(G + 1) * 512], start=(cc == 0), stop=(cc == 1),
                              r=["wuqr", "cqT"], w=["pqB"])
                    kb.ts("dve", qT[qs][0:64, :], pqA[0:64, :], SCALE, ALU.mult, r=["pqA"], w=["qTn%d" % qs])
                    kb.tt("dve", tq1[64:96, :], pqB[64:96, :], sinT[64:96, G * 512:(G + 1) * 512], ALU.mult, r=["pqB", "sinT"], w=["tq1"])
                    kb.tt("dve", tq2[64:96, :], pqA[64:96, :], cosT[64:96, G * 512:(G + 1) * 512], ALU.mult, r=["pqA", "cosT"], w=["tq2"])
                    kb.tt("dve", qT[qs][64:96, :], tq1[64:96, :], tq2[64:96, :], ALU.add, r=["tq1", "tq2"], w=["qTr%d" % qs])

                groups = [(h, G) for h in range(NH) for G in range(NT // 4)]
                blk = 0
                build_kv(0)
                build_q(0, 0, 0)
                for gi_, (h, G) in enumerate(groups):
                    hb = h % 2
                    qs = gi_ % 2
                    KTh, Vh = KT[hb], V[hb]
                    nk = BAND * G + BAND
                    pos_ = po[qs]
                    bufs = [((blk + k) % 4, (blk + k) % 5) for k in range(nk)]
                    blk += nk

                    def qk(k):
                        b3, b4 = bufs[k]
                        band = (MODE == "seq" and k >= BAND * G)
                        kb.mm(psS[b3][:], KTh[0:96, k * 128:(k + 1) * 128], qT[qs][0:96, :], start=True, stop=not band,
                              r=["KT%dn" % hb, "KT%dr" % hb, "qTn%d" % qs, "qTr%d" % qs], w=["psS%d" % b3])
                        if band:
                            kb.mm(psS[b3][:], ident_bf, amask[:, k - BAND * G, :], start=False, stop=True, r=["cst", "amask"], w=["psS%d" % b3])

                    for k_ in range(min(3, nk)):
                        qk(k_)
                    for kt in range(nk):
                        b3, b4 = bufs[kt]
                        kb.act(pt[b4][:], psS[b3][:], AF.Exp, r=["psS%d" % b3], w=["pt%d" % b4])
                        if kt >= BAND * G and MODE != "seq":
                            kb.tt("pool", pt[b4][:], pt[b4][:], amask[:, kt - BAND * G, :], ALU.mult, r=["pt%d" % b4, "amask"], w=["pt%d" % b4])
                        kb.mm(pos_[:], Vh[:, kt, :], pt[b4][:], start=(kt == 0), stop=(kt == nk - 1),
                              r=["V%d" % hb, "pt%d" % b4], w=["po%d" % qs])
                        if kt + 3 < nk:
                            qk(kt + 3)
                        if kt == 0 and gi_ + 1 < len(groups):
                            nh, nG = groups[gi_ + 1]
                            if nh != h:
                                build_kv(nh)
                            build_q(nh, nG, (gi_ + 1) % 2)
                    kb.recip(rl[0:64, :], pos_[64:128, :], r=["po%d" % qs], w=["rl"])
                    kb.tt("dve", oT[qs][0:64, :], pos_[0:64, :], rl[0:64, :], ALU.mult, r=["po%d" % qs, "rl"], w=["oT%d" % qs])
                    kb.dma("sp", o_dram[h * 64:(h + 1) * 64, G * 512:(G + 1) * 512], oT[qs][0:64, :], r=["oT%d" % qs], w=[],
                           stream="sto", K=2)
                S.flush()
        if attn_only:
            return
        with contextlib.ExitStack() as ph:
            T = tail_setup(ph, i, 0)
            R = router_setup(ph, i)
            wo = kb.sb(ph, "wo", [128, 8, D], BF16)
            kb.dma("pool", wo[:], W["w_o", i].rearrange("(c p) n -> p c n", p=128), w=["wo"], stream="c5", K=1)
            oTt = [kb.sb(ph, "oTt", [128, 8, 512], BF16) for _ in range(2)]
            xt = [kb.sb(ph, "xt", [128, D]) for _ in range(2)]
            u = [kb.sb(ph, "u", [128, 2, D]) for _ in range(2)]
            x1 = [kb.sb(ph, "x1", [128, D]) for _ in range(2)]
            st = [dict(junk=kb.sb(ph, "junk", [128, D]), s12=kb.sb(ph, "s12", [128, 2]), sm=kb.sb(ph, "sm", [128, 8])) for _ in range(2)]
            pao = [kb.ps(ph, "pao", [128, D]) for _ in range(2)]
            o4 = o_dram.rearrange("(c p) t -> p c t", p=128)

            def load(m):
                s = m % 2
                if m % 4 == 0:
                    gq = (m // 4) % 2
                    kb.dma("sp", oTt[gq][:], o4[:, :, m * 128:(m + 4) * 128], r=["o_dram"], w=["oTt%d" % gq], stream="ldo", K=2)
                kb.dma("sp", xt[s][:], x_src[m * 128:(m + 1) * 128, :], w=["xt%d" % s], stream="ldx", K=2)

            def front(m):
                s = m % 2
                gq = (m // 4) % 2
                for half in range(2):
                    for c in range(8):
                        kb.mm(pao[s][:, half * 512:(half + 1) * 512], oTt[gq][:, c, (m % 4) * 128:(m % 4 + 1) * 128], wo[:, c, half * 512:(half + 1) * 512],
                              start=(c == 0), stop=(c == 7), r=["oTt%d" % gq, "wo"], w=["pao%d" % s])
                kb.stt("dve", u[s][:, 0, :], xt[s][:], ALPHA, pao[s][:], ALU.mult, ALU.add, r=["xt%d" % s, "pao%d" % s], w=["u%d" % s])
                layernorm(st[s], u[s], x1[s][:], T["lng_b"][:], T["lnb_b"][:], "a%d" % s, "u%d" % s, "x1%d" % s)
                kb.dma("sp", x1_dram[m * 128:(m + 1) * 128, :], x1[s][:], r=["x1%d" % s], w=[], stream="stx1", K=2)

            load(0)
            for m in range(NT):
                s = m % 2
                if m + 1 < NT:
                    load(m + 1)
                a = S.capture(lambda: front(m))
                if m >= 1:
                    b = S.capture(lambda: router_tile(R, m - 1, x1[1 - s][:], "x1%d" % (1 - s), RI, RG))
                    S.emit_zipped(a, b)
                else:
                    S.emit_zipped(a)
            router_tile(R, NT - 1, x1[(NT - 1) % 2][:], "x1%d" % ((NT - 1) % 2), RI, RG)
            S.flush()

    def phase_pool(i, x_src, RI, RG):
        with contextlib.ExitStack() as ph:
            T = tail_setup(ph, i, 0)
            R = router_setup(ph, i)
            NPM = 4 * 128 * 2 + 8 * 4 * 128
            pmat = kb.sb(ph, "pmat", [128, NPM], BF16)
            for a_ in range(NPM // 1024):
                kb.dma("pool", pmat[:, a_ * 1024:(a_ + 1) * 1024], pmat_d[:, a_ * 1024:(a_ + 1) * 1024], w=["pmat"], stream="c5", K=1)
            Mdiag = pmat[:, 0:512].rearrange("p (g t) -> p g t", g=4)
            Mfirst = pmat[:, 512:1024].rearrange("p (g t) -> p g t", g=4)
            Mhalo = pmat[:, 1024:NPM].rearrange("p (j g t) -> p j g t", j=8, g=4)
            pwb = kb.sb(ph, "pwb", [128, 8, 256], BF16)
            kb.dma("pool", pwb[:], W["pw", i].rearrange("g (cc p) d -> p (g cc) d", p=128), w=["pwb"], stream="c6", K=1)
            pb_b = kb.sb(ph, "pb_b", [128, D])
            psc_b = kb.sb(ph, "psc_b", [128, D])
            kb.dma("sp", pb_b[:], W["pb", i].partition_broadcast(128), w=["pb"], stream="c7", K=1)
            kb.dma("sp", psc_b[:], W["psc", i].partition_broadcast(128), w=["psc"], stream="c8", K=1)
            hb8 = [kb.sb(ph, "hb8", [128, D], BF16) for _ in range(2)]
            kb.memset("pool", hb8[0][:], 0.0, w=["hb80"])
            xt = [kb.sb(ph, "xt", [128, D]) for _ in range(2)]
            xb = [kb.sb(ph, "xb", [128, D], BF16) for _ in range(2)]
            plT = [kb.sb(ph, "plT", [128, 8, 128], BF16) for _ in range(2)]
            f = [kb.sb(ph, "f", [128, D]) for _ in range(2)]
            u = [kb.sb(ph, "u", [128, 2, D]) for _ in range(2)]
            x1 = [kb.sb(ph, "x1", [128, D]) for _ in range(2)]
            st = [dict(junk=kb.sb(ph, "junk", [128, D]), s12=kb.sb(ph, "s12", [128, 2]), sm=kb.sb(ph, "sm", [128, 8])) for _ in range(2)]
            pp = kb.ps(ph, "pp", [128, 8, 128])
            pyy = kb.ps(ph, "pyy", [128, D])

            def load(m):
                s = m % 2
                if m % 8 == 0:
                    hq = (m // 8) % 2
                    if MODE == "pair":
                        kb.dma("pool", hb8[hq][:], W["halo", i][m // 8], w=["hb8%d" % hq], stream="ldh", K=2)
                    else:
                        for j_ in range(8):
                            if m + j_ == 0:
                                continue
                            r0 = (m + j_) * 128 - 16
                            kb.dma("pool", hb8[hq][16 * j_:16 * j_ + 16, :], x_src[r0:r0 + 16, :], w=["hb8%d" % hq], stream="ldh%d" % j_, K=2)
                kb.dma("sp", xt[s][:], x_src[m * 128:(m + 1) * 128, :], w=["xt%d" % s], stream="ldx", K=2)

            def front(m):
                s = m % 2
                hq = (m // 8) % 2
                kb.cp("act", xb[s][:], xt[s][:], r=["xt%d" % s], w=["xb%d" % s])
                Md = Mfirst if m == 0 else Mdiag
                for fc in range(8):
                    gi = fc // 2
                    kb.mm(pp[:, fc, :], xb[s][:, fc * 128:(fc + 1) * 128], Md[:, gi, :], start=True, stop=False, r=["xb%d" % s, "pmat"], w=["pp"])
                    kb.mm(pp[:, fc, :], hb8[hq][:, fc * 128:(fc + 1) * 128], Mhalo[:, m % 8, gi, :], start=False, stop=True,
                          r=["hb8%d" % hq, "pmat"], w=["pp"])
                kb.cp("dve", plT[s][:], pp[:], r=["pp"], w=["plT%d" % s])
                for gi in range(4):
                    for cc in range(2):
                        kb.mm(pyy[:, gi * 256:(gi + 1) * 256], plT[s][:, 2 * gi + cc, :], pwb[:, 2 * gi + cc, :], start=(cc == 0), stop=(cc == 1),
                              r=["plT%d" % s, "pwb"], w=["pyy"])
                kb.tt("dve", f[s][:], pyy[:], pb_b[:], ALU.add, r=["pyy", "pb"], w=["f%d" % s])
                kb.tt("dve", f[s][:], f[s][:], psc_b[:], ALU.mult, r=["f%d" % s, "psc"], w=["f%d" % s])
                kb.stt("dve", u[s][:, 0, :], xt[s][:], ALPHA, f[s][:], ALU.mult, ALU.add, r=["xt%d" % s, "f%d" % s], w=["u%d" % s])
                layernorm(st[s], u[s], x1[s][:], T["lng_b"][:], T["lnb_b"][:], "a%d" % s, "u%d" % s, "x1%d" % s)
                kb.dma("sp", x1_dram[m * 128:(m + 1) * 128, :], x1[s][:], r=["x1%d" % s], w=[], stream="stx1", K=2)

            load(0)
            for m in range(NT):
                s = m % 2
                if m + 1 < NT:
                    load(m + 1)
                a = S.capture(lambda: front(m))
                if m >= 1:
                    b = S.capture(lambda: router_tile(R, m - 1, x1[1 - s][:], "x1%d" % (1 - s), RI, RG))
                    S.emit_zipped(a, b)
                else:
                    S.emit_zipped(a)
            router_tile(R, NT - 1, x1[(NT - 1) % 2][:], "x1%d" % ((NT - 1) % 2), RI, RG)
            S.flush()

    x_cur = x_in
    nstep = 0
    for kind, i in steps:
        if kind == "P1":
            phase_P1(i, x_cur)
        elif kind == "ATTO":
            phase_att(i, x_cur, None, None, attn_only=True)
        elif kind == "EXPO":
            phase_experts(i)
        else:
            nstep += 1
            last = (kind, i) == [s_ for s_ in steps if s_[0] not in ("P1", "ATTO", "EXPO")][-1]
            x_dst = outs["x_out"] if (last and "x_out" in outs) else (xa_dram if nstep % 2 else xb_dram)
            with contextlib.ExitStack() as pr:
                RI = kb.sb(pr, "RI", [128, NT, 2], I32)
                RG = kb.sb(pr, "RG", [128, NT, 2])
                if kind == "ATT":
                    phase_att(i, x_cur, RI, RG)
                else:
                    phase_pool(i, x_cur, RI, RG)
                if DBG.get('stop') != 'P3':
                    phase_experts(i)
                if DBG.get('stop') not in ('P3', 'EXP'):
                    phase_combine(i, RI, RG, x_dst)
            x_cur = x_dst
    g.close()
    es.close()
    return nc


def _consts():
    ident = np.eye(128, dtype=np.float32)
    ones = np.ones((128, 128), np.float32)
    ustrict = np.triu(np.ones((128, 128), np.float32), 1)
    return np.concatenate([ident, ones, ustrict], axis=1), ident


def _pool_mats(r):
    wins = (2, 4, 8, 16)
    Mdiag = np.zeros((128, 4, 128), np.float32)
    Mfirst = np.zeros((128, 4, 128), np.float32)
    Mh = np.zeros((16, 4, 128), np.float32)
    for gi, w in enumerate(wins):
        for t in range(128):
            for s_ in range(t - w + 1, t + 1):
                if s_ >= 0:
                    Mdiag[s_, gi, t] += 1.0 / w
                else:
                    Mh[16 + s_, gi, t] += 1.0 / w
            cnt = min(t + 1, w)
            for s_ in range(max(t - w + 1, 0), t + 1):
                Mfirst[s_, gi, t] += 1.0 / cnt
            Mdiag[t, gi, t] -= 1.0
            Mfirst[t, gi, t] -= 1.0
    if r == 1:
        Mfirst = Mdiag.copy()
    Mhalo = np.zeros((128, 8, 4, 128), np.float32)
    for j in range(8):
        Mhalo[16 * j:16 * j + 16, j] = Mh
    return np.concatenate([Mdiag.reshape(128, -1), Mfirst.reshape(128, -1), Mhalo.reshape(128, -1)], axis=1)


def _amask_seq():
    m = np.zeros((128, 4, 512), np.float32)
    kp = np.arange(128)[:, None]
    qp = np.arange(128)[None, :]
    for kk in range(4):
        for j in range(4):
            if kk < j:
                m[:, kk, j * 128:(j + 1) * 128] = 1.0
            elif kk == j:
                m[:, kk, j * 128:(j + 1) * 128] = (kp <= qp).astype(np.float32)
    return (m - 1.0) * 30000.0


def _amask(r):
    m = np.zeros((128, 8, 512), np.float32)
    kp = np.arange(128)[:, None]
    for kk in range(8):
        for j in range(4):
            qp = np.arange(128)[None, :]
            d = 2 * j + r
            if kk < d:
                blk = np.ones((128, 128), np.float32)
            elif kk == d:
                blk = (kp <= qp).astype(np.float32)
            else:
                blk = np.zeros((128, 128), np.float32)
            m[:, kk, j * 128:(j + 1) * 128] = blk
    return m


def _pmajor(w):
    w = np.asarray(w, np.float32)
    return np.ascontiguousarray(w.reshape(NE, 8, 128, 256).transpose(0, 2, 1, 3)).reshape(NE, 128, 2048)


_PROG_CACHE = {}


def _get_prog(steps, want):
    key = (tuple(steps), tuple(sorted(want)))
    if key not in _PROG_CACHE:
        _PROG_CACHE[key] = build_program(list(steps), set(want))
    return _PROG_CACHE[key]


def _own_rows(a, b, r):
    t = a[b].reshape(64, 128, *a.shape[2:])
    return np.ascontiguousarray(t[r::2].reshape(TOK, *a.shape[2:]))


def kernel_pair(x, positions, ln_g, ln_b, mla_w_in, mla_q_norm, mla_w_uq, mla_kv_norm, mla_w_ukv, mla_w_o,
           pool_w, pool_b, pool_scale, moe_w_grp, moe_b_grp, moe_w_exp, moe_b_exp, moe_w1, moe_w3, moe_w2, _nlayers=DEPTH):
    set_mode("pair")
    f32 = np.float32
    x = np.asarray(x, f32)
    positions = np.asarray(positions, np.int32)
    cst, ident = _consts()
    invf = (10000.0 ** (-np.arange(0, 32, 2, dtype=f32) / f32(32))).astype(f32)
    invf_b = np.ascontiguousarray(np.broadcast_to(invf[None, :], (128, 16)))
    ecap = np.ascontiguousarray(np.broadcast_to((np.arange(NE, dtype=f32) * CAP)[None, :], (128, NE)))
    cores = [(b, r) for b in range(4) for r in range(2)]

    def common(b, r):
        return {"cst": cst, "identf": ident}

    def mla_w(j, att):
        wuq = np.asarray(mla_w_uq[j], f32)
        wr_ = wuq.reshape(256, NH, 96).copy()
        wr_[:, :, 64:80] = wuq.reshape(256, NH, 96)[:, :, 80:96]
        wr_[:, :, 80:96] = wuq.reshape(256, NH, 96)[:, :, 64:80]
        i = 2 * j
        d = {"w_in%d" % i: np.asarray(mla_w_in[j], f32), "w_uq%d" % i: wuq, "w_uqr%d" % i: wr_.reshape(256, 1536),
             "w_ukv%d" % i: np.asarray(mla_w_ukv[j], f32), "qn%d" % i: np.asarray(mla_q_norm[j], f32),
             "kvn%d" % i: np.asarray(mla_kv_norm[j], f32)}
        if att:
            d["w_o%d" % i] = np.asarray(mla_w_o[j], f32)
        return d

    def moe_w(i):
        return {"lng%d" % i: np.asarray(ln_g[i], f32), "lnb%d" % i: np.asarray(ln_b[i], f32),
                "wr%d" % i: np.ascontiguousarray(np.concatenate([moe_w_grp[i], moe_w_exp[i]], axis=1).astype(f32)),
                "br%d" % i: np.concatenate([moe_b_grp[i], moe_b_exp[i]]).astype(f32),
                "w1_%d" % i: np.asarray(moe_w1[i], f32), "w3_%d" % i: np.asarray(moe_w3[i], f32), "w2_%d" % i: np.asarray(moe_w2[i], f32),
                "ecap": ecap}

    def pos_in(b, r):
        p = _own_rows(positions[:, :, None], b, r)[:, 0]
        return {"pos": np.ascontiguousarray(p.reshape(NT, 128).T), "invf": invf_b}

    def run(steps, want, maps):
        nc = _get_prog(steps, want)
        res = run_bass_kernel_spmd(nc, maps, core_ids=list(range(8)))
        return res.results

    xs = [_own_rows(x, b, r) for (b, r) in cores]
    for i in range(_nlayers):
        j = i // 2
        if i % 2 == 0:
            maps = []
            for ci, (b, r) in enumerate(cores):
                d = {"x_in": xs[ci]}
                d.update(common(b, r)); d.update(pos_in(b, r)); d.update(mla_w(j, False))
                maps.append(d)
            res = run((("P1", i),), ("kv_own",), maps)
            kvo = [np.asarray(res[ci]["kv_own"]) for ci in range(8)]
            maps = []
            for ci, (b, r) in enumerate(cores):
                d = {"x_in": xs[ci]}
                d.update(common(b, r)); d.update(pos_in(b, r)); d.update(mla_w(j, True)); d.update(moe_w(i))
                d["kv_gath%d" % i] = np.stack([kvo[2 * b], kvo[2 * b + 1]], axis=0)
                d["amask"] = _amask(r)
                maps.append(d)
            res = run((("P1", i), ("ATT", i)), ("x_out",), maps)
        else:
            maps = []
            for ci, (b, r) in enumerate(cores):
                d = {"x_in": xs[ci]}
                d.update(common(b, r)); d.update(moe_w(i))
                d["pw%d" % i] = np.asarray(pool_w[j], f32)
                d["pb%d" % i] = np.asarray(pool_b[j], f32)
                d["psc%d" % i] = np.asarray(pool_scale[j], f32)
                d["pmat"] = _pool_mats(r)
                part = xs[2 * b + (1 - r)].reshape(NT, 128, D)[:, 112:128, :]
                halo = np.zeros((NT, 16, D), f32)
                if r == 1:
                    halo[:] = part
                else:
                    halo[1:] = part[:-1]
                d["halo%d" % i] = np.ascontiguousarray(halo.reshape(4, 128, D))
                maps.append(d)
            res = run((("POOL", i),), ("x_out",), maps)
        xs = [np.asarray(res[ci]["x_out"]) for ci in range(8)]
    out = np.zeros((4, 64, 128, D), f32)
    for ci, (b, r) in enumerate(cores):
        out[b, r::2] = xs[ci].reshape(NT, 128, D)
    return out.reshape(4, 8192, D)


def kernel(x, positions, ln_g, ln_b, mla_w_in, mla_q_norm, mla_w_uq, mla_kv_norm, mla_w_ukv, mla_w_o,
           pool_w, pool_b, pool_scale, moe_w_grp, moe_b_grp, moe_w_exp, moe_b_exp, moe_w1, moe_w3, moe_w2, _nlayers=DEPTH):
    set_mode("seq")
    f32 = np.float32
    x = np.asarray(x, f32)
    positions = np.asarray(positions, np.int32)
    cst, ident = _consts()
    invf = (10000.0 ** (-np.arange(0, 32, 2, dtype=f32) / f32(32))).astype(f32)
    invf_b = np.ascontiguousarray(np.broadcast_to(invf[None, :], (128, 16)))
    ecap = np.ascontiguousarray(np.broadcast_to((np.arange(NE, dtype=f32) * CAP)[None, :], (128, NE)))
    shared = {"cst": cst, "identf": ident, "invf": invf_b, "ecap": ecap, "amask": _amask_seq(), "pmat": _pool_mats(0)}
    steps = []
    for i in range(_nlayers):
        j = i // 2
        if i % 2 == 0:
            steps += [("P1", i), ("ATT", i)]
            wuq = np.asarray(mla_w_uq[j], f32)
            wr_ = wuq.reshape(256, NH, 96).copy()
            wr_[:, :, 64:80] = wuq.reshape(256, NH, 96)[:, :, 80:96]
            wr_[:, :, 80:96] = wuq.reshape(256, NH, 96)[:, :, 64:80]
            shared.update({"w_in%d" % i: np.asarray(mla_w_in[j], f32), "w_uq%d" % i: wuq, "w_uqr%d" % i: wr_.reshape(256, 1536),
                           "w_ukv%d" % i: np.asarray(mla_w_ukv[j], f32), "qn%d" % i: np.asarray(mla_q_norm[j], f32),
                           "kvn%d" % i: np.asarray(mla_kv_norm[j], f32), "w_o%d" % i: np.asarray(mla_w_o[j], f32)})
        else:
            steps += [("POOL", i)]
            shared.update({"pw%d" % i: np.asarray(pool_w[j], f32), "pb%d" % i: np.asarray(pool_b[j], f32),
                           "psc%d" % i: np.asarray(pool_scale[j], f32)})
        shared.update({"lng%d" % i: np.asarray(ln_g[i], f32), "lnb%d" % i: np.asarray(ln_b[i], f32),
                       "wr%d" % i: np.ascontiguousarray(np.concatenate([moe_w_grp[i], moe_w_exp[i]], axis=1).astype(f32)),
                       "br%d" % i: np.concatenate([moe_b_grp[i], moe_b_exp[i]]).astype(f32),
                       "w1_%d" % i: _pmajor(moe_w1[i]), "w3_%d" % i: _pmajor(moe_w3[i]),
                       "w2_%d" % i: np.asarray(moe_w2[i], f32)})
    if _nlayers < 2:
        shared.pop("pmat")
    maps = []
    for c in range(8):
        b = c % 4
        d = dict(shared)
        d["x_in"] = np.ascontiguousarray(x[b])
        d["pos"] = np.ascontiguousarray(positions[b].reshape(NT, 128).T)
        maps.append(d)
    nc = _get_prog(tuple(steps), ("x_out",))
    res = run_bass_kernel_spmd(nc, maps, core_ids=list(range(8)))
    return np.stack([np.asarray(res.results[b]["x_out"]) for b in range(4)], axis=0)
```

```python
import contextlib
import numpy as np
import concourse.bass as bass
import concourse.mybir as mybir
from concourse.bass_utils import run_bass_kernel_spmd

F32 = mybir.dt.float32
BF16 = mybir.dt.bfloat16
I32 = mybir.dt.int32
AF = mybir.ActivationFunctionType
ALU = mybir.AluOpType
AX = mybir.AxisListType

ENGS = ("pe", "act", "dve", "pool", "sp")

D = 1024
NH = 16
DEPTH = 4
NE = 64
MODE = "seq"
NT = 64
TOK = NT * 128
CAP = 384
NKV = 64
BAND = 4


def set_mode(mode):
    global MODE, NT, TOK, CAP, BAND
    MODE = mode
    NT = 64 if mode == "seq" else 32
    TOK = NT * 128
    CAP = 384 if mode == "seq" else 256
    BAND = 4 if mode == "seq" else 8
DBG = {}
ALPHA = float((2 * DEPTH) ** 0.25)
SCALE = float(96 ** -0.5)
LN_EPS = 1e-5
RMS_EPS = 1e-6
TWO_PI = float(2 * np.pi)
C1 = 6.28125
C2 = float(2 * np.pi - 6.28125)


class _Op:
    __slots__ = ("eng", "fn", "reads", "writes", "dma", "idx", "waits", "sig", "count", "dsem", "dval", "deps", "wdeps")

    def __init__(self, eng, fn, reads, writes, dma):
        self.eng, self.fn, self.reads, self.writes, self.dma = eng, fn, tuple(reads), tuple(writes), dma
        self.sig = False
        self.count = 0
        self.dsem = None
        self.dval = 0
        self.waits = []
        self.deps = ()


class Sched:
    def __init__(self, nc, es):
        self.nc = nc
        self.ops = []
        self.cnt = {e: 0 for e in ENGS}
        self.stream_n = {}
        self.stream_sems = {}
        self.eng_sem = {e: es.enter_context(nc.semaphore("s_" + e)) for e in ENGS}
        self.sem_pool = [es.enter_context(nc.semaphore("dsem%d" % k)) for k in range(90)]
        self.es = es
        self.nblocks = 0

    def op(self, eng, fn, reads=(), writes=()):
        o = _Op(eng, fn, reads, writes, None)
        self.ops.append(o)
        return o

    def dma(self, eng, fn, reads=(), writes=(), stream="d", K=2):
        o = _Op(eng, fn, reads, writes, (eng + "_" + stream, K))
        self.ops.append(o)
        return o

    def capture(self, fn):
        n0 = len(self.ops)
        fn()
        got = self.ops[n0:]
        del self.ops[n0:]
        return got

    def emit_zipped(self, *lists):
        its = [list(l) for l in lists]
        total = sum(len(l) for l in its)
        pos = [0] * len(its)
        while sum(pos) < total:
            best, bi = None, -1
            for i, l in enumerate(its):
                if pos[i] < len(l):
                    frac = pos[i] / max(1, len(l))
                    if best is None or frac < best:
                        best, bi = frac, i
            self.ops.append(its[bi][pos[bi]])
            pos[bi] += 1

    def flush(self):
        nc = self.nc
        ops = self.ops
        self.ops = []
        if not ops:
            return
        for i, o in enumerate(ops):
            o.idx = i
        last_w, rd_cmp, rd_dma, hist = {}, {}, {}, {}
        for o in ops:
            deps = set()
            wdeps = set()
            for k in o.reads:
                if k in last_w:
                    wdeps.add(last_w[k])
            for k in o.writes:
                if k in last_w:
                    wdeps.add(last_w[k])
                d = rd_cmp.get(k)
                if d:
                    deps.update(d.values())
                l = rd_dma.get(k)
                if l:
                    deps.update(l)
            deps.update(wdeps)
            if o.dma is not None:
                st, K = o.dma
                n = self.stream_n.get(st, 0)
                self.stream_n[st] = n + 1
                h = hist.setdefault(st, {})
                if (n % K) in h:
                    deps.add(h[n % K])
                h[n % K] = o.idx
                o.dsem = (st, n % K)
                o.dval = 16 * (n // K + 1)
                if o.dsem not in self.stream_sems:
                    self.stream_sems[o.dsem] = self.sem_pool.pop()
            for k in o.reads:
                if o.dma is not None:
                    rd_dma.setdefault(k, []).append(o.idx)
                else:
                    rd_cmp.setdefault(k, {})[o.eng] = o.idx
            for k in o.writes:
                last_w[k] = o.idx
                rd_cmp[k] = {}
                rd_dma[k] = []
            deps.discard(o.idx)
            wdeps.discard(o.idx)
            o.deps = deps
            o.wdeps = wdeps
        fin = _Op("sp", None, (), (), None)
        fin.idx = len(ops)
        fin.deps = set(i for h in hist.values() for i in h.values())
        fin.wdeps = set()
        ops.append(fin)
        waited = {e: {f: -1 for f in ENGS} for e in ENGS}
        waited_dma = {e: set() for e in ENGS}
        for o in ops:
            best = {}
            for i in sorted(o.deps):
                p = ops[i]
                if p.dma is not None:
                    if i not in waited_dma[o.eng]:
                        waited_dma[o.eng].add(i)
                        o.waits.append(("dma", i))
                else:
                    if p.eng == o.eng and o.eng == "pe":
                        continue
                    if i > waited[o.eng][p.eng] and i > best.get(p.eng, -1):
                        best[p.eng] = i
            for f, i in best.items():
                waited[o.eng][f] = i
                ops[i].sig = True
                o.waits.append(("eng", i))
        for o in ops:
            if o.sig:
                self.cnt[o.eng] += 1
                o.count = self.cnt[o.eng]
        eng_sem, stream_sems = self.eng_sem, self.stream_sems
        self.nblocks += 1
        with nc.Block() as block:
            def run(ename):
                def body(eng):
                    for o in ops:
                        if o.eng != ename:
                            continue
                        for kind, i in o.waits:
                            p = ops[i]
                            if kind == "dma":
                                eng.wait_ge(stream_sems[p.dsem], p.dval)
                            else:
                                eng.wait_ge(eng_sem[p.eng], p.count)
                        if o.fn is None:
                            continue
                        ins = o.fn(eng)
                        if o.dma is not None:
                            ins.then_inc(stream_sems[o.dsem], 16)
                        elif o.sig:
                            ins.then_inc(eng_sem[o.eng], 1)
                return body
            block.tensor(run("pe"))
            block.scalar(run("act"))
            block.vector(run("dve"))
            block.gpsimd(run("pool"))
            block.sync(run("sp"))


class _View:
    def __init__(self, t, n, dims):
        self.t, self.n, self.dims = t, n, dims

    def __getitem__(self, key):
        v = self.t[:, 0:self.n]
        if self.dims is not None:
            v = v.rearrange("p (a b) -> p a b", a=self.dims[0])
        return v[key]


class KB:
    def __init__(self, nc, es):
        self.nc = nc
        self.es = es
        self.S = Sched(nc, es)
        self.uid = 0

    def sb(self, stack, name, shape, dt=F32):
        self.uid += 1
        return stack.enter_context(self.nc.sbuf_tensor("%s_%d" % (name, self.uid), shape, dt))

    def ps(self, stack, name, shape, dt=F32):
        self.uid += 1
        esz = 4 if dt == F32 else 2
        n = int(np.prod(shape[1:]))
        per_bank = 2048 // esz
        npad = (n + per_bank - 1) // per_bank * per_bank
        t = stack.enter_context(self.nc.psum_tensor("%s_%d" % (name, self.uid), [128, npad], dt))
        if len(shape) == 2:
            return _View(t, n, None)
        return _View(t, n, shape[1:])

    def mm(self, out, lhsT, rhs, start=True, stop=True, r=(), w=()):
        self.S.op("pe", lambda e: e.matmul(out, lhsT=lhsT, rhs=rhs, start=start, stop=stop), r, w)

    def tr(self, out, in_, ident, r=(), w=()):
        self.S.op("pe", lambda e: e.transpose(out=out, in_=in_, identity=ident), r, w)

    def act(self, out, in_, func, r=(), w=(), bias=None, scale=None, accum_out=None):
        kw = {}
        if bias is not None:
            kw["bias"] = bias
        if scale is not None:
            kw["scale"] = scale
        if accum_out is not None:
            kw["accum_out"] = accum_out
        self.S.op("act", lambda e: e.activation(out=out, in_=in_, func=func, **kw), r, w)

    def cp(self, eng, out, in_, r=(), w=()):
        if eng == "act":
            self.S.op("act", lambda e: e.copy(out=out, in_=in_), r, w)
        else:
            self.S.op(eng, lambda e: e.tensor_copy(out=out, in_=in_), r, w)

    def tt(self, eng, out, in0, in1, op, r=(), w=()):
        self.S.op(eng, lambda e: e.tensor_tensor(out=out, in0=in0, in1=in1, op=op), r, w)

    def ts(self, eng, out, in0, s1, op0, s2=None, op1=None, r=(), w=(), accum_out=None):
        kw = {}
        if op1 is not None:
            kw["op1"] = op1
        if accum_out is not None:
            kw["accum_out"] = accum_out
        self.S.op(eng, lambda e: e.tensor_scalar(out=out, in0=in0, scalar1=s1, scalar2=s2, op0=op0, **kw), r, w)

    def stt(self, eng, out, in0, scalar, in1, op0, op1, r=(), w=(), accum_out=None):
        kw = {}
        if accum_out is not None:
            kw["accum_out"] = accum_out
        self.S.op(eng, lambda e: e.scalar_tensor_tensor(out=out, in0=in0, scalar=scalar, in1=in1, op0=op0, op1=op1, **kw), r, w)

    def red(self, eng, out, in_, op, r=(), w=()):
        self.S.op(eng, lambda e: e.tensor_reduce(out=out, in_=in_, axis=AX.X, op=op), r, w)

    def recip(self, out, in_, r=(), w=()):
        self.S.op("dve", lambda e: e.reciprocal(out=out, in_=in_), r, w)

    def memset(self, eng, ap, val, w=()):
        self.S.op(eng, lambda e: e.memset(ap, val), (), w)

    def dma(self, eng, out, in_, r=(), w=(), stream="d", K=2):
        self.S.dma(eng, lambda e: e.dma_start(out=out, in_=in_), r, w, stream, K)

    def gather(self, out, src, idx, r=(), w=(), stream="g", K=2):
        self.S.dma("pool", lambda e: e.indirect_dma_start(out=out, out_offset=None, in_=src,
                                                          in_offset=bass.IndirectOffsetOnAxis(ap=idx, axis=0)), r, w, stream, K)

    def scatter(self, dst, in_, idx, r=(), w=(), stream="sc", K=2):
        self.S.dma("pool", lambda e: e.indirect_dma_start(out=dst, out_offset=bass.IndirectOffsetOnAxis(ap=idx, axis=0),
                                                          in_=in_, in_offset=None), r, w, stream, K)


def build_program(steps, want):
    nc = bass.Bass("TRN2", target_bir_lowering=False)
    es = contextlib.ExitStack()
    kb = KB(nc, es)
    S = kb.S
    layers = sorted(set(i for _, i in steps))
    mla_layers = sorted(set(i for k, i in steps if k in ("P1", "ATT", "ATTO")))
    pool_layers = sorted(set(i for k, i in steps if k == "POOL"))
    moe_layers = sorted(set(i for k, i in steps if k in ("ATT", "POOL", "EXPO")))

    def din(name, shape, dt=F32):
        return nc.dram_tensor(name, list(shape), dt, kind="ExternalInput").ap()

    def dout(name, shape, dt=F32):
        return nc.dram_tensor(name, list(shape), dt, kind="ExternalOutput").ap()

    def dint(name, shape, dt=F32):
        return nc.dram_tensor(name, list(shape), dt, kind="Internal").ap()

    x_in = din("x_in", [TOK, D])
    cst = din("cst", [128, 3 * 128])
    identf_d = din("identf", [128, 128])
    W = {}
    if mla_layers:
        pos_d = din("pos", [128, NT], I32)
        invf_d = din("invf", [128, 16])
    for i in mla_layers:
        W["w_in", i] = din("w_in%d" % i, [D, 416])
        W["w_uq", i] = din("w_uq%d" % i, [256, 1536])
        W["w_uqr", i] = din("w_uqr%d" % i, [256, 1536])
        W["w_ukv", i] = din("w_ukv%d" % i, [128, 2048])
        W["qn", i] = din("qn%d" % i, [256])
        W["kvn", i] = din("kvn%d" % i, [128])
        if ("ATT", i) in steps or ("ATTO", i) in steps:
            W["w_o", i] = din("w_o%d" % i, [D, D])
            if MODE == "pair":
                W["kv_gath", i] = din("kv_gath%d" % i, [2, 2, 128, TOK], BF16)
    if any(k in ("ATT", "ATTO") for k, _ in steps):
        amask_d = din("amask", [128, BAND, 512])
    for i in pool_layers:
        W["pw", i] = din("pw%d" % i, [4, 256, 256])
        W["pb", i] = din("pb%d" % i, [D])
        W["psc", i] = din("psc%d" % i, [D])
        if MODE == "pair":
            W["halo", i] = din("halo%d" % i, [4, 128, D])
    if pool_layers:
        pmat_d = din("pmat", [128, 4 * 128 * 2 + 8 * 4 * 128])
    for i in moe_layers:
        W["lng", i] = din("lng%d" % i, [2, D])
        W["lnb", i] = din("lnb%d" % i, [2, D])
        W["wr", i] = din("wr%d" % i, [D, 72])
        W["br", i] = din("br%d" % i, [72])
        W["w1", i] = din("w1_%d" % i, [NE, 128, 2048])
        W["w3", i] = din("w3_%d" % i, [NE, 128, 2048])
        W["w2", i] = din("w2_%d" % i, [NE, 256, D])
    if moe_layers:
        ecap_d = din("ecap", [128, NE])
    outs = {}
    if "kv_own" in want:
        outs["kv_own"] = dout("kv_own", [2, 128, TOK], BF16)
    else:
        outs["kv_own"] = dint("kv_own_i", [2, 128, TOK], BF16)
    if "x_out" in want:
        outs["x_out"] = dout("x_out", [TOK, D])
    if "dbg" in want:
        outs["dbg"] = dout("dbg", [TOK, 8])
    if moe_layers:
        x1_dram = dint("x1_dram", [TOK, D])
        rows_dram = din("rows_in", [NE * CAP, D], BF16) if "y_out" in want else dint("rows_dram", [NE * CAP, D], BF16)
        y_dram = dout("y_out", [NE * CAP, D]) if "y_out" in want else dint("y_dram", [NE * CAP, D])
        xa_dram = dint("xa_dram", [TOK, D])
        xb_dram = dint("xb_dram", [TOK, D])
    if any(k in ("ATT", "ATTO") for k, _ in steps):
        o_dram = dout("o_out", [D, TOK], BF16) if "o_out" in want else dint("o_dram", [D, TOK], BF16)

    g = contextlib.ExitStack()
    cst_bf = kb.sb(g, "cst_bf", [128, 3 * 128], BF16)
    ident_bf = cst_bf[:, 0:128]
    ones_bf = cst_bf[:, 128:256]
    ustrict_bf = cst_bf[:, 256:384]
    ident_f = kb.sb(g, "ident_f", [128, 128])
    kb.dma("pool", cst_bf[:], cst, w=["cst"], stream="c0", K=1)
    kb.dma("sp", ident_f[:], identf_d, w=["identf"], stream="c1", K=1)
    with contextlib.ExitStack() as z0:
        if moe_layers and "y_out" not in want:
            zt = kb.sb(z0, "zt", [128, 4096], BF16)
            kb.memset("pool", zt[:], 0.0, w=["zt"])
            rz = rows_dram.rearrange("(n p f) d -> n p (f d)", p=128, f=4)
            for n_ in range(NE * CAP // 512):
                kb.dma("sp", rz[n_], zt[:], r=["zt"], w=[], stream="rz", K=4)
        S.flush()

    if mla_layers:
        ccs = kb.sb(g, "ccs", [128, NT, 32])
        ssn = kb.sb(g, "ssn", [128, NT, 32])
        cosT = kb.sb(g, "cosT", [128, TOK], BF16)
        sinT = kb.sb(g, "sinT", [128, TOK], BF16)
        cqT = kb.sb(g, "cqT", [128, 2, TOK], BF16)
        with contextlib.ExitStack() as ph:
            posi = kb.sb(ph, "posi", [128, NT], I32)
            posf = kb.sb(ph, "posf", [128, NT])
            invf = kb.sb(ph, "invf", [128, 16])
            ang = kb.sb(ph, "ang", [128, NT, 16])
            yk = kb.sb(ph, "yk", [128, NT * 16])
            ki = kb.sb(ph, "ki", [128, NT * 16], I32)
            kf = kb.sb(ph, "kf", [128, NT * 16])
            rr = kb.sb(ph, "rr", [128, NT * 16])
            sres = kb.sb(ph, "sres", [128, NT, 16])
            cres = kb.sb(ph, "cres", [128, NT, 16])
            stg = kb.sb(ph, "stg", [128, 2, 96])
            ptab = kb.ps(ph, "ptab", [128, 4, 128])
            kb.dma("sp", posi[:], pos_d, w=["posi"], stream="c0", K=1)
            kb.dma("sp", invf[:], invf_d, w=["invf"], stream="c1", K=1)
            kb.cp("dve", posf[:], posi[:], r=["posi"], w=["posf"])
            for m in range(NT):
                kb.ts("dve", ang[:, m, :], invf[:], posf[:, m:m + 1], ALU.mult, r=["posf", "invf"], w=["ang"])
            angf = ang[:].rearrange("p m i -> p (m i)")

            kb.ts("dve", yk[:], angf, 1.0 / TWO_PI, ALU.mult, r=["ang"], w=["yk"])
            kb.cp("dve", ki[:], yk[:], r=["yk"], w=["ki"])
            kb.cp("dve", kf[:], ki[:], r=["ki"], w=["kf"])
            kb.stt("dve", rr[:], kf[:], -C1, angf, ALU.mult, ALU.add, r=["kf", "ang"], w=["rr"])
            kb.stt("dve", rr[:], kf[:], -C2, rr[:], ALU.mult, ALU.add, r=["kf", "rr"], w=["rr"])
            kb.ts("dve", rr[:], rr[:], 3.14159, ALU.min, -3.14159, ALU.max, r=["rr"], w=["rr"])
            kb.act(sres[:].rearrange("p m i -> p (m i)"), rr[:], AF.Sin, r=["rr"], w=["sres"])
            kb.stt("dve", yk[:], rr[:], -1.0, rr[:], ALU.mult, ALU.max, r=["rr"], w=["yk"])
            kb.ts("dve", yk[:], yk[:], -1.0, ALU.mult, float(np.pi / 2), ALU.add, r=["yk"], w=["yk"])
            kb.act(cres[:].rearrange("p m i -> p (m i)"), yk[:], AF.Sin, r=["yk"], w=["cres"])
            kb.cp("dve", ccs[:, :, 0:16], cres[:], r=["cres"], w=["ccs"])
            kb.cp("dve", ccs[:, :, 16:32], cres[:], r=["cres"], w=["ccs"])
            kb.ts("dve", ssn[:, :, 0:16], sres[:], -1.0, ALU.mult, r=["sres"], w=["ssn"])
            kb.cp("dve", ssn[:, :, 16:32], sres[:], r=["sres"], w=["ssn"])
            kb.memset("pool", stg[:], 0.0, w=["stg0", "stg1"])
            for tab, dstT, nm in ((ccs, cosT, "cosT"), (ssn, sinT, "sinT")):
                for m4 in range(NT // 4):
                    for j in range(4):
                        m = m4 * 4 + j
                        s = m % 2
                        kb.ts("dve", stg[:, s, 64:96], tab[:, m, :], SCALE, ALU.mult, r=["ccs", "ssn"], w=["stg%d" % s])
                        kb.tr(ptab[0:96, j, :], stg[:, s, :], ident_f[:], r=["stg%d" % s, "identf"], w=["ptab"])
                    kb.cp("act", dstT[64:96, m4 * 512:(m4 + 1) * 512],
                          ptab[64:96, :, :].rearrange("p a b -> p (a b)"), r=["ptab"], w=[nm])
            S.flush()

    def layernorm(st, uu, xo, lng_b, lnb_b, tag, ukey, wkey):
        junk, s12, sm = st["junk"], st["s12"], st["sm"]
        u = uu[:, 0, :]
        kb.act(uu[:, 1, :], u, AF.Square, r=[ukey], w=[tag + "usq"])
        kb.red("dve", s12[:], uu[:], ALU.add, r=[ukey, tag + "usq"], w=[tag + "sm"])
        kb.ts("dve", sm[:, 0:2], s12[:], 1.0 / D, ALU.mult, r=[tag + "sm"], w=[tag + "sm"])
        kb.tt("dve", sm[:, 2:3], sm[:, 0:1], sm[:, 0:1], ALU.mult, r=[tag + "sm"], w=[tag + "sm"])
        kb.stt("dve", sm[:, 3:4], sm[:, 1:2], LN_EPS, sm[:, 2:3], ALU.add, ALU.subtract, r=[tag + "sm"], w=[tag + "sm"])
        kb.act(sm[:, 4:5], sm[:, 3:4], AF.Ln, r=[tag + "sm"], w=[tag + "sm"])
        kb.act(sm[:, 5:6], sm[:, 4:5], AF.Exp, r=[tag + "sm"], w=[tag + "sm"], scale=-0.5)
        kb.stt("dve", sm[:, 6:7], sm[:, 0:1], -1.0, sm[:, 5:6], ALU.mult, ALU.mult, r=[tag + "sm"], w=[tag + "sm"])
        kb.act(junk[:], u, AF.Identity, r=[ukey, tag + "sm"], w=[tag + "junk"], bias=sm[:, 6:7], scale=sm[:, 5:6])
        kb.tt("dve", junk[:], junk[:], lng_b, ALU.mult, r=[tag + "junk", "lnw"], w=[tag + "junk"])
        kb.tt("dve", xo, junk[:], lnb_b, ALU.add, r=[tag + "junk", "lnw"], w=[wkey])

    def phase_P1(i, x_src):
        with contextlib.ExitStack() as ph:
            w_in_bf = kb.sb(ph, "w_in_bf", [128, 8, 416], BF16)
            qn_b = kb.sb(ph, "qn_b", [128, 256])
            kvn_b = kb.sb(ph, "kvn_b", [128, 128])
            xt = [kb.sb(ph, "xt", [128, D]) for _ in range(2)]
            xb = [kb.sb(ph, "xb", [128, D], BF16) for _ in range(2)]
            xT = [kb.sb(ph, "xT", [128, 8, 128], BF16) for _ in range(2)]
            kvt = [kb.sb(ph, "kvt", [128, 160]) for _ in range(2)]
            cqn = [kb.sb(ph, "cqn", [128, 256], BF16) for _ in range(2)]
            junk = kb.sb(ph, "junk", [128, 384])
            ssq = [kb.sb(ph, "ssq", [128, 8]) for _ in range(2)]
            t1 = [kb.sb(ph, "t1", [128, 32]) for _ in range(2)]
            t2 = [kb.sb(ph, "t2", [128, 32]) for _ in range(2)]
            pT = [kb.ps(ph, "pT", [128, 8, 128], BF16) for _ in range(2)]
            plat = [kb.ps(ph, "plat", [128, 416]) for _ in range(2)]
            pcq = kb.ps(ph, "pcq", [128, 2, 128], BF16)
            pkvT = kb.ps(ph, "pkvT", [128, 2, 128], BF16)
            kvtb = [kb.sb(ph, "kvtb", [128, 160], BF16) for _ in range(2)]
            kvTs = [kb.sb(ph, "kvTs", [128, 2, 128], BF16) for _ in range(2)]
            kb.memset("pool", kvTs[0][:], 0.0, w=["kvTs0"])
            kb.memset("pool", kvTs[1][:], 0.0, w=["kvTs1"])
            epst = kb.sb(ph, "epst", [128, 1])
            kb.memset("pool", epst[:], RMS_EPS, w=["epst"])
            kb.dma("pool", w_in_bf[:], W["w_in", i].rearrange("(c p) n -> p c n", p=128), w=["w_in"], stream="c0", K=1)
            kb.dma("sp", qn_b[:], W["qn", i].partition_broadcast(128), w=["qn"], stream="c1", K=1)
            kb.dma("sp", kvn_b[:], W["kvn", i].partition_broadcast(128), w=["kvn"], stream="c2", K=1)

            def load(m):
                s = m % 2
                kb.dma("sp", xt[s][:], x_src[m * 128:(m + 1) * 128, :], w=["xt%d" % s], stream="ldx", K=2)

            load(0)
            for m in range(NT):
                s = m % 2
                if m + 1 < NT:
                    load(m + 1)
                X, XB, XT, PT, PL = "xt%d" % s, "xb%d" % s, "xT%d" % s, "pT%d" % s, "plat%d" % s
                kb.cp("act", xb[s][:], xt[s][:], r=[X], w=[XB])
                for c in range(8):
                    kb.tr(pT[s][:, c, :], xb[s][:, c * 128:(c + 1) * 128], ident_bf, r=[XB, "cst"], w=[PT])
                kb.cp("dve", xT[s][:], pT[s][:], r=[PT], w=[XT])
                for c in range(8):
                    kb.mm(plat[s][:], xT[s][:, c, :], w_in_bf[:, c, :], start=(c == 0), stop=(c == 7), r=[XT, "w_in"], w=[PL])
                kb.act(junk[:, 0:384], plat[s][:, 0:384], AF.Square, r=[PL], w=["junk"])
                kb.red("dve", ssq[s][:, 4:7], junk[:, 0:384].rearrange("p (a b) -> p a b", a=3), ALU.add, r=["junk"], w=["ssq%d" % s])
                kb.tt("dve", ssq[s][:, 0:1], ssq[s][:, 4:5], ssq[s][:, 5:6], ALU.add, r=["ssq%d" % s], w=["ssq%d" % s])
                kb.cp("dve", ssq[s][:, 1:2], ssq[s][:, 6:7], r=["ssq%d" % s], w=["ssq%d" % s])
                kb.act(ssq[s][:, 2:3], ssq[s][:, 0:1], AF.Ln, r=["ssq%d" % s, "epst"], w=["ssq%d" % s], bias=epst[:, 0:1], scale=1.0 / 256)
                kb.act(ssq[s][:, 3:4], ssq[s][:, 1:2], AF.Ln, r=["ssq%d" % s, "epst"], w=["ssq%d" % s], bias=epst[:, 0:1], scale=1.0 / 128)
                kb.act(ssq[s][:, 2:4], ssq[s][:, 2:4], AF.Exp, r=["ssq%d" % s], w=["ssq%d" % s], scale=-0.5)
                kb.stt("dve", cqn[s][:], plat[s][:, 0:256], ssq[s][:, 2:3], qn_b[:], ALU.mult, ALU.mult,
                       r=[PL, "ssq%d" % s, "qn"], w=["cqn%d" % s])
                kb.stt("dve", kvt[s][:, 0:128], plat[s][:, 256:384], ssq[s][:, 3:4], kvn_b[:], ALU.mult, ALU.mult,
                       r=[PL, "ssq%d" % s, "kvn"], w=["kvt%d" % s])
                kb.tt("dve", t1[s][:], plat[s][:, 384:416], ccs[:, m, :], ALU.mult, r=[PL, "ccs", "junk"], w=["t1%d" % s])
                kb.tt("dve", t2[s][:, 0:16], plat[s][:, 400:416], ssn[:, m, 0:16], ALU.mult, r=[PL, "ssn"], w=["t2%d" % s])
                kb.tt("dve", t2[s][:, 16:32], plat[s][:, 384:400], ssn[:, m, 16:32], ALU.mult, r=[PL, "ssn"], w=["t2%d" % s])
                kb.tt("dve", kvt[s][:, 128:160], t1[s][:], t2[s][:], ALU.add, r=["t1%d" % s, "t2%d" % s], w=["kvt%d" % s])
                for cc in range(2):
                    kb.tr(pcq[:, cc, :], cqn[s][:, cc * 128:(cc + 1) * 128], ident_bf, r=["cqn%d" % s, "cst"], w=["pcq"])
                kb.cp("act", cqT[:, :, m * 128:(m + 1) * 128], pcq[:], r=["pcq"], w=["cqT"])
                kb.cp("dve", kvtb[s][:], kvt[s][:], r=["kvt%d" % s], w=["kvtb%d" % s])
                kb.tr(pkvT[:, 0, :], kvtb[s][:, 0:128], ident_bf, r=["kvtb%d" % s, "cst"], w=["pkvT"])
                kb.tr(pkvT[0:96, 1, :], kvtb[s][:, 64:160], ident_bf, r=["kvtb%d" % s, "cst"], w=["pkvT"])
                kb.cp("act", kvTs[s][:, 0, :], pkvT[:, 0, :], r=["pkvT"], w=["kvTs%d" % s])
                kb.cp("act", kvTs[s][64:96, 1, :], pkvT[64:96, 1, :], r=["pkvT"], w=["kvTs%d" % s])
                kb.dma("sp", outs["kv_own"][:, :, m * 128:(m + 1) * 128].rearrange("a p t -> p a t"), kvTs[s][:],
                       r=["kvTs%d" % s], w=[], stream="stkv", K=2)
            S.flush()

    def tail_setup(ph, i, which):
        T = {}
        T["lng_b"] = kb.sb(ph, "lng_b", [128, D])
        T["lnb_b"] = kb.sb(ph, "lnb_b", [128, D])
        kb.dma("sp", T["lng_b"][:], W["lng", i][which, :].partition_broadcast(128), w=["lnw"], stream="c0", K=1)
        kb.dma("sp", T["lnb_b"][:], W["lnb", i][which, :].partition_broadcast(128), w=["lnw"], stream="c1", K=1)
        return T

    def router_setup(ph, i):
        R = {}
        R["wr"] = kb.sb(ph, "wr", [128, 8, 72])
        R["br_b"] = kb.sb(ph, "br_b", [128, 72])
        R["ecap"] = kb.sb(ph, "ecap", [128, NE])
        R["cnt"] = kb.sb(ph, "cnt", [128, NE])
        kb.dma("sp", R["wr"][:], W["wr", i].rearrange("(c p) n -> p c n", p=128), w=["wr"], stream="c2", K=1)
        kb.dma("sp", R["br_b"][:], W["br", i].partition_broadcast(128), w=["br"], stream="c3", K=1)
        kb.dma("sp", R["ecap"][:], ecap_d, w=["ecap"], stream="c4", K=1)
        kb.memset("pool", R["cnt"][:], 0.0, w=["cnt"])
        R["x1T"] = kb.sb(ph, "x1T", [128, 8, 128])
        R["lg"] = kb.sb(ph, "lg", [128, 72])
        R["sm"] = kb.sb(ph, "rsm", [128, 16])
        R["goh"] = kb.sb(ph, "goh", [128, 8])
        R["gd"] = kb.sb(ph, "gd", [128, 8])
        R["tmp64"] = kb.sb(ph, "tmp64", [128, 8, 8])
        R["els"] = kb.sb(ph, "els", [128, 8])
        R["els2"] = kb.sb(ph, "els2", [128, 8])
        R["oh"] = [kb.sb(ph, "oh", [128, 8]) for _ in range(2)]
        R["oh64"] = [kb.sb(ph, "oh64", [128, 8, 8]) for _ in range(2)]
        R["A"] = kb.sb(ph, "A", [128, NE], BF16)
        R["pos"] = kb.sb(ph, "pos", [128, NE])
        R["destf"] = kb.sb(ph, "destf", [128, 2])
        R["xbf"] = [kb.sb(ph, "xbf", [128, D], BF16) for _ in range(2)]
        R["pxT"] = kb.ps(ph, "pxT", [128, 8, 128])
        R["plg"] = kb.ps(ph, "plg", [128, 256])
        return R

    def router_tile(R, m, x1, x1key, RI, RG):
        s = m % 2
        for c in range(8):
            kb.tr(R["pxT"][:, c, :], x1[:, c * 128:(c + 1) * 128], ident_f[:], r=[x1key, "identf"], w=["pxT"])
        kb.cp("dve", R["x1T"][:, 0:4, :], R["pxT"][:, 0:4, :], r=["pxT"], w=["x1Ta"])
        kb.cp("act", R["x1T"][:, 4:8, :], R["pxT"][:, 4:8, :], r=["pxT"], w=["x1Tb"])
        for c in range(8):
            kb.mm(R["plg"][:, 0:72], R["x1T"][:, c, :], R["wr"][:, c, :], start=(c == 0), stop=(c == 7),
                  r=["x1Ta", "x1Tb", "wr"], w=["plg"])
        lg, sm = R["lg"], R["sm"]
        kb.tt("dve", lg[:], R["plg"][:, 0:72], R["br_b"][:], ALU.add, r=["plg", "br"], w=["lg"])
        kb.red("dve", sm[:, 0:1], lg[:, 0:8], ALU.max, r=["lg"], w=["rsm"])
        kb.ts("dve", R["goh"][:], lg[:, 0:8], sm[:, 0:1], ALU.is_equal, r=["lg", "rsm"], w=["goh"])
        kb.ts("dve", R["gd"][:], lg[:, 0:8], sm[:, 0:1], ALU.subtract, r=["lg", "rsm"], w=["gd"])
        kb.act(R["gd"][:], R["gd"][:], AF.Exp, r=["gd"], w=["gd"])
        kb.red("dve", sm[:, 1:2], R["gd"][:], ALU.add, r=["gd"], w=["rsm"])
        kb.recip(sm[:, 2:3], sm[:, 1:2], r=["rsm"], w=["rsm"])
        kb.tt("dve", R["tmp64"][:], lg[:, 8:72].rearrange("p (g j) -> p g j", g=8),
              R["goh"][:].unsqueeze(2).to_broadcast([128, 8, 8]), ALU.mult, r=["lg", "goh"], w=["tmp64"])
        kb.red("dve", R["els"][:], R["tmp64"][:].rearrange("p g j -> p j g"), ALU.add, r=["tmp64"], w=["els"])
        kb.red("dve", sm[:, 3:4], R["els"][:], ALU.max, r=["els"], w=["rsm"])
        kb.ts("dve", R["oh"][0][:], R["els"][:], sm[:, 3:4], ALU.is_equal, r=["els", "rsm"], w=["oh0"])
        kb.stt("dve", R["els2"][:], R["oh"][0][:], -1e30, R["els"][:], ALU.mult, ALU.add, r=["oh0", "els"], w=["els2"])
        kb.red("dve", sm[:, 4:5], R["els2"][:], ALU.max, r=["els2"], w=["rsm"])
        kb.ts("dve", R["oh"][1][:], R["els2"][:], sm[:, 4:5], ALU.is_equal, r=["els2", "rsm"], w=["oh1"])
        kb.tt("dve", sm[:, 5:6], sm[:, 4:5], sm[:, 3:4], ALU.subtract, r=["rsm"], w=["rsm"])
        kb.act(sm[:, 6:7], sm[:, 5:6], AF.Exp, r=["rsm"], w=["rsm"])
        kb.ts("dve", sm[:, 7:8], sm[:, 6:7], 1.0, ALU.add, r=["rsm"], w=["rsm"])
        kb.recip(sm[:, 8:9], sm[:, 7:8], r=["rsm"], w=["rsm"])
        kb.tt("dve", RG[:, m, 0:1], sm[:, 2:3], sm[:, 8:9], ALU.mult, r=["rsm"], w=["RG"])
        kb.tt("dve", RG[:, m, 1:2], RG[:, m, 0:1], sm[:, 6:7], ALU.mult, r=["rsm", "RG"], w=["RG"])
        for k in range(2):
            kb.tt("dve", R["oh64"][k][:], R["goh"][:].unsqueeze(2).to_broadcast([128, 8, 8]),
                  R["oh"][k][:].unsqueeze(1).to_broadcast([128, 8, 8]), ALU.mult, r=["goh", "oh%d" % k], w=["oh64%d" % k])
        kb.tt("dve", R["A"][:], R["oh64"][0][:].rearrange("p g j -> p (g j)"), R["oh64"][1][:].rearrange("p g j -> p (g j)"),
              ALU.add, r=["oh640", "oh641"], w=["A"])
        kb.mm(R["plg"][:, 128:192], ustrict_bf, R["A"][:], r=["A", "cst"], w=["ppos"])
        kb.mm(R["plg"][:, 192:256], ones_bf, R["A"][:], r=["A", "cst"], w=["ppos"])
        kb.tt("dve", R["pos"][:], R["plg"][:, 128:192], R["cnt"][:], ALU.add, r=["ppos", "cnt"], w=["pos"])
        kb.tt("dve", R["cnt"][:], R["cnt"][:], R["plg"][:, 192:256], ALU.add, r=["ppos", "cnt"], w=["cnt"])
        kb.ts("dve", R["pos"][:], R["pos"][:], float(CAP - 1), ALU.min, r=["pos"], w=["pos"])
        kb.tt("dve", R["pos"][:], R["pos"][:], R["ecap"][:], ALU.add, r=["pos", "ecap"], w=["pos"])
        for k in range(2):
            kb.tt("dve", R["oh64"][k][:].rearrange("p g j -> p (g j)"), R["oh64"][k][:].rearrange("p g j -> p (g j)"),
                  R["pos"][:], ALU.mult, r=["oh64%d" % k, "pos"], w=["oh64%d" % k])
            kb.red("dve", R["destf"][:, k:k + 1], R["oh64"][k][:].rearrange("p g j -> p (g j)"), ALU.add,
                   r=["oh64%d" % k], w=["destf"])
        kb.cp("dve", RI[:, m, :], R["destf"][:], r=["destf"], w=["RI"])
        kb.cp("act", R["xbf"][s][:], x1, r=[x1key], w=["xbf%d" % s])
        for k in range(2):
            kb.scatter(rows_dram, R["xbf"][s][:], RI[:, m, k:k + 1], r=["xbf%d" % s, "RI"], w=[], stream="sc%d" % k, K=2)

    def phase_experts(i):
        NSB = CAP // 128
        with contextlib.ExitStack() as ph:
            w1b = [kb.sb(ph, "w1b", [128, 8, 256], BF16) for _ in range(2)]
            w3b = [kb.sb(ph, "w3b", [128, 8, 256], BF16) for _ in range(2)]
            w2b = [kb.sb(ph, "w2b", [128, 2, D], BF16) for _ in range(2)]
            rows = [kb.sb(ph, "rows", [128, NSB, D], BF16) for _ in range(2)]
            xgT = [kb.sb(ph, "xgT", [128, 8, 128], BF16) for _ in range(2)]
            sl = [kb.sb(ph, "sl", [128, 2, 128]) for _ in range(2)]
            hT = [kb.sb(ph, "hT", [128, 2, 128], BF16) for _ in range(2)]
            ys = [kb.sb(ph, "ys", [128, D]) for _ in range(3)]
            pT = [kb.ps(ph, "pT", [128, 8, 128], BF16) for _ in range(2)]
            phh = [kb.ps(ph, "phh", [128, 4, 128]) for _ in range(2)]
            py = [kb.ps(ph, "py", [128, D]) for _ in range(2)]

            def load(e):
                s = e % 2
                for hh_ in range(2):
                    kb.dma("pool", w1b[s][:, 4 * hh_:4 * hh_ + 4, :].rearrange("p c f -> p (c f)"), W["w1", i][e][:, 1024 * hh_:1024 * hh_ + 1024],
                           w=["w13a%d" % s], stream="w1%d" % hh_, K=2)
                    kb.dma("pool", w3b[s][:, 4 * hh_:4 * hh_ + 4, :].rearrange("p c f -> p (c f)"), W["w3", i][e][:, 1024 * hh_:1024 * hh_ + 1024],
                           w=["w13b%d" % s], stream="w3%d" % hh_, K=2)
                kb.dma("pool", w2b[s][:], W["w2", i][e].rearrange("(c p) n -> p c n", p=128), w=["w2b%d" % s], stream="w2", K=2)
                kb.dma("sp", rows[s][:], rows_dram[e * CAP:(e + 1) * CAP, :].rearrange("(sb p) d -> p sb d", p=128),
                       w=["rows%d" % s], stream="ldr", K=2)

            load(0)
            blk = 0
            NER = DBG.get('ne', NE)
            for e in range(NER):
                s = e % 2
                if e + 1 < NER:
                    load(e + 1)
                for sbk in range(NSB):
                    b2 = blk % 2
                    b3 = blk % 3
                    blk += 1
                    for c in range(8):
                        kb.tr(pT[b2][:, c, :], rows[s][:, sbk, c * 128:(c + 1) * 128], ident_bf, r=["rows%d" % s, "cst"], w=["pT%d" % b2])
                    kb.cp("dve" if b2 == 0 else "act", xgT[b2][:], pT[b2][:], r=["pT%d" % b2], w=["xgTa%d" % b2, "xgTb%d" % b2])
                    for fi in range(4):
                        for c in range(8):
                            wsrc = w1b[s] if fi < 2 else w3b[s]
                            kb.mm(phh[b2][:, fi, :], wsrc[:, c, (fi % 2) * 128:(fi % 2 + 1) * 128], xgT[b2][:, c, :], start=(c == 0), stop=(c == 7),
                                  r=["w13a%d" % s, "w13b%d" % s, "xgTa%d" % b2, "xgTb%d" % b2], w=["phh%d" % b2])
                    kb.act(sl[b2][:], phh[b2][:, 0:2, :], AF.Silu, r=["phh%d" % b2], w=["sl%d" % b2])
                    kb.tt("dve", hT[b2][:], sl[b2][:], phh[b2][:, 2:4, :], ALU.mult, r=["sl%d" % b2, "phh%d" % b2], w=["hT%d" % b2])
                    for half in range(2):
                        for fc in range(2):
                            kb.mm(py[b2][:, half * 512:(half + 1) * 512], hT[b2][:, fc, :],
                                  w2b[s][:, fc, half * 512:(half + 1) * 512], start=(fc == 0), stop=(fc == 1),
                                  r=["hT%d" % b2, "w2b%d" % s], w=["py%d" % b2])
                    kb.cp("act", ys[b3][:, 0:512], py[b2][:, 0:512], r=["py%d" % b2], w=["ysa%d" % b3])
                    kb.cp("dve", ys[b3][:, 512:1024], py[b2][:, 512:1024], r=["py%d" % b2], w=["ysb%d" % b3])
                    kb.dma("sp", y_dram[e * CAP + sbk * 128: e * CAP + (sbk + 1) * 128, :], ys[b3][:], r=["ysa%d" % b3, "ysb%d" % b3], w=[],
                           stream="sty", K=3)
            S.flush()

    def phase_combine(i, RI, RG, x_dst):
        NB = 4
        with contextlib.ExitStack() as ph:
            T = tail_setup(ph, i, 1)
            y0 = [kb.sb(ph, "y0", [128, D]) for _ in range(NB)]
            y1 = [kb.sb(ph, "y1", [128, D]) for _ in range(NB)]
            x1t = [kb.sb(ph, "x1t", [128, D]) for _ in range(NB)]
            u = [kb.sb(ph, "u", [128, 2, D]) for _ in range(NB)]
            st = [dict(junk=kb.sb(ph, "junk", [128, D]), s12=kb.sb(ph, "s12", [128, 2]), sm=kb.sb(ph, "sm", [128, 8])) for _ in range(NB)]

            def load(m):
                s = m % NB
                kb.gather(y0[s][:], y_dram, RI[:, m, 0:1], r=["RI", "y_dram"], w=["y0%d" % s], stream="g0", K=NB)
                kb.gather(y1[s][:], y_dram, RI[:, m, 1:2], r=["RI", "y_dram"], w=["y1%d" % s], stream="g1", K=NB)
                kb.dma("sp", x1t[s][:], x1_dram[m * 128:(m + 1) * 128, :], r=["x1_dram"], w=["x1t%d" % s], stream="ldxc", K=NB)

            def body(m):
                s = m % NB
                kb.act(u[s][:, 0, :], x1t[s][:], AF.Copy, r=["x1t%d" % s], w=["u%d" % s], scale=ALPHA)
                kb.stt("dve", u[s][:, 0, :], y0[s][:], RG[:, m, 0:1], u[s][:, 0, :], ALU.mult, ALU.add, r=["y0%d" % s, "u%d" % s, "RG"], w=["u%d" % s])
                kb.stt("dve", u[s][:, 0, :], y1[s][:], RG[:, m, 1:2], u[s][:, 0, :], ALU.mult, ALU.add, r=["y1%d" % s, "u%d" % s, "RG"], w=["u%d" % s])
                layernorm(st[s], u[s], st[s]["junk"][:], T["lng_b"][:], T["lnb_b"][:], "c%d" % s, "u%d" % s, "c%djunk" % s)
                kb.dma("sp", x_dst[m * 128:(m + 1) * 128, :], st[s]["junk"][:], r=["c%djunk" % s], w=[], stream="stx", K=NB)

            load(0)
            load(1)
            for m in range(0, NT, 2):
                for mm in (m + 2, m + 3):
                    if mm < NT:
                        load(mm)
                a = S.capture(lambda: body(m))
                b = S.capture(lambda: body(m + 1))
                S.emit_zipped(a, b)
            S.flush()

    def phase_att(i, x_src, RI, RG, attn_only=False):
        with contextlib.ExitStack() as pa:
            ckvT = kb.sb(pa, "ckvT", [128, 8192], BF16)
            KT = [kb.sb(pa, "KT", [128, 8192], BF16) for _ in range(2)]
            V = [kb.sb(pa, "V", [128, 64, 128], BF16) for _ in range(2)]
            wuq = kb.sb(pa, "wuq", [128, 2, 1536], BF16)
            wuqr = kb.sb(pa, "wuqr", [128, 2, 1536], BF16)
            wukv = kb.sb(pa, "wukv", [128, 2048], BF16)
            amask = kb.sb(pa, "amask", [128, BAND, 512], BF16)
            sk = DBG.get('skip', '')
            if 'q' not in sk:
                kb.dma("pool", wuq[:], W["w_uq", i].rearrange("(c p) n -> p c n", p=128), w=["wuq"], stream="c0", K=1)
                kb.dma("pool", wuqr[:], W["w_uqr", i].rearrange("(c p) n -> p c n", p=128), w=["wuqr"], stream="c1", K=1)
            if 'k' not in sk:
                for a_ in range(2):
                    kb.dma("pool", wukv[:, a_ * 1024:(a_ + 1) * 1024], W["w_ukv", i][:, a_ * 1024:(a_ + 1) * 1024], w=["wukv"], stream="c2", K=1)
            if 'a' not in sk:
                for a_ in range(BAND // 2):
                    kb.dma("pool", amask[:, 2 * a_:2 * a_ + 2, :], amask_d[:, 2 * a_:2 * a_ + 2, :], w=["amask"], stream="c3", K=1)
            if 'm' not in sk:
                kb.memset("pool", V[0][:, :, 64:128], 1.0, w=["V0"])
                kb.memset("pool", V[1][:, :, 64:128], 1.0, w=["V1"])
            if MODE == "pair":
                kvg = W["kv_gath", i]
                for r_ in range(2):
                    kb.dma("sp", ckvT[:].rearrange("p (lt r t) -> p lt r t", r=2, t=128)[:, :, r_, :],
                           kvg[r_, 0].rearrange("p (lt t) -> p lt t", t=128), w=["ckvT"], stream="ldkv%d" % r_, K=1)
                    for hb_ in range(2):
                        kb.dma("sp", KT[hb_][64:96, :].rearrange("p (lt r t) -> p lt r t", r=2, t=128)[:, :, r_, :],
                               kvg[r_, 1, 64:96, :].rearrange("p (lt t) -> p lt t", t=128), w=["KT%dr" % hb_], stream="ldkr%d%d" % (r_, hb_), K=1)
            else:
                kb.dma("sp", ckvT[:], outs["kv_own"][0], w=["ckvT"], stream="ldkv0", K=1)
                for hb_ in range(2):
                    kb.dma("sp", KT[hb_][64:96, :], outs["kv_own"][1, 64:96, :], w=["KT%dr" % hb_], stream="ldkr0%d" % hb_, K=1)
            S.flush()
            if DBG.get('stop') == 'P2a':
                return
            with contextlib.ExitStack() as ph:
                qT = [kb.sb(ph, "qT", [128, 512], BF16) for _ in range(2)]
                tq1 = kb.sb(ph, "tq1", [128, 512])
                tq2 = kb.sb(ph, "tq2", [128, 512])
                pt = [kb.sb(ph, "pt", [128, 512], BF16) for _ in range(5)]
                rl = kb.sb(ph, "rl", [128, 512])
                oT = [kb.sb(ph, "oT", [128, 512], BF16) for _ in range(2)]
                psS = [kb.ps(ph, "psS", [128, 512]) for _ in range(4)]
                po = [kb.ps(ph, "po", [128, 512]) for _ in range(2)]
                pqA = kb.ps(ph, "pqA", [128, 512])
                pqB = kb.ps(ph, "pqB", [128, 512])
                pkv = pqB
                def build_kv(h):
                    hb = h % 2
                    KTh, Vh = KT[hb], V[hb]
                    for kc in range(16):
                        kb.mm(pkv[0:64, :], wukv[:, h * 128:h * 128 + 64], ckvT[:, kc * 512:(kc + 1) * 512], r=["wukv", "ckvT"], w=["pqB"])
                        kb.cp("dve", KTh[0:64, kc * 512:(kc + 1) * 512], pkv[0:64, :], r=["pqB"], w=["KT%dn" % hb])
                    for k8 in range(8):
                        for j in range(8):
                            kt = k8 * 8 + j
                            kb.mm(pkv[:, j * 64:(j + 1) * 64], ckvT[:, kt * 128:(kt + 1) * 128], wukv[:, h * 128 + 64:h * 128 + 128],
                                  r=["wukv", "ckvT"], w=["pqB"])
                        kb.cp("dve", Vh[:, k8 * 8:(k8 + 1) * 8, 0:64], pkv[:].rearrange("p (a b) -> p a b", a=8),
                              r=["pqB"], w=["V%d" % hb])

                def build_q(h, G, qs):
                    for cc in range(2):
                        kb.mm(pqA[0:96, :], wuq[:, cc, h * 96:(h + 1) * 96], cqT[:, cc, G * 512:(G + 1) * 512], start=(cc == 0), stop=(cc == 1),
                              r=["wuq", "cqT"], w=["pqA"])
                    for cc in range(2):
                        kb.mm(pqB[0:96, :], wuqr[:, cc, h * 96:(h + 1) * 96], cqT[:, cc, G * 512:(G + 1) * 512], start=(cc == 0), stop=(cc == 1),
                              r=["wuqr", "cqT"], w=["pqB"])
                    kb.ts("dve", qT[qs][0:64, :], pqA[0:64, :], SCALE, ALU.mult, r=["pqA"], w=["qTn%d" % qs])
                    kb.tt("dve", tq1[64:96, :], pqB[64:96, :], sinT[64:96, G * 512:(G + 1) * 512], ALU.mult, r=["pqB", "sinT"], w=["tq1"])
                    kb.tt("dve", tq2[64:96, :], pqA[64:96, :], cosT[64:96, G * 512:(G + 1) * 512], ALU.mult, r=["pqA", "cosT"], w=["tq2"])
                    kb.tt("dve", qT[qs][64:96, :], tq1[64:96, :], tq2[64:96, :], ALU.add, r=["tq1", "tq2"], w=["qTr%d" % qs])

                groups = [(h, G) for h in range(NH) for G in range(NT // 4)]
                blk = 0
                build_kv(0)
                build_q(0, 0, 0)
                for gi_, (h, G) in enumerate(groups):
                    hb = h % 2
                    qs = gi_ % 2
                    KTh, Vh = KT[hb], V[hb]
                    nk = BAND * G + BAND
                    pos_ = po[qs]
                    bufs = [((blk + k) % 4, (blk + k) % 5) for k in range(nk)]
                    blk += nk

                    def qk(k):
                        b3, b4 = bufs[k]
                        band = (MODE == "seq" and k >= BAND * G)
                        kb.mm(psS[b3][:], KTh[0:96, k * 128:(k + 1) * 128], qT[qs][0:96, :], start=True, stop=not band,
                              r=["KT%dn" % hb, "KT%dr" % hb, "qTn%d" % qs, "qTr%d" % qs], w=["psS%d" % b3])
                        if band:
                            kb.mm(psS[b3][:], ident_bf, amask[:, k - BAND * G, :], start=False, stop=True, r=["cst", "amask"], w=["psS%d" % b3])

                    for k_ in range(min(3, nk)):
                        qk(k_)
                    for kt in range(nk):
                        b3, b4 = bufs[kt]
                        kb.act(pt[b4][:], psS[b3][:], AF.Exp, r=["psS%d" % b3], w=["pt%d" % b4])
                        if kt >= BAND * G and MODE != "seq":
                            kb.tt("pool", pt[b4][:], pt[b4][:], amask[:, kt - BAND * G, :], ALU.mult, r=["pt%d" % b4, "amask"], w=["pt%d" % b4])
                        kb.mm(pos_[:], Vh[:, kt, :], pt[b4][:], start=(kt == 0), stop=(kt == nk - 1),
                              r=["V%d" % hb, "pt%d" % b4], w=["po%d" % qs])
                        if kt + 3 < nk:
                            qk(kt + 3)
                        if kt == 0 and gi_ + 1 < len(groups):
                            nh, nG = groups[gi_ + 1]
                            if nh != h:
                                build_kv(nh)
                            build_q(nh, nG, (gi_ + 1) % 2)
                    kb.recip(rl[0:64, :], pos_[64:128, :], r=["po%d" % qs], w=["rl"])
                    kb.tt("dve", oT[qs][0:64, :], pos_[0:64, :], rl[0:64, :], ALU.mult, r=["po%d" % qs, "rl"], w=["oT%d" % qs])
                    kb.dma("sp", o_dram[h * 64:(h + 1) * 64, G * 512:(G + 1) * 512], oT[qs][0:64, :], r=["oT%d" % qs], w=[],
                           stream="sto", K=2)
                S.flush()
        if attn_only:
            return
        with contextlib.ExitStack() as ph:
            T = tail_setup(ph, i, 0)
            R = router_setup(ph, i)
            wo = kb.sb(ph, "wo", [128, 8, D], BF16)
            kb.dma("pool", wo[:], W["w_o", i].rearrange("(c p) n -> p c n", p=128), w=["wo"], stream="c5", K=1)
            oTt = [kb.sb(ph, "oTt", [128, 8, 512], BF16) for _ in range(2)]
            xt = [kb.sb(ph, "xt", [128, D]) for _ in range(2)]
            u = [kb.sb(ph, "u", [128, 2, D]) for _ in range(2)]
            x1 = [kb.sb(ph, "x1", [128, D]) for _ in range(2)]
            st = [dict(junk=kb.sb(ph, "junk", [128, D]), s12=kb.sb(ph, "s12", [128, 2]), sm=kb.sb(ph, "sm", [128, 8])) for _ in range(2)]
            pao = [kb.ps(ph, "pao", [128, D]) for _ in range(2)]
            o4 = o_dram.rearrange("(c p) t -> p c t", p=128)

            def load(m):
                s = m % 2
                if m % 4 == 0:
                    gq = (m // 4) % 2
                    kb.dma("sp", oTt[gq][:], o4[:, :, m * 128:(m + 4) * 128], r=["o_dram"], w=["oTt%d" % gq], stream="ldo", K=2)
                kb.dma("sp", xt[s][:], x_src[m * 128:(m + 1) * 128, :], w=["xt%d" % s], stream="ldx", K=2)

            def front(m):
                s = m % 2
                gq = (m // 4) % 2
                for half in range(2):
                    for c in range(8):
                        kb.mm(pao[s][:, half * 512:(half + 1) * 512], oTt[gq][:, c, (m % 4) * 128:(m % 4 + 1) * 128], wo[:, c, half * 512:(half + 1) * 512],
                              start=(c == 0), stop=(c == 7), r=["oTt%d" % gq, "wo"], w=["pao%d" % s])
                kb.stt("dve", u[s][:, 0, :], xt[s][:], ALPHA, pao[s][:], ALU.mult, ALU.add, r=["xt%d" % s, "pao%d" % s], w=["u%d" % s])
                layernorm(st[s], u[s], x1[s][:], T["lng_b"][:], T["lnb_b"][:], "a%d" % s, "u%d" % s, "x1%d" % s)
                kb.dma("sp", x1_dram[m * 128:(m + 1) * 128, :], x1[s][:], r=["x1%d" % s], w=[], stream="stx1", K=2)

            load(0)
            for m in range(NT):
                s = m % 2
                if m + 1 < NT:
                    load(m + 1)
                a = S.capture(lambda: front(m))
                if m >= 1:
                    b = S.capture(lambda: router_tile(R, m - 1, x1[1 - s][:], "x1%d" % (1 - s), RI, RG))
                    S.emit_zipped(a, b)
                else:
                    S.emit_zipped(a)
            router_tile(R, NT - 1, x1[(NT - 1) % 2][:], "x1%d" % ((NT - 1) % 2), RI, RG)
            S.flush()

    def phase_pool(i, x_src, RI, RG):
        with contextlib.ExitStack() as ph:
            T = tail_setup(ph, i, 0)
            R = router_setup(ph, i)
            NPM = 4 * 128 * 2 + 8 * 4 * 128
            pmat = kb.sb(ph, "pmat", [128, NPM], BF16)
            for a_ in range(NPM // 1024):
                kb.dma("pool", pmat[:, a_ * 1024:(a_ + 1) * 1024], pmat_d[:, a_ * 1024:(a_ + 1) * 1024], w=["pmat"], stream="c5", K=1)
            Mdiag = pmat[:, 0:512].rearrange("p (g t) -> p g t", g=4)
            Mfirst = pmat[:, 512:1024].rearrange("p (g t) -> p g t", g=4)
            Mhalo = pmat[:, 1024:NPM].rearrange("p (j g t) -> p j g t", j=8, g=4)
            pwb = kb.sb(ph, "pwb", [128, 8, 256], BF16)
            kb.dma("pool", pwb[:], W["pw", i].rearrange("g (cc p) d -> p (g cc) d", p=128), w=["pwb"], stream="c6", K=1)
            pb_b = kb.sb(ph, "pb_b", [128, D])
            psc_b = kb.sb(ph, "psc_b", [128, D])
            kb.dma("sp", pb_b[:], W["pb", i].partition_broadcast(128), w=["pb"], stream="c7", K=1)
            kb.dma("sp", psc_b[:], W["psc", i].partition_broadcast(128), w=["psc"], stream="c8", K=1)
            hb8 = [kb.sb(ph, "hb8", [128, D], BF16) for _ in range(2)]
            kb.memset("pool", hb8[0][:], 0.0, w=["hb80"])
            xt = [kb.sb(ph, "xt", [128, D]) for _ in range(2)]
            xb = [kb.sb(ph, "xb", [128, D], BF16) for _ in range(2)]
            plT = [kb.sb(ph, "plT", [128, 8, 128], BF16) for _ in range(2)]
            f = [kb.sb(ph, "f", [128, D]) for _ in range(2)]
            u = [kb.sb(ph, "u", [128, 2, D]) for _ in range(2)]
            x1 = [kb.sb(ph, "x1", [128, D]) for _ in range(2)]
            st = [dict(junk=kb.sb(ph, "junk", [128, D]), s12=kb.sb(ph, "s12", [128, 2]), sm=kb.sb(ph, "sm", [128, 8])) for _ in range(2)]
            pp = kb.ps(ph, "pp", [128, 8, 128])
            pyy = kb.ps(ph, "pyy", [128, D])

            def load(m):
                s = m % 2
                if m % 8 == 0:
                    hq = (m // 8) % 2
                    if MODE == "pair":
                        kb.dma("pool", hb8[hq][:], W["halo", i][m // 8], w=["hb8%d" % hq], stream="ldh", K=2)
                    else:
                        for j_ in range(8):
                            if m + j_ == 0:
                                continue
                            r0 = (m + j_) * 128 - 16
                            kb.dma("pool", hb8[hq][16 * j_:16 * j_ + 16, :], x_src[r0:r0 + 16, :], w=["hb8%d" % hq], stream="ldh%d" % j_, K=2)
                kb.dma("sp", xt[s][:], x_src[m * 128:(m + 1) * 128, :], w=["xt%d" % s], stream="ldx", K=2)

            def front(m):
                s = m % 2
                hq = (m // 8) % 2
                kb.cp("act", xb[s][:], xt[s][:], r=["xt%d" % s], w=["xb%d" % s])
                Md = Mfirst if m == 0 else Mdiag
                for fc in range(8):
                    gi = fc // 2
                    kb.mm(pp[:, fc, :], xb[s][:, fc * 128:(fc + 1) * 128], Md[:, gi, :], start=True, stop=False, r=["xb%d" % s, "pmat"], w=["pp"])
                    kb.mm(pp[:, fc, :], hb8[hq][:, fc * 128:(fc + 1) * 128], Mhalo[:, m % 8, gi, :], start=False, stop=True,
                          r=["hb8%d" % hq, "pmat"], w=["pp"])
                kb.cp("dve", plT[s][:], pp[:], r=["pp"], w=["plT%d" % s])
                for gi in range(4):
                    for cc in range(2):
                        kb.mm(pyy[:, gi * 256:(gi + 1) * 256], plT[s][:, 2 * gi + cc, :], pwb[:, 2 * gi + cc, :], start=(cc == 0), stop=(cc == 1),
                              r=["plT%d" % s, "pwb"], w=["pyy"])
                kb.tt("dve", f[s][:], pyy[:], pb_b[:], ALU.add, r=["pyy", "pb"], w=["f%d" % s])
                kb.tt("dve", f[s][:], f[s][:], psc_b[:], ALU.mult, r=["f%d" % s, "psc"], w=["f%d" % s])
                kb.stt("dve", u[s][:, 0, :], xt[s][:], ALPHA, f[s][:], ALU.mult, ALU.add, r=["xt%d" % s, "f%d" % s], w=["u%d" % s])
                layernorm(st[s], u[s], x1[s][:], T["lng_b"][:], T["lnb_b"][:], "a%d" % s, "u%d" % s, "x1%d" % s)
                kb.dma("sp", x1_dram[m * 128:(m + 1) * 128, :], x1[s][:], r=["x1%d" % s], w=[], stream="stx1", K=2)

            load(0)
            for m in range(NT):
                s = m % 2
                if m + 1 < NT:
                    load(m + 1)
                a = S.capture(lambda: front(m))
                if m >= 1:
                    b = S.capture(lambda: router_tile(R, m - 1, x1[1 - s][:], "x1%d" % (1 - s), RI, RG))
                    S.emit_zipped(a, b)
                else:
                    S.emit_zipped(a)
            router_tile(R, NT - 1, x1[(NT - 1) % 2][:], "x1%d" % ((NT - 1) % 2), RI, RG)
            S.flush()

    x_cur = x_in
    nstep = 0
    for kind, i in steps:
        if kind == "P1":
            phase_P1(i, x_cur)
        elif kind == "ATTO":
            phase_att(i, x_cur, None, None, attn_only=True)
        elif kind == "EXPO":
            phase_experts(i)
        else:
            nstep += 1
            last = (kind, i) == [s_ for s_ in steps if s_[0] not in ("P1", "ATTO", "EXPO")][-1]
            x_dst = outs["x_out"] if (last and "x_out" in outs) else (xa_dram if nstep % 2 else xb_dram)
            with contextlib.ExitStack() as pr:
                RI = kb.sb(pr, "RI", [128, NT, 2], I32)
                RG = kb.sb(pr, "RG", [128, NT, 2])
                if kind == "ATT":
                    phase_att(i, x_cur, RI, RG)
                else:
                    phase_pool(i, x_cur, RI, RG)
                if DBG.get('stop') != 'P3':
                    phase_experts(i)
                if DBG.get('stop') not in ('P3', 'EXP'):
                    phase_combine(i, RI, RG, x_dst)
            x_cur = x_dst
    g.close()
    es.close()
    return nc


def _consts():
    ident = np.eye(128, dtype=np.float32)
    ones = np.ones((128, 128), np.float32)
    ustrict = np.triu(np.ones((128, 128), np.float32), 1)
    return np.concatenate([ident, ones, ustrict], axis=1), ident


def _pool_mats(r):
    wins = (2, 4, 8, 16)
    Mdiag = np.zeros((128, 4, 128), np.float32)
    Mfirst = np.zeros((128, 4, 128), np.float32)
    Mh = np.zeros((16, 4, 128), np.float32)
    for gi, w in enumerate(wins):
        for t in range(128):
            for s_ in range(t - w + 1, t + 1):
                if s_ >= 0:
                    Mdiag[s_, gi, t] += 1.0 / w
                else:
                    Mh[16 + s_, gi, t] += 1.0 / w
            cnt = min(t + 1, w)
            for s_ in range(max(t - w + 1, 0), t + 1):
                Mfirst[s_, gi, t] += 1.0 / cnt
            Mdiag[t, gi, t] -= 1.0
            Mfirst[t, gi, t] -= 1.0
    if r == 1:
        Mfirst = Mdiag.copy()
    Mhalo = np.zeros((128, 8, 4, 128), np.float32)
    for j in range(8):
        Mhalo[16 * j:16 * j + 16, j] = Mh
    return np.concatenate([Mdiag.reshape(128, -1), Mfirst.reshape(128, -1), Mhalo.reshape(128, -1)], axis=1)


def _amask_seq():
    m = np.zeros((128, 4, 512), np.float32)
    kp = np.arange(128)[:, None]
    qp = np.arange(128)[None, :]
    for kk in range(4):
        for j in range(4):
            if kk < j:
                m[:, kk, j * 128:(j + 1) * 128] = 1.0
            elif kk == j:
                m[:, kk, j * 128:(j + 1) * 128] = (kp <= qp).astype(np.float32)
    return (m - 1.0) * 30000.0


def _amask(r):
    m = np.zeros((128, 8, 512), np.float32)
    kp = np.arange(128)[:, None]
    for kk in range(8):
        for j in range(4):
            qp = np.arange(128)[None, :]
            d = 2 * j + r
            if kk < d:
                blk = np.ones((128, 128), np.float32)
            elif kk == d:
                blk = (kp <= qp).astype(np.float32)
            else:
                blk = np.zeros((128, 128), np.float32)
            m[:, kk, j * 128:(j + 1) * 128] = blk
    return m


def _pmajor(w):
    w = np.asarray(w, np.float32)
    return np.ascontiguousarray(w.reshape(NE, 8, 128, 256).transpose(0, 2, 1, 3)).reshape(NE, 128, 2048)


_PROG_CACHE = {}


def _get_prog(steps, want):
    key = (tuple(steps), tuple(sorted(want)))
    if key not in _PROG_CACHE:
        _PROG_CACHE[key] = build_program(list(steps), set(want))
    return _PROG_CACHE[key]


def _own_rows(a, b, r):
    t = a[b].reshape(64, 128, *a.shape[2:])
    return np.ascontiguousarray(t[r::2].reshape(TOK, *a.shape[2:]))


def kernel_pair(x, positions, ln_g, ln_b, mla_w_in, mla_q_norm, mla_w_uq, mla_kv_norm, mla_w_ukv, mla_w_o,
           pool_w, pool_b, pool_scale, moe_w_grp, moe_b_grp, moe_w_exp, moe_b_exp, moe_w1, moe_w3, moe_w2, _nlayers=DEPTH):
    set_mode("pair")
    f32 = np.float32
    x = np.asarray(x, f32)
    positions = np.asarray(positions, np.int32)
    cst, ident = _consts()
    invf = (10000.0 ** (-np.arange(0, 32, 2, dtype=f32) / f32(32))).astype(f32)
    invf_b = np.ascontiguousarray(np.broadcast_to(invf[None, :], (128, 16)))
    ecap = np.ascontiguousarray(np.broadcast_to((np.arange(NE, dtype=f32) * CAP)[None, :], (128, NE)))
    cores = [(b, r) for b in range(4) for r in range(2)]

    def common(b, r):
        return {"cst": cst, "identf": ident}

    def mla_w(j, att):
        wuq = np.asarray(mla_w_uq[j], f32)
        wr_ = wuq.reshape(256, NH, 96).copy()
        wr_[:, :, 64:80] = wuq.reshape(256, NH, 96)[:, :, 80:96]
        wr_[:, :, 80:96] = wuq.reshape(256, NH, 96)[:, :, 64:80]
        i = 2 * j
        d = {"w_in%d" % i: np.asarray(mla_w_in[j], f32), "w_uq%d" % i: wuq, "w_uqr%d" % i: wr_.reshape(256, 1536),
             "w_ukv%d" % i: np.asarray(mla_w_ukv[j], f32), "qn%d" % i: np.asarray(mla_q_norm[j], f32),
             "kvn%d" % i: np.asarray(mla_kv_norm[j], f32)}
        if att:
            d["w_o%d" % i] = np.asarray(mla_w_o[j], f32)
        return d

    def moe_w(i):
        return {"lng%d" % i: np.asarray(ln_g[i], f32), "lnb%d" % i: np.asarray(ln_b[i], f32),
                "wr%d" % i: np.ascontiguousarray(np.concatenate([moe_w_grp[i], moe_w_exp[i]], axis=1).astype(f32)),
                "br%d" % i: np.concatenate([moe_b_grp[i], moe_b_exp[i]]).astype(f32),
                "w1_%d" % i: np.asarray(moe_w1[i], f32), "w3_%d" % i: np.asarray(moe_w3[i], f32), "w2_%d" % i: np.asarray(moe_w2[i], f32),
                "ecap": ecap}

    def pos_in(b, r):
        p = _own_rows(positions[:, :, None], b, r)[:, 0]
        return {"pos": np.ascontiguousarray(p.reshape(NT, 128).T), "invf": invf_b}

    def run(steps, want, maps):
        nc = _get_prog(steps, want)
        res = run_bass_kernel_spmd(nc, maps, core_ids=list(range(8)))
        return res.results

    xs = [_own_rows(x, b, r) for (b, r) in cores]
    for i in range(_nlayers):
        j = i // 2
        if i % 2 == 0:
            maps = []
            for ci, (b, r) in enumerate(cores):
                d = {"x_in": xs[ci]}
                d.update(common(b, r)); d.update(pos_in(b, r)); d.update(mla_w(j, False))
                maps.append(d)
            res = run((("P1", i),), ("kv_own",), maps)
            kvo = [np.asarray(res[ci]["kv_own"]) for ci in range(8)]
            maps = []
            for ci, (b, r) in enumerate(cores):
                d = {"x_in": xs[ci]}
                d.update(common(b, r)); d.update(pos_in(b, r)); d.update(mla_w(j, True)); d.update(moe_w(i))
                d["kv_gath%d" % i] = np.stack([kvo[2 * b], kvo[2 * b + 1]], axis=0)
                d["amask"] = _amask(r)
                maps.append(d)
            res = run((("P1", i), ("ATT", i)), ("x_out",), maps)
        else:
            maps = []
            for ci, (b, r) in enumerate(cores):
                d = {"x_in": xs[ci]}
                d.update(common(b, r)); d.update(moe_w(i))
                d["pw%d" % i] = np.asarray(pool_w[j], f32)
                d["pb%d" % i] = np.asarray(pool_b[j], f32)
                d["psc%d" % i] = np.asarray(pool_scale[j], f32)
                d["pmat"] = _pool_mats(r)
                part = xs[2 * b + (1 - r)].reshape(NT, 128, D)[:, 112:128, :]
                halo = np.zeros((NT, 16, D), f32)
                if r == 1:
                    halo[:] = part
                else:
                    halo[1:] = part[:-1]
                d["halo%d" % i] = np.ascontiguousarray(halo.reshape(4, 128, D))
                maps.append(d)
            res = run((("POOL", i),), ("x_out",), maps)
        xs = [np.asarray(res[ci]["x_out"]) for ci in range(8)]
    out = np.zeros((4, 64, 128, D), f32)
    for ci, (b, r) in enumerate(cores):
        out[b, r::2] = xs[ci].reshape(NT, 128, D)
    return out.reshape(4, 8192, D)


def kernel(x, positions, ln_g, ln_b, mla_w_in, mla_q_norm, mla_w_uq, mla_kv_norm, mla_w_ukv, mla_w_o,
           pool_w, pool_b, pool_scale, moe_w_grp, moe_b_grp, moe_w_exp, moe_b_exp, moe_w1, moe_w3, moe_w2, _nlayers=DEPTH):
    set_mode("seq")
    f32 = np.float32
    x = np.asarray(x, f32)
    positions = np.asarray(positions, np.int32)
    cst, ident = _consts()
    invf = (10000.0 ** (-np.arange(0, 32, 2, dtype=f32) / f32(32))).astype(f32)
    invf_b = np.ascontiguousarray(np.broadcast_to(invf[None, :], (128, 16)))
    ecap = np.ascontiguousarray(np.broadcast_to((np.arange(NE, dtype=f32) * CAP)[None, :], (128, NE)))
    shared = {"cst": cst, "identf": ident, "invf": invf_b, "ecap": ecap, "amask": _amask_seq(), "pmat": _pool_mats(0)}
    steps = []
    for i in range(_nlayers):
        j = i // 2
        if i % 2 == 0:
            steps += [("P1", i), ("ATT", i)]
            wuq = np.asarray(mla_w_uq[j], f32)
            wr_ = wuq.reshape(256, NH, 96).copy()
            wr_[:, :, 64:80] = wuq.reshape(256, NH, 96)[:, :, 80:96]
            wr_[:, :, 80:96] = wuq.reshape(256, NH, 96)[:, :, 64:80]
            shared.update({"w_in%d" % i: np.asarray(mla_w_in[j], f32), "w_uq%d" % i: wuq, "w_uqr%d" % i: wr_.reshape(256, 1536),
                           "w_ukv%d" % i: np.asarray(mla_w_ukv[j], f32), "qn%d" % i: np.asarray(mla_q_norm[j], f32),
                           "kvn%d" % i: np.asarray(mla_kv_norm[j], f32), "w_o%d" % i: np.asarray(mla_w_o[j], f32)})
        else:
            steps += [("POOL", i)]
            shared.update({"pw%d" % i: np.asarray(pool_w[j], f32), "pb%d" % i: np.asarray(pool_b[j], f32),
                           "psc%d" % i: np.asarray(pool_scale[j], f32)})
        shared.update({"lng%d" % i: np.asarray(ln_g[i], f32), "lnb%d" % i: np.asarray(ln_b[i], f32),
                       "wr%d" % i: np.ascontiguousarray(np.concatenate([moe_w_grp[i], moe_w_exp[i]], axis=1).astype(f32)),
                       "br%d" % i: np.concatenate([moe_b_grp[i], moe_b_exp[i]]).astype(f32),
                       "w1_%d" % i: _pmajor(moe_w1[i]), "w3_%d" % i: _pmajor(moe_w3[i]),
                       "w2_%d" % i: np.asarray(moe_w2[i], f32)})
    if _nlayers < 2:
        shared.pop("pmat")
    maps = []
    for c in range(8):
        b = c % 4
        d = dict(shared)
        d["x_in"] = np.ascontiguousarray(x[b])
        d["pos"] = np.ascontiguousarray(positions[b].reshape(NT, 128).T)
        maps.append(d)
    nc = _get_prog(tuple(steps), ("x_out",))
    res = run_bass_kernel_spmd(nc, maps, core_ids=list(range(8)))
    return np.stack([np.asarray(res.results[b]["x_out"]) for b in range(4)], axis=0)
```
